# Optimizing a Trainium2 kernel written in Bass

```python
import math
import jax, jax.numpy as jnp
from jax import lax
import numpy as np

D_MODEL = 2048
BATCH = 1
SEQ = 8192
DEPTH = 1
DEC_BATCH = 4
DEC_SEQ = 4096
PAST_LEN = 128

HEAD_DIM = 128
A_HEADS = 16
A_KV = 4
A_GROUP = A_HEADS // A_KV
A_HALF = 128
A_Q = A_HEADS * HEAD_DIM
A_KVW = A_KV * HEAD_DIM
DILATED_GROUPS = ((128, 1), (512, 4), (2048, 16))
N_DIL = len(DILATED_GROUPS)
B_HPG = 4
B_HEADS = N_DIL * B_HPG
B_W = B_HEADS * HEAD_DIM
B_OUT = B_HPG * HEAD_DIM
IN_COLS = A_Q + 2 * A_KVW + 3 * B_W
SPLITS = (A_Q, A_Q + A_KVW, A_Q + 2 * A_KVW, A_Q + 2 * A_KVW + B_W, A_Q + 2 * A_KVW + 2 * B_W)
N_GROUPS = 8
EXPERTS_PER_GROUP = 8
N_EXPERTS = N_GROUPS * EXPERTS_PER_GROUP
TOP_K = 2
D_FF = 512
ROW_BLOCK = 128
EPS = 1e-6

kernel_name = "hybrid_window_dilated_hmoe_encoder"


def rms_norm(x, g):
    x32 = x.astype(jnp.float32)
    y = x32 * lax.rsqrt(jnp.mean(x32 * x32, axis=-1, keepdims=True) + EPS)
    return (y * g.astype(jnp.float32)).astype(x.dtype)


def alibi_slopes(n):
    return jnp.asarray(np.power(2.0, -8.0 * (np.arange(n) + 1) / n), dtype=jnp.float32)


def banded_attention(q, k, v, slopes, half, step):
    n, L, hk, g, dh = q.shape
    blk = half
    nb = -(-L // blk)
    lp = nb * blk
    pad = lp - L
    qp = jnp.pad(q, ((0, 0), (0, pad), (0, 0), (0, 0), (0, 0))).reshape(n, nb, blk, hk, g, dh)
    kp = jnp.pad(k, ((0, 0), (blk, pad + blk), (0, 0), (0, 0))).reshape(n, nb + 2, blk, hk, dh)
    vp = jnp.pad(v, ((0, 0), (blk, pad + blk), (0, 0), (0, 0))).reshape(n, nb + 2, blk, hk, dh)
    kb = jnp.concatenate([kp[:, :-2], kp[:, 1:-1], kp[:, 2:]], axis=2)
    vb = jnp.concatenate([vp[:, :-2], vp[:, 1:-1], vp[:, 2:]], axis=2)
    s = jnp.einsum('nbqhgd,nbkhd->nbhgqk', qp, kb, preferred_element_type=jnp.float32) * (dh ** -0.5)
    qi = jnp.arange(blk)
    kj = jnp.arange(3 * blk)
    rel = kj[None, :] - blk - qi[:, None]
    kpos = jnp.arange(nb)[:, None] * blk - blk + kj[None, :]
    valid = (jnp.abs(rel) <= half)[None] & ((kpos >= 0) & (kpos < L))[:, None, :]
    dist = (step * jnp.abs(rel)).astype(jnp.float32)
    s = s - slopes.astype(jnp.float32)[:, :, None, None] * dist
    s = jnp.where(valid[None, :, None, None], s, -jnp.inf)
    m = jnp.max(s, axis=-1, keepdims=True)
    p = jnp.exp(s - m)
    l = jnp.sum(p, axis=-1, keepdims=True)
    o = jnp.einsum('nbhgqk,nbkhd->nbhgqd', p, vb.astype(jnp.float32)) / l
    lse = (m + jnp.log(l))[..., 0]
    o = o.transpose(0, 1, 4, 2, 3, 5).reshape(n, lp, hk, g, dh)[:, :L]
    lse = lse.transpose(0, 1, 4, 2, 3).reshape(n, lp, hk, g)[:, :L]
    return o, lse


def window_gqa(q, k, v, sink):
    b, s, _ = q.shape
    qh = q.reshape(b, s, A_KV, A_GROUP, HEAD_DIM)
    kh = k.reshape(b, s, A_KV, HEAD_DIM)
    vh = v.reshape(b, s, A_KV, HEAD_DIM)
    slopes = alibi_slopes(A_HEADS).reshape(A_KV, A_GROUP)
    o, lse = banded_attention(qh, kh, vh, slopes, A_HALF, 1)
    sk = sink.astype(jnp.float32).reshape(A_KV, A_GROUP)
    lse_tot = jnp.logaddexp(lse, sk)
    o = o * jnp.exp(lse - lse_tot)[..., None]
    return o.reshape(b, s, A_Q).astype(q.dtype)


def dilated_attention(q, k, v):
    b, s, _ = q.shape
    slopes = alibi_slopes(B_HEADS).reshape(N_DIL, B_HPG)
    outs, lses = [], []
    for gi, (w, r) in enumerate(DILATED_GROUPS):
        lo, hi = gi * B_HPG * HEAD_DIM, (gi + 1) * B_HPG * HEAD_DIM
        L = s // r

        def strided(t):
            return t[..., lo:hi].reshape(b, L, r, B_HPG, HEAD_DIM).transpose(0, 2, 1, 3, 4).reshape(b * r, L, B_HPG, HEAD_DIM)

        qg, kg, vg = strided(q), strided(k), strided(v)
        o, lse = banded_attention(qg[:, :, :, None], kg, vg, slopes[gi][:, None], w // (2 * r), r)
        o = o[:, :, :, 0].reshape(b, r, L, B_HPG, HEAD_DIM).transpose(0, 2, 1, 3, 4).reshape(b, s, B_HPG, HEAD_DIM)
        lse = lse[:, :, :, 0].reshape(b, r, L, B_HPG).transpose(0, 2, 1, 3).reshape(b, s, B_HPG)
        outs.append(o)
        lses.append(lse)
    alpha = jax.nn.softmax(jnp.stack(lses), axis=0)
    o = jnp.sum(alpha[..., None] * jnp.stack(outs), axis=0)
    return o.reshape(b, s, B_OUT).astype(q.dtype)


def hier_moe(xn, w_rg, b_rg, w_re, b_re, w1, w3, w2):
    bsz, s, d = xn.shape
    xt = xn.reshape(-1, d)
    n = xt.shape[0]
    lg = jnp.matmul(xt, w_rg, preferred_element_type=jnp.float32) + b_rg.astype(jnp.float32)
    pg = jax.nn.softmax(lg, axis=-1)
    grp = jnp.argmax(lg, axis=-1)
    pgrp = jnp.take_along_axis(pg, grp[:, None], axis=1)[:, 0]
    le = jnp.einsum('nd,gde->nge', xt, w_re, preferred_element_type=jnp.float32) + b_re.astype(jnp.float32)
    le_sel = jnp.take_along_axis(le, grp[:, None, None], axis=1)[:, 0]
    top_l, top_i = lax.top_k(le_sel, TOP_K)
    wts = jax.nn.softmax(top_l, axis=-1) * pgrp[:, None]
    eid = grp[:, None] * EXPERTS_PER_GROUP + top_i
    flat_e = eid.reshape(-1).astype(jnp.int32)
    flat_w = wts.reshape(-1)
    flat_t = jnp.repeat(jnp.arange(n, dtype=jnp.int32), TOP_K)
    order = jnp.argsort(flat_e)
    se, st, sw = flat_e[order], flat_t[order], flat_w[order]
    counts = jnp.bincount(flat_e, length=N_EXPERTS)
    starts = jnp.cumsum(counts) - counts
    pcounts = (counts + ROW_BLOCK - 1) // ROW_BLOCK * ROW_BLOCK
    pends = jnp.cumsum(pcounts)
    pstarts = pends - pcounts
    dest = pstarts[se] + (jnp.arange(n * TOP_K) - starts[se])
    nblk = (n * TOP_K + N_EXPERTS * (ROW_BLOCK - 1) + ROW_BLOCK - 1) // ROW_BLOCK
    rows = nblk * ROW_BLOCK
    row_tok = jnp.full((rows,), n, dtype=jnp.int32).at[dest].set(st)
    row_w = jnp.zeros((rows,), jnp.float32).at[dest].set(sw)
    blk_e = jnp.minimum(jnp.searchsorted(pends, jnp.arange(nblk) * ROW_BLOCK, side='right'), N_EXPERTS - 1)
    xpad = jnp.concatenate([xt, jnp.zeros((1, d), xt.dtype)], axis=0)
    xr = xpad[row_tok].reshape(nblk, ROW_BLOCK, d)

    def expert_rows(args):
        xb, e = args
        h = jax.nn.silu(xb @ w1[e]) * (xb @ w3[e])
        return h @ w2[e]

    yr = lax.map(expert_rows, (xr, blk_e)).reshape(rows, d)
    y = jnp.zeros((n + 1, d), jnp.float32).at[row_tok].add(row_w[:, None] * yr.astype(jnp.float32))[:n]
    return y.reshape(bsz, s, d).astype(xn.dtype)


def encoder_layer(x, norm1, w_in, sink, w_proj_a, w_proj_b, w_gate, b_gate, w_out,
                  norm2, w_rg, b_rg, w_re, b_re, w1, w3, w2):
    xn = rms_norm(x, norm1)
    proj = xn @ w_in
    qa, ka, va, qb, kb, vb = jnp.split(proj, SPLITS, axis=-1)
    oa = window_gqa(qa, ka, va, sink)
    ob = dilated_attention(qb, kb, vb)
    gates = jax.nn.sigmoid((xn @ w_gate + b_gate).astype(jnp.float32))
    ga, gb = jnp.split(gates, 2, axis=-1)
    merged = ga * (oa @ w_proj_a).astype(jnp.float32) + gb * (ob @ w_proj_b).astype(jnp.float32)
    h = x + merged.astype(x.dtype) @ w_out
    return h + hier_moe(rms_norm(h, norm2), w_rg, b_rg, w_re, b_re, w1, w3, w2)


def trunk(x, norm1, w_in, sink, w_proj_a, w_proj_b, w_gate, b_gate, w_out,
          norm2, w_rg, b_rg, w_re, b_re, w1, w3, w2, norm_final):
    for l in range(DEPTH):
        x = encoder_layer(x, norm1[l], w_in[l], sink[l], w_proj_a[l], w_proj_b[l], w_gate[l], b_gate[l],
                          w_out[l], norm2[l], w_rg[l], b_rg[l], w_re[l], b_re[l], w1[l], w3[l], w2[l])
    return rms_norm(x, norm_final)


def setup_inputs(seed: int = 0) -> dict:
    key = jax.random.key(seed)
    ks = jax.random.split(key, 20)
    f32 = jnp.float32
    D = D_MODEL

    def nrm(k, shape, scale):
        return jax.random.normal(k, shape, f32) * scale

    return {
        "x_prompt": jax.random.normal(ks[0], (BATCH, SEQ, D), f32),
        "x_sample": jax.random.normal(ks[1], (DEC_BATCH, DEC_SEQ, D), f32),
        "norm1": 1.0 + nrm(ks[2], (DEPTH, D), 0.02),
        "w_in": nrm(ks[3], (DEPTH, D, IN_COLS), D ** -0.5),
        "attn_sink": nrm(ks[4], (DEPTH, A_HEADS), 0.5),
        "w_proj_a": nrm(ks[5], (DEPTH, A_Q, D), A_Q ** -0.5),
        "w_proj_b": nrm(ks[6], (DEPTH, B_OUT, D), B_OUT ** -0.5),
        "w_gate": nrm(ks[7], (DEPTH, D, 2 * D), D ** -0.5),
        "b_gate": nrm(ks[8], (DEPTH, 2 * D), 0.02),
        "w_out": nrm(ks[9], (DEPTH, D, D), D ** -0.5),
        "norm2": 1.0 + nrm(ks[10], (DEPTH, D), 0.02),
        "w_router_group": nrm(ks[11], (DEPTH, D, N_GROUPS), D ** -0.5),
        "b_router_group": nrm(ks[12], (DEPTH, N_GROUPS), 0.01),
        "w_router_expert": nrm(ks[13], (DEPTH, N_GROUPS, D, EXPERTS_PER_GROUP), D ** -0.5),
        "b_router_expert": nrm(ks[14], (DEPTH, N_GROUPS, EXPERTS_PER_GROUP), 0.01),
        "w_expert_gate": nrm(ks[15], (DEPTH, N_EXPERTS, D, D_FF), D ** -0.5),
        "w_expert_up": nrm(ks[16], (DEPTH, N_EXPERTS, D, D_FF), D ** -0.5),
        "w_expert_down": nrm(ks[17], (DEPTH, N_EXPERTS, D_FF, D), D_FF ** -0.5),
        "norm_final": 1.0 + nrm(ks[18], (D,), 0.02),
    }


def reference(x_prompt, x_sample, norm1, w_in, attn_sink, w_proj_a, w_proj_b, w_gate, b_gate, w_out,
              norm2, w_router_group, b_router_group, w_router_expert, b_router_expert,
              w_expert_gate, w_expert_up, w_expert_down, norm_final):
    y_prompt = trunk(x_prompt, norm1, w_in, attn_sink, w_proj_a, w_proj_b, w_gate, b_gate, w_out,
                     norm2, w_router_group, b_router_group, w_router_expert, b_router_expert,
                     w_expert_gate, w_expert_up, w_expert_down, norm_final)
    y_sample = trunk(x_sample, norm1, w_in, attn_sink, w_proj_a, w_proj_b, w_gate, b_gate, w_out,
                     norm2, w_router_group, b_router_group, w_router_expert, b_router_expert,
                     w_expert_gate, w_expert_up, w_expert_down, norm_final)
    return (y_prompt, y_sample)
```

```python
import math
import numpy as np
import ml_dtypes
import concourse.bass as bass
import concourse.mybir as mybir
from concourse.bass_utils import run_bass_kernel_spmd

F32 = mybir.dt.float32
BF16 = mybir.dt.bfloat16
I32 = mybir.dt.int32
AF = mybir.ActivationFunctionType
ALU = mybir.AluOpType
AX = mybir.AxisListType

D = 2048
KC = D // 128
HEAD = 128
A_HEADS, A_KV, A_GROUP = 16, 4, 4
B_HPG = 4
DIL = ((128, 1), (512, 4), (2048, 16))
IN_COLS = 7680
N_EXP = 64
D_FF = 512
EPS = 1e-6
SCALE = HEAD ** -0.5
NCORES = 8
T = 512
HALO = 1024
P_OWN, S_OWN = 1024, 2048
P_EXT, S_EXT = P_OWN + 2 * HALO, S_OWN + 2 * HALO
EXT = P_EXT + S_EXT
OWN = P_OWN + S_OWN
CAP = 256
NSLOT = N_EXP * CAP
YT_STRIDE = OWN + 128
BIG_IDX = 1 << 24
SEM_ROLL = 20000


class Buf:
    __slots__ = ("name", "writers", "readers")

    def __init__(self, name):
        self.name = name
        self.writers = []
        self.readers = []


class Op:
    __slots__ = ("eng", "fn", "deps", "signal", "event", "is_dma", "idx")

    def __init__(self, eng, fn, is_dma):
        self.eng = eng
        self.fn = fn
        self.deps = []
        self.signal = False
        self.event = None
        self.is_dma = is_dma
        self.idx = -1


ENGS = ("pe", "act", "dve", "pool", "sp")


class Sched:
    def __init__(self):
        self.streams = {e: [] for e in ENGS}
        self.nops = 0

    def op(self, eng, fn, reads=(), writes=(), pwrites=(), dma=False):
        o = Op(eng, fn, dma)
        o.idx = self.nops
        self.nops += 1
        deps = []
        for b in reads:
            deps.extend(b.writers)
        for b in writes:
            deps.extend(b.writers)
            deps.extend(b.readers)
        for b in pwrites:
            deps.extend(b.readers)
        for b in reads:
            b.readers.append(o)
        for b in writes:
            b.writers = [o]
            b.readers = []
        for b in pwrites:
            if b.readers:
                b.writers = [o]
                b.readers = []
            else:
                b.writers.append(o)
        latest = {}
        out = []
        seen = set()
        for d in deps:
            if d is o or id(d) in seen:
                continue
            seen.add(id(d))
            if d.is_dma:
                out.append(d)
            else:
                if d.eng == eng and eng == "pe":
                    continue
                cur = latest.get(d.eng)
                if cur is None or d.idx > cur.idx:
                    latest[d.eng] = d
        out.extend(latest.values())
        o.deps = out
        self.streams[eng].append(o)
        return o

    def barrier(self):
        deps = []
        for e in ENGS:
            st = self.streams[e]
            if not st:
                continue
            last_c = None
            for o in reversed(st):
                if o.fn is None:
                    break
                if o.is_dma:
                    deps.append(o)
                elif last_c is None:
                    last_c = o
            if last_c is not None:
                deps.append(last_c)
        for e in ENGS:
            o = Op(e, None, False)
            o.idx = self.nops
            self.nops += 1
            o.deps = [d for d in deps]
            self.streams[e].append(o)


def emit_program(nc, sched, extra_ctx):
    for e in ENGS:
        for o in sched.streams[e]:
            for d in o.deps:
                d.signal = True
            if o.is_dma:
                o.signal = True
    n_dma_sems = {"sp": 16, "act": 4, "pool": 12, "dve": 2, "pe": 2}
    n_eng_sems = {}
    for e in ENGS:
        cnt = sum(1 for o in sched.streams[e] if o.signal and not o.is_dma)
        n_eng_sems[e] = max(1, (cnt + SEM_ROLL - 1) // SEM_ROLL)
    from contextlib import ExitStack
    with ExitStack() as st:
        eng_sems = {e: [st.enter_context(nc.semaphore(f"s_{e}_{i}")) for i in range(n_eng_sems[e])] for e in ENGS}
        dma_sems = {e: [st.enter_context(nc.semaphore(f"d_{e}_{i}")) for i in range(n_dma_sems[e])] for e in ENGS}
        for e in ENGS:
            cnt = 0
            dcnt = 0
            dma_last = [None] * n_dma_sems[e]
            dma_uses = [0] * n_dma_sems[e]
            for o in sched.streams[e]:
                if o.is_dma:
                    k = dcnt % n_dma_sems[e]
                    dcnt += 1
                    if dma_last[k] is not None:
                        o.deps.append(dma_last[k])
                    dma_uses[k] += 1
                    o.event = (dma_sems[e][k], 16 * dma_uses[k], 16)
                    dma_last[k] = o
                elif o.signal:
                    si = cnt // SEM_ROLL
                    o.event = (eng_sems[e][si], cnt % SEM_ROLL + 1, 1)
                    cnt += 1
        block = st.enter_context(nc.Block())

        def make_section(e):
            ops = sched.streams[e]

            def section(h):
                waited = {}
                for o in ops:
                    need = {}
                    for d in o.deps:
                        sem, val, _ = d.event
                        key = id(sem)
                        if key not in need or need[key][1] < val:
                            need[key] = (sem, val)
                    for key, (sem, val) in need.items():
                        if waited.get(key, 0) >= val:
                            continue
                        h.wait_ge(sem, val)
                        waited[key] = val
                    if o.fn is None:
                        continue
                    ins = o.fn(h)
                    if o.signal:
                        sem, val, inc = o.event
                        ins.then_inc(sem, inc)
            return section

        block.tensor(make_section("pe"))
        block.scalar(make_section("act"))
        block.vector(make_section("dve"))
        block.gpsimd(make_section("pool"))
        block.sync(make_section("sp"))


KV_GROUPS = (4, 5, 9, 12, 10, 13, 11, 14)
KVS_COLS = 4096


def default_cfg():
    chunks = []
    for seg0, own0, own1, ext in ((0, HALO, HALO + P_OWN, P_EXT), (P_EXT, HALO, HALO + S_OWN, S_EXT)):
        for c in range(0, ext, T):
            if seg0 > 0 and c < HALO:
                continue
            near = (c + T > own0 - 256) and (c < own1 + 256)
            chunks.append((seg0 + c, list(range(8)) if near else [6, 7], own0 <= c < own1))
    tiles = [HALO, HALO + T] + [P_EXT + HALO + i * T for i in range(4)]
    return dict(chunks=chunks, tiles=tiles, phases=(1, 2, 3, 4, 5), experts=list(range(N_EXP)), debug=False)


def own_row(o):
    return o - HALO if o < P_EXT else P_OWN + (o - P_EXT - HALO)


def build(cfg):
    from contextlib import ExitStack
    nc = bass.Bass("TRN2", target_bir_lowering=False)
    S = Sched()
    es = ExitStack()
    dbg = cfg["debug"]

    def din(name, shape, dt=F32):
        return nc.dram_tensor(name, list(shape), dt, kind="ExternalInput").ap()

    def dscratch(name, shape, dt):
        kind = "ExternalOutput" if dbg else "Internal"
        return nc.dram_tensor(name, list(shape), dt, kind=kind).ap()

    cur = [es]

    def sb(name, shape, dt):
        return cur[0].enter_context(nc.sbuf_tensor(name, list(shape), dt))

    class Rot:
        def __init__(self, items):
            self.items = list(items)
            self.i = 0

        def next(self):
            it = self.items[self.i % len(self.items)]
            self.i += 1
            return it

    def run_phase(fn, nbf=6):
        with ExitStack() as pes:
            cur[0] = pes
            ring["n"] = nbf
            ring["bf"] = [sb(f"wbf_{fn.__name__}_{i}", [128, 4, 512], BF16) for i in range(nbf)]
            ring["b"] = [Buf(f"wbf{i}") for i in range(nbf)]
            wcount[0] = 0
            fn()
            assert not wq and not wissued
            S.barrier()
        cur[0] = es

    def ps(name, shape, dt):
        return es.enter_context(nc.psum_tensor(name, list(shape), dt))

    xin = din("xin", [EXT, D])
    w_in = din("w_in", [D, IN_COLS])
    w_gate = din("w_gate", [D, 2 * D])
    w_pa = din("w_pa", [D, D])
    w_pb = din("w_pb", [512, D])
    w_out = din("w_out", [D, D])
    w1 = din("w1", [N_EXP * D, D_FF])
    w3 = din("w3", [N_EXP * D, D_FF])
    w2 = din("w2", [N_EXP * D_FF, D])
    g1 = din("g1", [1, D])
    g2 = din("g2", [1, D])
    gf = din("gf", [1, D])
    bgT = din("bgT", [128, 32])
    wr = din("wr", [128, KC * 72])
    br = din("br", [1, 72])
    sink = din("sink", [1, 16])
    eA = din("eA", [128, 4 * 3 * 512], BF16)
    eB = din("eB", [128, 2 * 4 * 2 * 128 + 4 * 2 * 32], BF16)
    vmask = din("vmask", [len(cfg["tiles"]) * 128, 51 * 128], BF16)
    yout = nc.dram_tensor("yout", [OWN, D], F32, kind="ExternalOutput").ap()

    XNT = dscratch("XNT", [KC * 128, EXT], BF16)
    KVS = dscratch("KVS", [EXT, KVS_COLS], BF16)
    OTS = dscratch("OTS", [20 * 128, OWN], BF16)
    HS = dscratch("HS", [OWN, D], F32)
    HN = dscratch("HN", [OWN + 128, D], BF16)
    SLOTI = dscratch("SLOTI", [NSLOT, 4], I32)
    YS = dscratch("YS", [2 * YT_STRIDE + NSLOT, D], BF16)
    b_XNT, b_KVS, b_OTS, b_HS, b_HN, b_SLOTI, b_YS, b_yout = (Buf(n) for n in "XNT KVS OTS HS HN SLOTI YS yout".split())

    ident = sb("ident", [128, 128], BF16)
    identf = sb("identf", [128, 128], F32)
    ones_bf = sb("ones_bf", [128, 128], BF16)
    neghalf = sb("neghalf", [128, 1], F32)
    b_const = Buf("const")

    S.op("pool", lambda h: h.memset(identf[:], 0.0), writes=[b_const])
    S.op("pool", lambda h: h.memset(neghalf[:], -0.5), pwrites=[b_const])
    S.op("pool", lambda h: h.memset(ones_bf[:], 1.0), pwrites=[b_const])
    b_identf = Buf("identf")
    S.op("pool", lambda h: h.affine_select(out=identf[:], in_=identf[:], pattern=[[1, 128]], compare_op=ALU.not_equal,
                                           fill=1.0, base=0, channel_multiplier=-1), reads=[b_const], writes=[b_identf])
    S.op("pool", lambda h: h.tensor_copy(out=ident[:], in_=identf[:]), reads=[b_identf], writes=[b_const])

    banks = [ps(f"bank{i}", [128, 512], F32) for i in range(8)]
    b_bank = [Buf(f"bank{i}") for i in range(8)]

    ring = {"bf": [], "b": [], "n": 0}
    wq = []
    wissued = []
    wcount = [0]

    def w_issue():
        src = wq.pop(0)
        i = wcount[0]
        wcount[0] += 1
        b = i % ring["n"]
        t_, b_ = ring["bf"][b], ring["b"][b]
        S.op("pool", lambda h: h.dma_start(out=t_[:], in_=src.rearrange("(kk p) c -> p kk c", p=128)),
             writes=[b_], dma=True)
        wissued.append((t_, b_))

    def w_push(srcs):
        wq.extend(srcs)

    def w_get():
        depth = ring["n"] - 2
        while wq and len(wissued) < depth + 1:
            w_issue()
        return wissued.pop(0)

    def piece(w, rg, cg):
        return w[rg * 512:(rg + 1) * 512, cg * 512:(cg + 1) * 512]

    def dma_split(eng, out_ap, in_ap, nsplit, **kw):
        n1 = out_ap.shape[1]
        step = n1 // nsplit
        first_w = kw.pop("writes", ())
        pw = kw.pop("pwrites", ())
        for i in range(nsplit):
            o_, i_ = out_ap[:, i * step:(i + 1) * step], in_ap[:, i * step:(i + 1) * step]
            if i == 0:
                S.op(eng, lambda h, o_=o_, i_=i_: h.dma_start(out=o_, in_=i_), writes=first_w, pwrites=pw, dma=True, **kw)
            else:
                S.op(eng, lambda h, o_=o_, i_=i_: h.dma_start(out=o_, in_=i_), pwrites=list(first_w) + list(pw), dma=True, **kw)

    evac_rr = [0]

    def evac_copy(out_ap, in_ap, reads, writes=(), pwrites=()):
        e = ("act", "dve")[evac_rr[0] % 2]
        evac_rr[0] += 1
        if e == "act":
            S.op("act", lambda h: h.copy(out=out_ap, in_=in_ap), reads=reads, writes=writes, pwrites=pwrites)
        else:
            S.op("dve", lambda h: h.tensor_copy(out=out_ap, in_=in_ap), reads=reads, writes=writes, pwrites=pwrites)

    def phase1():
        g1b = sb("g1b", [128, D], F32)
        b_g1b = Buf("g1b")
        S.op("sp", lambda h: h.dma_start(out=g1b[:], in_=g1[0, :].partition_broadcast(128)), writes=[b_g1b], dma=True)
        xst = [sb(f"xst{i}", [128, D], F32) for i in range(4)]
        b_xst = [Buf(f"xst{i}") for i in range(4)]
        junk = sb("junk1", [128, D], BF16)
        xnb = [sb(f"xnb{i}", [128, D], BF16) for i in range(8)]
        b_xnb = [Buf(f"xnb{i}") for i in range(8)]
        stat = [sb(f"stat{i}", [128, 4], F32) for i in range(4)]
        b_stat = [Buf(f"stat{i}") for i in range(4)]
        b_ms = [Buf(f"ms{i}") for i in range(4)]
        b_rstd = [Buf(f"rstd{i}") for i in range(4)]
        xnT = [sb(f"xnT{i}", [128, KC, T], BF16) for i in range(2)]
        b_xnT = [Buf(f"xnT{i}") for i in range(2)]
        kvo = [sb(f"kvo{i}", [128, 512], BF16) for i in range(4)]
        b_kvo = [Buf(f"kvo{i}") for i in range(4)]
        nkvo = 0
        w_push([piece(w_in, kg, KV_GROUPS[cg]) for (_c0, cgs_, _o) in cfg["chunks"] for cg in cgs_ for kg in range(4)])

        def norm_elem(ci):
            c0 = cfg["chunks"][ci][0]
            for tb in range(4):
                i = (ci * 4 + tb) % 4
                xi = (ci % 2) * 4 + tb
                r0 = c0 + tb * 128
                S.op("sp", lambda h, i=i, r0=r0: h.dma_start(out=xst[i][:], in_=xin[r0:r0 + 128, :]),
                     writes=[b_xst[i]], dma=True)
                S.op("act", lambda h, i=i: h.activation(out=junk[:], in_=xst[i][:], func=AF.Square,
                                                        accum_out=stat[i][:, 0:1]),
                     reads=[b_xst[i]], writes=[b_stat[i]])
                S.op("dve", lambda h, i=i: h.tensor_scalar(out=stat[i][:, 1:2], in0=stat[i][:, 0:1], scalar1=1.0 / D,
                                                          scalar2=EPS, op0=ALU.mult, op1=ALU.add),
                     reads=[b_stat[i]], writes=[b_ms[i]])
                S.op("pool", lambda h, i=i: h.tensor_tensor(out=stat[i][:, 2:3], in0=stat[i][:, 1:2], in1=neghalf[:], op=ALU.pow),
                     reads=[b_const, b_ms[i]], writes=[b_rstd[i]])
                S.op("dve", lambda h, i=i, xi=xi: h.scalar_tensor_tensor(out=xnb[xi][:], in0=xst[i][:], scalar=stat[i][:, 2:3],
                                                                        in1=g1b[:], op0=ALU.mult, op1=ALU.mult),
                     reads=[b_xst[i], b_rstd[i], b_g1b], writes=[b_xnb[xi]])

        if cfg.get("zero_left", True):
            zkv = sb("zkv", [128, KVS_COLS], BF16); b_zkv = Buf("zkv")
            S.op("pool", lambda h: h.memset(zkv[:], 0.0), writes=[b_zkv])
            for zb in range(HALO // 128):
                S.op("sp", lambda h, zb=zb: h.dma_start(out=KVS[P_EXT + zb * 128:P_EXT + (zb + 1) * 128, :], in_=zkv[:]),
                     reads=[b_zkv], pwrites=[b_KVS], dma=True)
        norm_elem(0)
        for ci, (c0, cgs, is_own) in enumerate(cfg["chunks"]):
            xt, b_xt = xnT[ci % 2], b_xnT[ci % 2]
            for tb in range(4):
                xi = (ci % 2) * 4 + tb
                for half in range(2):
                    bk = (2 * tb + half) % 8
                    tpv = banks[bk][:].bitcast(BF16)
                    for k8 in range(8):
                        k = half * 8 + k8
                        S.op("pe", lambda h, xi=xi, k=k, k8=k8, tpv=tpv: h.transpose(
                            out=tpv[:, k8 * 128:(k8 + 1) * 128], in_=xnb[xi][:, k * 128:(k + 1) * 128], identity=ident[:]),
                            reads=[b_xnb[xi], b_const], writes=[b_bank[bk]] if k8 == 0 else (), pwrites=() if k8 == 0 else [b_bank[bk]])
                    evac_copy(xt[:, half * 8:(half + 1) * 8, tb * 128:(tb + 1) * 128],
                              tpv.rearrange("p (k t) -> p k t", k=8), reads=[b_bank[bk]], pwrites=[b_xt])
            if ci + 1 < len(cfg["chunks"]):
                norm_elem(ci + 1)
            if is_own:
                dma_split("sp", XNT.rearrange("(k p) t -> p k t", p=128)[:, :, c0:c0 + T], xt[:], 4,
                          reads=[b_xt], pwrites=[b_XNT])
            for gi_, cg in enumerate(cgs):
                bset = (gi_ % 2) * 4
                for kg in range(4):
                    wt, b_wt = w_get()
                    for tb in range(4):
                        for kk in range(4):
                            first = (kg == 0 and kk == 0)
                            last = (kg == 3 and kk == 3)
                            S.op("pe", lambda h, tb=tb, kk=kk, kg=kg, wt=wt, xt=xt, first=first, last=last, bset=bset:
                                 h.matmul(banks[bset + tb][:], xt[:, kg * 4 + kk, tb * 128:(tb + 1) * 128], wt[:, kk, :],
                                          start=first, stop=last),
                                 reads=[b_xt, b_wt], writes=[b_bank[bset + tb]] if first else (),
                                 pwrites=() if first else [b_bank[bset + tb]])
                for tb in range(4):
                    j = nkvo % 4
                    nkvo += 1
                    evac_copy(kvo[j][:], banks[bset + tb][:], reads=[b_bank[bset + tb]], writes=[b_kvo[j]])
                    r0 = c0 + tb * 128
                    S.op("sp", lambda h, j=j, r0=r0, cg=cg: h.dma_start(out=KVS[r0:r0 + 128, cg * 512:(cg + 1) * 512], in_=kvo[j][:]),
                         reads=[b_kvo[j]], pwrites=[b_KVS], dma=True)


    def pe_mm(out_ap, lhsT, rhs, start, stop, reads, bbuf, first):
        S.op("pe", lambda h: h.matmul(out_ap, lhsT, rhs, start=start, stop=stop),
             reads=reads, writes=[bbuf] if first else (), pwrites=() if first else [bbuf])

    def phase2():
        eA_sb = sb("eA_sb", [128, 4 * 3 * 512], BF16)
        eB_sb = sb("eB_sb", [128, 2304], BF16)
        b_tab = Buf("tab")
        S.op("sp", lambda h: h.dma_start(out=eA_sb[:], in_=eA), writes=[b_tab], dma=True)
        S.op("sp", lambda h: h.dma_start(out=eB_sb[:], in_=eB), pwrites=[b_tab], dma=True)
        sink_sb = sb("sink_sb", [1, 16], F32)
        sexp = sb("sexp", [1, 16], F32)
        sinkrow = sb("sinkrow", [1, 16 * 128], BF16)
        b_sink, b_sexp, b_sinkrow = Buf("sink"), Buf("sexp"), Buf("sinkrow")
        S.op("sp", lambda h: h.dma_start(out=sink_sb[:], in_=sink), writes=[b_sink], dma=True)
        S.op("act", lambda h: h.activation(out=sexp[:], in_=sink_sb[:], func=AF.Exp), reads=[b_sink], writes=[b_sexp])
        S.op("dve", lambda h: h.tensor_copy(out=sinkrow[:].rearrange("p (h q) -> p h q", q=128),
                                            in_=sexp[:].unsqueeze(2).broadcast_to([1, 16, 128])),
             reads=[b_sexp], writes=[b_sinkrow])
        xT = sb("xT2", [128, KC, T], BF16); b_xT = Buf("xT2")
        vm2 = [sb(f"vm{i}", [128, 51, 128], BF16) for i in range(2)]; b_vm2 = [Buf(f"vm{i}") for i in range(2)]
        OT = sb("OT", [128, 20, T], BF16); b_OT = Buf("OT")
        QA = [sb(f"QA{i}", [128, 4, T], BF16) for i in range(2)]; b_QA = [Buf(f"QA{i}") for i in range(2)]
        QB = sb("QB", [128, 12, T], BF16); b_QB = Buf("QB")
        KnA = [sb(f"KnA{i}", [128, 6, 128], BF16) for i in range(2)]; b_KnA = [Buf(f"KnA{i}") for i in range(2)]
        VA = [sb(f"VA{i}", [128, 6, 128], BF16) for i in range(2)]; b_VA = [Buf(f"VA{i}") for i in range(2)]
        KTA = [sb(f"KTA{i}", [128, 768], BF16) for i in range(2)]; b_KTA = [Buf(f"KTA{i}") for i in range(2)]
        Kn1 = sb("Kn1", [128, 5, 128], BF16)
        Kn2 = sb("Kn2", [128, 4, 2, 128], BF16)
        Kn3a = sb("Kn3a", [128, 16, 128], BF16)
        Kn3b = sb("Kn3b", [32, 16, 128], BF16)
        V1 = [sb(f"V1_{i}", [128, 5, 128], BF16) for i in range(2)]
        V2 = [sb(f"V2_{i}", [128, 4, 2, 128], BF16) for i in range(2)]
        V3a = [sb(f"V3a_{i}", [128, 16, 128], BF16) for i in range(2)]
        V3b = [sb(f"V3b_{i}", [32, 16, 128], BF16) for i in range(2)]
        b_KnB = Buf("KnB")
        b_VB = [Buf(f"VB{i}") for i in range(2)]
        KT1 = [sb(f"KT1_{i}", [128, 5 * 128], BF16) for i in range(2)]
        KT2 = [sb(f"KT2_{i}", [128, 8 * 128], BF16) for i in range(2)]
        KT3 = [sb(f"KT3_{i}", [128, 16, 160], BF16) for i in range(2)]
        b_KTB = [Buf(f"KTB{i}") for i in range(2)]
        NP = 8
        Pb = [sb(f"P{i}", [128, 512], BF16) for i in range(NP)]
        b_P = [Buf(f"P{i}") for i in range(NP)]
        prot = Rot(range(NP))
        Rb = [sb(f"R{i}", [128, 512], F32) for i in range(2)]
        b_R = [Buf(f"R{i}") for i in range(2)]
        rrot = Rot(range(2))
        srot = Rot(range(4))
        orot = Rot((4, 5))
        lrot = Rot((6, 7))
        mulrot = Rot(("dve",))

        def load_rows(dst_t, src, bbuf, first, nsplit=1):
            four = len(dst_t.shape) == 4
            n1 = dst_t.shape[2] if four else dst_t.shape[1]
            step = n1 // nsplit
            for i in range(nsplit):
                if four:
                    d_, s_ = dst_t[:, :, i, :], src[:, :, i, :]
                else:
                    d_, s_ = dst_t[:, i * step:(i + 1) * step], src[:, i * step:(i + 1) * step]
                f_ = first and i == 0
                S.op("sp", lambda h, d_=d_, s_=s_: h.dma_start(out=d_, in_=s_), reads=[b_KVS],
                     writes=[bbuf] if f_ else (), pwrites=() if f_ else [bbuf], dma=True)

        def kv_src(kind, o, col):
            cs = slice(col, col + 128)
            if kind == "A":
                return KVS[o - 128:o - 128 + 768, cs].rearrange("(j p) d -> p j d", p=128)
            if kind == "1":
                return KVS[o - 64:o - 64 + 640, cs].rearrange("(j p) d -> p j d", p=128)
            if kind == "2":
                return KVS[o - 256:o - 256 + 1024, cs].rearrange("(s p ph) d -> p ph s d", s=2, p=128, ph=4)
            if kind == "3a":
                return KVS[o - 1024:o - 1024 + 2048, cs].rearrange("(p ph) d -> p ph d", ph=16)
            if kind == "3b":
                return KVS[o + 1024:o + 1024 + 512, cs].rearrange("(p ph) d -> p ph d", ph=16)

        def softmax_slot(sbk, nk, tab_ap):
            pi = prot.next()
            S.op("act", lambda h: h.activation(out=Pb[pi][0:nk, :], in_=banks[sbk][0:nk, :], func=AF.Exp, scale=SCALE),
                 reads=[b_bank[sbk]], writes=[b_P[pi]])
            me = mulrot.next()
            S.op(me, lambda h: h.tensor_tensor(out=Pb[pi][0:nk, :], in0=Pb[pi][0:nk, :], in1=tab_ap, op=ALU.mult),
                 reads=[b_tab, b_P[pi]], writes=[b_P[pi]])
            return pi

        def finish_unit(ob, lb, out_ap, split=None):
            ri = rrot.next()
            S.op("dve", lambda h: h.reciprocal(out=Rb[ri][:], in_=banks[lb][:]), reads=[b_bank[lb]], writes=[b_R[ri]])
            i0, i1 = banks[ob][:], Rb[ri][:]
            if split is not None:
                i0 = i0.rearrange("p (g q) -> p g q", g=split)
                i1 = i1.rearrange("p (g q) -> p g q", g=split)
            S.op("dve", lambda h: h.tensor_tensor(out=out_ap, in0=i0, in1=i1, op=ALU.mult),
                 reads=[b_bank[ob], b_R[ri]], pwrites=[b_OT])

        def qproj(cgs, dst, b_dst):
            for i, cg in enumerate(cgs):
                bks = [srot.next() for _ in range(4)]
                for kg in range(4):
                    wt, b_wt = w_get()
                    for cc in range(4):
                        for kk in range(4):
                            first = (kg == 0 and kk == 0)
                            pe_mm(banks[bks[cc]][:], wt[:, kk, cc * 128:(cc + 1) * 128], xT[:, kg * 4 + kk, :],
                                  first, (kg == 3 and kk == 3), [b_wt, b_xT], b_bank[bks[cc]], first)
                for cc in range(4):
                    evac_copy(dst[:, 4 * i + cc, :], banks[bks[cc]][:], reads=[b_bank[bks[cc]]], pwrites=[b_dst])

        for _ in cfg["tiles"]:
            w_push([piece(w_in, kg, kvh) for kvh in range(4) for kg in range(4)])
            w_push([piece(w_in, kg, 6 + gi) for gi in range(3) for kg in range(4)])
        class Step:
            def __init__(self):
                self.prep = None
                self.s1 = None
                self.s2 = None

        def tile_steps(ti, o):
            steps = []
            vm, b_vm = vm2[ti % 2], b_vm2[ti % 2]

            def tile_prep():
                dma_split("sp", xT[:], XNT.rearrange("(k p) t -> p k t", p=128)[:, :, o:o + T], 4, reads=[b_XNT], writes=[b_xT])
                S.op("sp", lambda h: h.dma_start(out=vm[:].rearrange("p s q -> p (s q)"), in_=vmask[ti * 128:(ti + 1) * 128, :]),
                     writes=[b_vm], dma=True)

            def a_prep(kvh):
                par = kvh % 2
                load_rows(KnA[par], kv_src("A", o, 0 * 512 + kvh * 128), b_KnA[par], True, 2)
                load_rows(VA[par], kv_src("A", o, 1 * 512 + kvh * 128), b_VA[par], True, 2)
                qproj([kvh], QA[par], b_QA[par])
                bk = srot.next()
                tpv = banks[bk][:].bitcast(BF16)
                for j in range(6):
                    S.op("pe", lambda h, j=j: h.transpose(out=tpv[:, j * 128:(j + 1) * 128], in_=KnA[par][:, j, :], identity=ident[:]),
                         reads=[b_KnA[par], b_const], writes=[b_bank[bk]] if j == 0 else (), pwrites=() if j == 0 else [b_bank[bk]])
                evac_copy(KTA[par][:], tpv[:, 0:768], reads=[b_bank[bk]], writes=[b_KTA[par]])

            def a_step(kvh, qb):
                par = kvh % 2
                st_ = Step()
                state = {}
                if qb == 0:
                    if kvh == 0:
                        st_.prep = lambda: (tile_prep(), a_prep(kvh))
                    else:
                        st_.prep = lambda: a_prep(kvh)

                def s1():
                    pis = []
                    for s_ in range(3):
                        j = qb + s_
                        sbk = srot.next()
                        pe_mm(banks[sbk][:], KTA[par][:, j * 128:(j + 1) * 128], QA[par][:, :, qb * 128:(qb + 1) * 128],
                              True, True, [b_KTA[par], b_QA[par]], b_bank[sbk], True)
                        pis.append(softmax_slot(sbk, 128, eA_sb[:, (kvh * 3 + s_) * 512:(kvh * 3 + s_ + 1) * 512]))
                    state["pis"] = pis

                def s2():
                    pis = state["pis"]
                    ob, lb = orot.next(), lrot.next()
                    for s_ in range(3):
                        j = qb + s_
                        pe_mm(banks[ob][:], VA[par][:, j, :], Pb[pis[s_]][:], s_ == 0, s_ == 2, [b_VA[par], b_P[pis[s_]]], b_bank[ob], s_ == 0)
                    for s_ in range(3):
                        j = qb + s_
                        pe_mm(banks[lb][:], vm[:, j, :], Pb[pis[s_]][:], s_ == 0, False, [b_vm, b_P[pis[s_]]], b_bank[lb], s_ == 0)
                    pe_mm(banks[lb][:], ones_bf[0:1, :], sinkrow[0:1, kvh * 512:(kvh + 1) * 512], False, True,
                          [b_const, b_sinkrow], b_bank[lb], False)
                    finish_unit(ob, lb, OT[:, kvh * 4:(kvh + 1) * 4, qb * 128:(qb + 1) * 128], split=4)
                st_.s1, st_.s2 = s1, s2
                return st_

            for kvh in range(4):
                for qb in range(4):
                    steps.append(a_step(kvh, qb))

            def b_prep(hh):
                par = hh % 2
                c = hh * 128
                load_rows(Kn1, kv_src("1", o, 2 * 512 + c), b_KnB, True)
                load_rows(Kn2, kv_src("2", o, 4 * 512 + c), b_KnB, False, 2)
                load_rows(Kn3a, kv_src("3a", o, 6 * 512 + c), b_KnB, False, 4)
                load_rows(Kn3b, kv_src("3b", o, 6 * 512 + c), b_KnB, False)
                load_rows(V1[par], kv_src("1", o, 3 * 512 + c), b_VB[par], True)
                load_rows(V2[par], kv_src("2", o, 5 * 512 + c), b_VB[par], False, 2)
                load_rows(V3a[par], kv_src("3a", o, 7 * 512 + c), b_VB[par], False, 4)
                load_rows(V3b[par], kv_src("3b", o, 7 * 512 + c), b_VB[par], False)
                groups = [
                    ([(Kn1[:, j, :], 128) for j in range(5)], 128, KT1[par][:, 0:640], None),
                    ([(Kn2[:, ph, s_, :], 128) for ph in range(4) for s_ in range(2)], 128, KT2[par][:, :], None),
                    ([(Kn3a[:, ph, :], 128) for ph in range(8)], 128, KT3[par][:, 0:8, 0:128], 8),
                    ([(Kn3a[:, ph, :], 128) for ph in range(8, 16)], 128, KT3[par][:, 8:16, 0:128], 8),
                    ([(Kn3b[:, ph, :], 32) for ph in range(16)], 32, KT3[par][:, :, 128:160], 16),
                ]
                for gidx, (jobs, w_, dst, nsplit) in enumerate(groups):
                    bk = srot.next()
                    tpv = banks[bk][:].bitcast(BF16)
                    for n_, (src, npart) in enumerate(jobs):
                        S.op("pe", lambda h, n_=n_, src=src, npart=npart, tpv=tpv, w_=w_: h.transpose(
                            out=tpv[:, n_ * w_:(n_ + 1) * w_], in_=src, identity=ident[0:npart, 0:npart]),
                            reads=[b_KnB, b_const], writes=[b_bank[bk]] if n_ == 0 else (), pwrites=() if n_ == 0 else [b_bank[bk]])
                    srcv = tpv[:, 0:len(jobs) * w_]
                    if nsplit is not None:
                        srcv = srcv.rearrange("p (n w) -> p n w", n=nsplit)
                    evac_copy(dst, srcv, reads=[b_bank[bk]], writes=[b_KTB[par]] if gidx == 0 else (),
                              pwrites=() if gidx == 0 else [b_KTB[par]])

            def b_step(hh, nslot, shared):
                par = hh % 2
                gi, s_ = nslot // 2, nslot % 2
                st_ = Step()
                state = {}
                if nslot == 0:
                    if hh == 0:
                        st_.prep = lambda: (qproj([6, 7, 8], QB, b_QB), b_prep(hh))
                    else:
                        st_.prep = lambda: b_prep(hh)
                nsub, nq = (4, 128) if gi < 2 else (16, 32)
                nk = 32 if (gi == 2 and s_ == 1) else 128
                qv = QB[:, gi * 4 + hh, :]

                def s1():
                    sbk = srot.next()
                    for sp in range(nsub):
                        if gi == 0:
                            lhsT = KT1[par][:, (sp + s_) * 128:(sp + s_ + 1) * 128]
                            rhs = qv[:, sp * 128:(sp + 1) * 128]
                        elif gi == 1:
                            lhsT = KT2[par][:, (sp * 2 + s_) * 128:(sp * 2 + s_ + 1) * 128]
                            rhs = qv.rearrange("p (u r) -> p r u", r=4)[:, sp, :]
                        else:
                            lhsT = KT3[par][:, sp, s_ * 128:s_ * 128 + nk]
                            rhs = qv.rearrange("p (u r) -> p r u", r=16)[:, sp, :]
                        pe_mm(banks[sbk][0:nk, sp * nq:(sp + 1) * nq], lhsT, rhs, sp == 0, sp == nsub - 1,
                              [b_KTB[par], b_QB], b_bank[sbk], sp == 0)
                    if gi < 2:
                        tab = eB_sb[:, ((gi * 4 + hh) * 2 + s_) * 128:((gi * 4 + hh) * 2 + s_ + 1) * 128]
                    else:
                        tab = eB_sb[0:nk, 2048 + (hh * 2 + s_) * 32:2048 + (hh * 2 + s_ + 1) * 32]
                    tab_b = tab.unsqueeze(1).broadcast_to([nk, nsub, nq])
                    pi = prot.next()
                    S.op("act", lambda h: h.activation(out=Pb[pi][0:nk, :], in_=banks[sbk][0:nk, :], func=AF.Exp, scale=SCALE),
                         reads=[b_bank[sbk]], writes=[b_P[pi]])
                    S.op("dve", lambda h: h.tensor_tensor(
                        out=Pb[pi][0:nk, :].rearrange("p (s q) -> p s q", s=nsub),
                        in0=Pb[pi][0:nk, :].rearrange("p (s q) -> p s q", s=nsub), in1=tab_b, op=ALU.mult),
                        reads=[b_tab, b_P[pi]], writes=[b_P[pi]])
                    state["pi"] = pi

                def s2():
                    pi = state["pi"]
                    if nslot == 0:
                        shared["ob"], shared["lb"] = orot.next(), lrot.next()
                    ob, lb = shared["ob"], shared["lb"]
                    for which, bkx in ((0, ob), (1, lb)):
                        for sp in range(nsub):
                            if gi == 0:
                                vv = V1[par][:, sp + s_, :]
                                sc = 6 + sp + s_
                                out_ap = banks[bkx][:, sp * 128:(sp + 1) * 128]
                            elif gi == 1:
                                vv = V2[par][:, sp, s_, :]
                                sc = 11 + sp * 2 + s_
                                out_ap = banks[bkx][:].rearrange("p (u r) -> p r u", r=4)[:, sp, :]
                            else:
                                vv = (V3a[par] if s_ == 0 else V3b[par])[0:nk, sp, :]
                                sc = 19 + sp * 2 + s_
                                out_ap = banks[bkx][:].rearrange("p (u r) -> p r u", r=16)[:, sp, :]
                            lhsT = vv if which == 0 else vm[0:nk, sc, :]
                            very_first = (nslot == 0 and sp == 0)
                            very_last = (nslot == 5 and sp == nsub - 1)
                            pe_mm(out_ap, lhsT, Pb[pi][0:nk, sp * nq:(sp + 1) * nq], very_first, very_last,
                                  [b_VB[par] if which == 0 else b_vm, b_P[pi]], b_bank[bkx], very_first)
                    if nslot == 5:
                        finish_unit(ob, lb, OT[:, 16 + hh, :])
                        if hh == 3:
                            ro = own_row(o)
                            dma_split("sp", OTS.rearrange("(n p) t -> p n t", p=128)[:, :, ro:ro + T], OT[:], 5,
                                      reads=[b_OT], pwrites=[b_OTS])
                st_.s1, st_.s2 = s1, s2
                return st_

            for hh in range(4):
                shared = {}
                for nslot in range(6):
                    steps.append(b_step(hh, nslot, shared))
            return steps

        steps = []
        for ti, o in enumerate(cfg["tiles"]):
            steps.extend(tile_steps(ti, o))
        for i, st_ in enumerate(steps):
            if i == 0:
                if st_.prep:
                    st_.prep()
                st_.s1()
            if i + 1 < len(steps):
                nx = steps[i + 1]
                if nx.prep:
                    nx.prep()
                nx.s1()
            st_.s2()


    NBLK = OWN // 128
    SLOTS = sb("SLOTS", [128, NBLK * 2], I32); b_SLOTS = Buf("SLOTS")
    Asum = sb("Asum", [128, 64], BF16); b_Asum = Buf("Asum")
    Umat = sb("Umat", [128, 128], BF16)
    ECi = sb("ECi", [128, 64], I32)
    ECf = sb("ECf", [128, 64], F32)
    tokid = sb("tokid", [128, NBLK], I32)
    tokid2 = sb("tokid2", [128, NBLK], I32)
    b_c3 = Buf("c3")

    def phase3():
        S.op("pool", lambda h: h.memset(Asum[:], 0.0), writes=[b_Asum])
        S.op("pool", lambda h: h.memset(Umat[:], 1.0), writes=[b_c3])
        b_U = Buf("U")
        S.op("pool", lambda h: h.affine_select(out=Umat[:], in_=Umat[:], pattern=[[1, 128]], compare_op=ALU.is_gt,
                                               fill=0.0, base=0, channel_multiplier=-1), reads=[b_c3], writes=[b_U])
        S.op("pool", lambda h: h.iota(ECi[:], pattern=[[CAP, 64]], base=0, channel_multiplier=0), pwrites=[b_c3])
        S.op("pool", lambda h: h.iota(tokid[:], pattern=[[128, NBLK]], base=0, channel_multiplier=1), pwrites=[b_c3])
        S.op("pool", lambda h: h.iota(tokid2[:], pattern=[[128, NBLK]], base=YT_STRIDE, channel_multiplier=1), pwrites=[b_c3])
        b_EC = Buf("EC")
        S.op("pool", lambda h: h.tensor_copy(out=ECf[:], in_=ECi[:]), reads=[b_c3], writes=[b_EC])
        zt = sb("zt", [128, D], BF16); b_zt = Buf("zt")
        S.op("pool", lambda h: h.memset(zt[:], 0.0), writes=[b_zt])
        S.op("sp", lambda h: h.dma_start(out=HN[OWN:OWN + 128, :], in_=zt[:]), reads=[b_zt], pwrites=[b_HN], dma=True)
        initI = sb("initI", [128, 128, 4], I32); b_initI = Buf("initI")
        S.op("pool", lambda h: h.memset(initI[:], 0), writes=[b_initI])
        b_initI2 = Buf("initI2")
        S.op("pool", lambda h: h.memset(initI[:, :, 0:1], OWN), reads=[b_initI], writes=[b_initI2])
        S.op("pool", lambda h: h.iota(initI[:, :, 2:3], pattern=[[1, 128], [1, 1]], base=2 * YT_STRIDE, channel_multiplier=128),
             reads=[b_initI], pwrites=[b_initI2])
        S.op("sp", lambda h: h.dma_start(out=SLOTI.rearrange("(p j) c -> p j c", p=128), in_=initI[:]),
             reads=[b_initI, b_initI2], writes=[b_SLOTI], dma=True)
        g2b = sb("g2b", [128, D], F32); b_g2b = Buf("g2b")
        S.op("sp", lambda h: h.dma_start(out=g2b[:], in_=g2[0, :].partition_broadcast(128)), writes=[b_g2b], dma=True)
        brb = sb("brb", [128, 72], F32)
        wr_sb = sb("wr_sb", [128, KC, 72], F32)
        bg_sb = sb("bg_sb", [128, 32], F32)
        b_rc = Buf("rconst")
        S.op("sp", lambda h: h.dma_start(out=brb[:], in_=br[0, :].partition_broadcast(128)), writes=[b_rc], dma=True)
        S.op("sp", lambda h: h.dma_start(out=wr_sb[:].rearrange("p k c -> p (k c)"), in_=wr), pwrites=[b_rc], dma=True)
        S.op("sp", lambda h: h.dma_start(out=bg_sb[:], in_=bgT), pwrites=[b_rc], dma=True)

        xT = sb("xT3", [128, KC, T], BF16); b_xT = Buf("xT3")
        OTt = sb("OTt", [128, 20, T], BF16); b_OTt = Buf("OTt")
        GA = sb("GA", [128, 4, T], F32); b_GA = Buf("GA")
        GB = sb("GB", [128, 4, T], F32); b_GB = Buf("GB")
        MT = sb("MT", [128, KC, T], BF16); b_MT = Buf("MT")
        Hh = sb("Hh", [128, 4, D], F32); b_Hh = [Buf(f"Hh{i}") for i in range(4)]
        xres = [sb(f"xres{i}", [128, 512], F32) for i in range(4)]
        b_xres = [Buf(f"xres{i}") for i in range(4)]
        xrot = Rot(range(4))
        junk = sb("junk3", [128, D], BF16)
        hnf = sb("hnf", [128, D], F32); b_hnf = Buf("hnf")
        hnb = sb("hnb", [128, D], BF16); b_hnb = Buf("hnb")
        hnT = sb("hnT", [128, KC, 128], F32); b_hnT = Buf("hnT")
        st = sb("st3", [128, 8], F32)
        b_ssq, b_ms, b_rstd = Buf("ssq3"), Buf("ms3"), Buf("rstd3")
        LG = sb("LG", [128, 72], F32); b_LG = Buf("LG")
        rt = sb("rt", [128, 16], F32)
        ohg = sb("ohg", [128, 8], F32)
        eg = sb("eg", [128, 8], F32)
        tmp64 = sb("tmp64", [128, 64], F32)
        lsel = sb("lsel", [128, 8], F32)
        msk = sb("msk", [128, 8], F32)
        oh1 = sb("oh1", [128, 8], F32)
        oh2 = sb("oh2", [128, 8], F32)
        A1 = sb("A1", [128, 64], F32)
        A2 = sb("A2", [128, 64], F32)
        Abf = sb("Abf", [128, 64], BF16)
        rk = sb("rk", [128, 64], F32)
        junk64 = sb("junk64", [128, 64], F32)
        sl_f = sb("sl_f", [128, 2], F32)
        sl_i = sb("sl_i", [128, 2], I32)
        info = [sb(f"info{i}", [128, 4], I32) for i in range(2)]
        bR = {n: Buf("r_" + n) for n in "mg ohg negmg se pg lsel m1 oh1 negm1 msk m2 oh2 e2 den rden w1 w2 A1 A2 Abf rk slf sli info0 info1 tmp64 A2j".split()}

        def dve(fn, reads, writes, pw=()):
            S.op("dve", fn, reads=[bR[r] if isinstance(r, str) else r for r in reads],
                 writes=[bR[w] if isinstance(w, str) else w for w in writes],
                 pwrites=[bR[w] if isinstance(w, str) else w for w in pw])

        def proj_ws(w, cg, nkg, rhs_fn, rhs_reads, bset):
            for kg in range(nkg):
                wt, b_wt = w_get()
                for cc in range(4):
                    for kk in range(4):
                        first = (kg == 0 and kk == 0)
                        pe_mm(banks[bset + cc][:], wt[:, kk, cc * 128:(cc + 1) * 128], rhs_fn(kg * 4 + kk),
                              first, (kg == nkg - 1 and kk == 3), [b_wt] + rhs_reads, b_bank[bset + cc], first)

        for _ in cfg["tiles"]:
            for fg in range(4):
                w_push([piece(w_gate, kg, fg) for kg in range(4)] + [piece(w_gate, kg, 4 + fg) for kg in range(4)]
                       + [piece(w_pa, kg, fg) for kg in range(4)] + [piece(w_pb, 0, fg)])
            for cg in range(4):
                w_push([piece(w_out, kg, cg) for kg in range(4)])
        def tail1(tb, ro, blk):
            r0 = ro + tb * 128
            S.op("sp", lambda h, tb=tb, r0=r0: h.dma_start(out=HS[r0:r0 + 128, :], in_=Hh[:, tb, :]),
                 reads=[b_Hh[tb]], pwrites=[b_HS], dma=True)
            S.op("act", lambda h, tb=tb: h.activation(out=junk[:], in_=Hh[:, tb, :], func=AF.Square, accum_out=st[:, 0:1]),
                 reads=[b_Hh[tb]], writes=[b_ssq])
            S.op("dve", lambda h: h.tensor_scalar(out=st[:, 1:2], in0=st[:, 0:1], scalar1=1.0 / D, scalar2=EPS,
                                                  op0=ALU.mult, op1=ALU.add), reads=[b_ssq], writes=[b_ms])
            S.op("pool", lambda h: h.tensor_tensor(out=st[:, 2:3], in0=st[:, 1:2], in1=neghalf[:], op=ALU.pow),
                 reads=[b_ms, b_const], writes=[b_rstd])
            S.op("dve", lambda h, tb=tb: h.scalar_tensor_tensor(out=hnf[:], in0=Hh[:, tb, :], scalar=st[:, 2:3], in1=g2b[:],
                                                               op0=ALU.mult, op1=ALU.mult),
                 reads=[b_Hh[tb], b_rstd, b_g2b], writes=[b_hnf])
            S.op("act", lambda h: h.copy(out=hnb[:], in_=hnf[:]), reads=[b_hnf], writes=[b_hnb])
            S.op("sp", lambda h, r0=r0: h.dma_start(out=HN[r0:r0 + 128, :], in_=hnb[:]), reads=[b_hnb], pwrites=[b_HN], dma=True)

        def tail2(tb, ro, blk):
            for q4 in range(4):
                bk = q4
                for k4 in range(4):
                    k = q4 * 4 + k4
                    S.op("pe", lambda h, k=k, k4=k4, bk=bk: h.transpose(out=banks[bk][:, k4 * 128:(k4 + 1) * 128],
                                                                      in_=hnf[:, k * 128:(k + 1) * 128], identity=identf[:]),
                         reads=[b_hnf, b_identf], writes=[b_bank[bk]] if k4 == 0 else (), pwrites=() if k4 == 0 else [b_bank[bk]])
                evac_copy(hnT[:, q4 * 4:(q4 + 1) * 4, :], banks[bk][:].rearrange("p (k t) -> p k t", k=4),
                          reads=[b_bank[bk]], writes=[b_hnT] if q4 == 0 else (), pwrites=() if q4 == 0 else [b_hnT])
            lb_ = 4
            for k in range(KC):
                pe_mm(banks[lb_][:, 0:72], hnT[:, k, :], wr_sb[:, k, :], k == 0, k == KC - 1, [b_hnT, b_rc], b_bank[lb_], k == 0)
            dve(lambda h: h.tensor_tensor(out=LG[:], in0=banks[lb_][:, 0:72], in1=brb[:], op=ALU.add), [b_bank[lb_], b_rc], [b_LG])
            dve(lambda h: h.tensor_reduce(out=rt[:, 0:1], in_=LG[:, 0:8], axis=AX.X, op=ALU.max), [b_LG], ["mg"])
            dve(lambda h: h.tensor_scalar(out=ohg[:], in0=LG[:, 0:8], scalar1=rt[:, 0:1], scalar2=None, op0=ALU.is_equal), [b_LG, "mg"], ["ohg"])
            dve(lambda h: h.tensor_scalar(out=rt[:, 1:2], in0=rt[:, 0:1], scalar1=-1.0, scalar2=None, op0=ALU.mult), ["mg"], ["negmg"])
            S.op("act", lambda h: h.activation(out=eg[:], in_=LG[:, 0:8], func=AF.Exp, bias=rt[:, 1:2], accum_out=rt[:, 2:3]),
                 reads=[b_LG, bR["negmg"]], writes=[bR["se"]])
            dve(lambda h: h.reciprocal(out=rt[:, 3:4], in_=rt[:, 2:3]), ["se"], ["pg"])
            dve(lambda h: h.tensor_tensor(out=tmp64[:].rearrange("p (g j) -> p g j", g=8),
                                          in0=LG[:, 8:72].rearrange("p (g j) -> p g j", g=8),
                                          in1=ohg[:].unsqueeze(2).broadcast_to([128, 8, 8]), op=ALU.mult), [b_LG, "ohg"], ["tmp64"])
            dve(lambda h: h.tensor_reduce(out=lsel[:], in_=tmp64[:].rearrange("p (g j) -> p j g", g=8), axis=AX.X, op=ALU.add),
                ["tmp64"], ["lsel"])
            dve(lambda h: h.tensor_reduce(out=rt[:, 4:5], in_=lsel[:], axis=AX.X, op=ALU.max), ["lsel"], ["m1"])
            dve(lambda h: h.tensor_scalar(out=oh1[:], in0=lsel[:], scalar1=rt[:, 4:5], scalar2=None, op0=ALU.is_equal), ["lsel", "m1"], ["oh1"])
            dve(lambda h: h.tensor_scalar(out=rt[:, 5:6], in0=rt[:, 4:5], scalar1=-1.0, scalar2=None, op0=ALU.mult), ["m1"], ["negm1"])
            dve(lambda h: h.scalar_tensor_tensor(out=msk[:], in0=oh1[:], scalar=-1e30, in1=lsel[:], op0=ALU.mult, op1=ALU.add),
                ["oh1", "lsel"], ["msk"])
            dve(lambda h: h.tensor_reduce(out=rt[:, 6:7], in_=msk[:], axis=AX.X, op=ALU.max), ["msk"], ["m2"])
            dve(lambda h: h.tensor_scalar(out=oh2[:], in0=msk[:], scalar1=rt[:, 6:7], scalar2=None, op0=ALU.is_equal), ["msk", "m2"], ["oh2"])
            S.op("act", lambda h: h.activation(out=rt[:, 7:8], in_=rt[:, 6:7], func=AF.Exp, bias=rt[:, 5:6]),
                 reads=[bR["m2"], bR["negm1"]], writes=[bR["e2"]])
            dve(lambda h: h.tensor_scalar(out=rt[:, 8:9], in0=rt[:, 7:8], scalar1=1.0, scalar2=None, op0=ALU.add), ["e2"], ["den"])
            dve(lambda h: h.reciprocal(out=rt[:, 9:10], in_=rt[:, 8:9]), ["den"], ["rden"])
            dve(lambda h: h.tensor_tensor(out=rt[:, 10:11], in0=rt[:, 9:10], in1=rt[:, 3:4], op=ALU.mult), ["rden", "pg"], ["w1"])
            dve(lambda h: h.tensor_tensor(out=rt[:, 11:12], in0=rt[:, 10:11], in1=rt[:, 7:8], op=ALU.mult), ["w1", "e2"], ["w2"])
            dve(lambda h: h.tensor_tensor(out=A1[:].rearrange("p (g j) -> p g j", g=8),
                                          in0=ohg[:].unsqueeze(2).broadcast_to([128, 8, 8]),
                                          in1=oh1[:].unsqueeze(1).broadcast_to([128, 8, 8]), op=ALU.mult), ["ohg", "oh1"], ["A1"])
            dve(lambda h: h.tensor_tensor(out=A2[:].rearrange("p (g j) -> p g j", g=8),
                                          in0=ohg[:].unsqueeze(2).broadcast_to([128, 8, 8]),
                                          in1=oh2[:].unsqueeze(1).broadcast_to([128, 8, 8]), op=ALU.mult), ["ohg", "oh2"], ["A2"])
            dve(lambda h: h.tensor_tensor(out=Abf[:], in0=A1[:], in1=A2[:], op=ALU.add), ["A1", "A2"], ["Abf"])

        def tail3(tb, ro, blk):
            rb_ = 5
            pe_mm(banks[rb_][:, 0:64], Umat[:], Abf[:], True, False, [b_U, bR["Abf"]], b_bank[rb_], True)
            pe_mm(banks[rb_][:, 0:64], ones_bf[:], Asum[:], False, True, [b_const, b_Asum], b_bank[rb_], False)
            dve(lambda h: h.tensor_tensor(out=rk[:], in0=banks[rb_][:, 0:64], in1=ECf[:], op=ALU.add), [b_bank[rb_], b_EC], ["rk"])
            S.op("pool", lambda h: h.tensor_tensor(out=Asum[:], in0=Asum[:], in1=Abf[:], op=ALU.add),
                 reads=[bR["Abf"], b_Asum], writes=[b_Asum])
            dve(lambda h: h.tensor_tensor(out=tmp64[:], in0=rk[:], in1=A1[:], op=ALU.mult), ["rk", "A1"], ["tmp64"])
            dve(lambda h: h.tensor_reduce(out=sl_f[:, 0:1], in_=tmp64[:], axis=AX.X, op=ALU.add), ["tmp64"], [], ["slf"])
            dve(lambda h: h.tensor_tensor(out=junk64[:], in0=rk[:], in1=A2[:], op=ALU.mult), ["rk", "A2"], ["A2j"])
            dve(lambda h: h.tensor_reduce(out=sl_f[:, 1:2], in_=junk64[:], axis=AX.X, op=ALU.add), ["A2j"], [], ["slf"])
            dve(lambda h: h.tensor_copy(out=sl_i[:], in_=sl_f[:]), ["slf"], ["sli"])
            dve(lambda h, blk=blk: h.tensor_copy(out=SLOTS[:, blk * 2:blk * 2 + 2], in_=sl_i[:]), ["sli"], [], [b_SLOTS])
            for kx in range(2):
                nm = f"info{kx}"
                dve(lambda h, kx=kx, blk=blk: h.tensor_copy(out=info[kx][:, 0:1], in_=tokid[:, blk:blk + 1]), [b_c3], [nm])
                dve(lambda h, kx=kx, blk=blk: h.tensor_copy(out=info[kx][:, 2:3], in_=(tokid if kx == 0 else tokid2)[:, blk:blk + 1]),
                    [b_c3, nm], [nm])
                dve(lambda h, kx=kx: h.tensor_copy(out=info[kx][:, 1:2].bitcast(F32), in_=rt[:, 10 + kx:11 + kx]),
                    ["w1", "w2", nm], [nm])
                S.op("pool", lambda h, kx=kx: h.indirect_dma_start(
                    out=SLOTI, out_offset=bass.IndirectOffsetOnAxis(ap=sl_i[:, kx:kx + 1], axis=0),
                    in_=info[kx][:], in_offset=None),
                    reads=[bR[nm], bR["sli"], b_SLOTI], pwrites=[b_SLOTI], dma=True)

        pending = []
        done = set()

        def run_slot(slot):
            if not pending:
                return
            for part, fn_, b in ((3, tail3, slot - 2), (2, tail2, slot - 1), (1, tail1, slot)):
                if 0 <= b < 4:
                    key = (pending[b][1], b, part)
                    if key in done:
                        continue
                    done.add(key)
                    fn_(*pending[b])

        blk = 0
        for ti, o in enumerate(cfg["tiles"]):
            ro = own_row(o)
            dma_split("sp", xT[:], XNT.rearrange("(k p) t -> p k t", p=128)[:, :, o:o + T], 4, reads=[b_XNT], writes=[b_xT])
            dma_split("sp", OTt[:], OTS.rearrange("(n p) t -> p n t", p=128)[:, :, ro:ro + T], 5, reads=[b_OTS], writes=[b_OTt])
            for fg in range(4):
                proj_ws(w_gate, fg, 4, lambda k: xT[:, k, :], [b_xT], 0)
                for cc in range(4):
                    c_ = fg * 4 + cc
                    S.op("act", lambda h, cc=cc, c_=c_: h.activation(out=GA[:, cc, :], in_=banks[cc][:], func=AF.Sigmoid,
                                                                     bias=bg_sb[:, c_:c_ + 1]),
                         reads=[b_bank[cc], b_rc], writes=[b_GA] if cc == 0 else (), pwrites=() if cc == 0 else [b_GA])
                proj_ws(w_gate, 4 + fg, 4, lambda k: xT[:, k, :], [b_xT], 4)
                for cc in range(4):
                    c_ = 16 + fg * 4 + cc
                    S.op("act", lambda h, cc=cc, c_=c_: h.activation(out=GB[:, cc, :], in_=banks[4 + cc][:], func=AF.Sigmoid,
                                                                     bias=bg_sb[:, c_:c_ + 1]),
                         reads=[b_bank[4 + cc], b_rc], writes=[b_GB] if cc == 0 else (), pwrites=() if cc == 0 else [b_GB])
                proj_ws(w_pa, fg, 4, lambda k: OTt[:, k, :], [b_OTt], 0)
                for cc in range(4):
                    S.op("dve", lambda h, cc=cc: h.tensor_tensor(out=GA[:, cc, :], in0=banks[cc][:], in1=GA[:, cc, :], op=ALU.mult),
                         reads=[b_bank[cc], b_GA], pwrites=[b_GA])
                proj_ws(w_pb, fg, 1, lambda k: OTt[:, 16 + k, :], [b_OTt], 4)
                for cc in range(4):
                    S.op("dve", lambda h, cc=cc: h.tensor_tensor(out=GB[:, cc, :], in0=banks[4 + cc][:], in1=GB[:, cc, :], op=ALU.mult),
                         reads=[b_bank[4 + cc], b_GB], pwrites=[b_GB])
                S.op("pool", lambda h, fg=fg: h.tensor_tensor(out=MT[:, fg * 4:(fg + 1) * 4, :], in0=GA[:], in1=GB[:], op=ALU.add),
                     reads=[b_GA, b_GB], writes=[b_MT] if fg == 0 else (), pwrites=() if fg == 0 else [b_MT])
                run_slot(fg)
            for cg in range(4):
                bset = (cg % 2) * 4
                for kg in range(4):
                    wt, b_wt = w_get()
                    for tb in range(4):
                        for kk in range(4):
                            first = (kg == 0 and kk == 0)
                            pe_mm(banks[bset + tb][:], MT[:, kg * 4 + kk, tb * 128:(tb + 1) * 128], wt[:, kk, :],
                                  first, (kg == 3 and kk == 3), [b_wt, b_MT], b_bank[bset + tb], first)
                for tb in range(4):
                    xi = xrot.next()
                    r0 = o + tb * 128
                    S.op("sp", lambda h, xi=xi, r0=r0, cg=cg: h.dma_start(out=xres[xi][:], in_=xin[r0:r0 + 128, cg * 512:(cg + 1) * 512]),
                         writes=[b_xres[xi]], dma=True)
                    S.op("dve", lambda h, xi=xi, tb=tb, cg=cg, bset=bset: h.tensor_tensor(
                        out=Hh[:, tb, cg * 512:(cg + 1) * 512], in0=banks[bset + tb][:], in1=xres[xi][:], op=ALU.add),
                        reads=[b_bank[bset + tb], b_xres[xi]], writes=[b_Hh[tb]] if cg == 0 else (), pwrites=() if cg == 0 else [b_Hh[tb]])
                run_slot(4 + cg)
            pending = [(tb, ro, blk + tb) for tb in range(4)]
            blk += 4
        for slot in range(8):
            run_slot(slot)

    def phase4():
        si = [sb(f"si{i}", [128, 4], I32) for i in range(4)]; b_si = [Buf(f"si{i}") for i in range(4)]
        X = [sb(f"X{i}", [128, D], BF16) for i in range(4)]; b_X = [Buf(f"X{i}") for i in range(4)]
        XT = [sb(f"XT{i}", [128, KC, 256], BF16) for i in range(2)]; b_XTe = [Buf(f"XT{i}") for i in range(2)]
        S1 = sb("S1", [128, 1024], F32); b_S1 = Buf("S1")
        HT = [sb(f"HT{i}", [128, 4, 256], BF16) for i in range(2)]; b_HT = [Buf(f"HT{i}") for i in range(2)]
        Y = [sb(f"Y{i}", [128, D], BF16) for i in range(4)]; b_Y = [Buf(f"Y{i}") for i in range(4)]
        yev = Rot(("act", "dve"))
        for e in cfg["experts"]:
            w_push([piece(w1, e * 4 + kg, 0) for kg in range(4)] + [piece(w3, e * 4 + kg, 0) for kg in range(4)]
                   + [piece(w2, e, cg) for cg in range(4)])
        for ei, e in enumerate(cfg["experts"]):
            xt, b_xt = XT[ei % 2], b_XTe[ei % 2]
            sis = []
            for sb_ in range(2):
                i4 = (ei % 2) * 2 + sb_
                sis.append(i4)
                r0 = e * CAP + sb_ * 128
                S.op("sp", lambda h, i4=i4, r0=r0: h.dma_start(out=si[i4][:], in_=SLOTI[r0:r0 + 128, :]),
                     reads=[b_SLOTI], writes=[b_si[i4]], dma=True)
                S.op("pool", lambda h, i4=i4: h.indirect_dma_start(
                    out=X[i4][:], out_offset=None, in_=HN, in_offset=bass.IndirectOffsetOnAxis(ap=si[i4][:, 0:1], axis=0)),
                    reads=[b_si[i4], b_HN], writes=[b_X[i4]], dma=True)
                for half in range(2):
                    bk = 4 + (2 * sb_ + half) % 4
                    tpv = banks[bk][:].bitcast(BF16)
                    for k8 in range(8):
                        k = half * 8 + k8
                        S.op("pe", lambda h, i4=i4, k=k, k8=k8, tpv=tpv: h.transpose(
                            out=tpv[:, k8 * 128:(k8 + 1) * 128], in_=X[i4][:, k * 128:(k + 1) * 128], identity=ident[:]),
                            reads=[b_X[i4], b_const], writes=[b_bank[bk]] if k8 == 0 else (), pwrites=() if k8 == 0 else [b_bank[bk]])
                    first_w = (sb_ == 0 and half == 0)
                    evac_copy(xt[:, half * 8:(half + 1) * 8, sb_ * 128:(sb_ + 1) * 128], tpv.rearrange("p (k t) -> p k t", k=8),
                              reads=[b_bank[bk]], writes=[b_xt] if first_w else (), pwrites=() if first_w else [b_xt])
            for which in range(2):
                for kg in range(4):
                    wt, b_wt = w_get()
                    for cc in range(4):
                        bk = which * 2 + cc // 2
                        for kk in range(4):
                            vfirst = (kg == 0 and kk == 0 and cc % 2 == 0)
                            vlast = (kg == 3 and kk == 3 and cc % 2 == 1)
                            pe_mm(banks[bk][:, (cc % 2) * 256:(cc % 2 + 1) * 256], wt[:, kk, cc * 128:(cc + 1) * 128],
                                  xt[:, kg * 4 + kk, :], vfirst, vlast, [b_wt, b_xt], b_bank[bk], vfirst)
            ht, b_ht = HT[ei % 2], b_HT[ei % 2]
            for hb_ in range(2):
                S.op("act", lambda h, hb_=hb_: h.activation(out=S1[:, hb_ * 512:(hb_ + 1) * 512], in_=banks[hb_][:], func=AF.Silu),
                     reads=[b_bank[hb_]], writes=[b_S1] if hb_ == 0 else (), pwrites=() if hb_ == 0 else [b_S1])
            for hb_ in range(2):
                S.op("dve", lambda h, hb_=hb_, ht=ht: h.tensor_tensor(
                    out=ht[:, hb_ * 2:(hb_ + 1) * 2, :], in0=banks[2 + hb_][:].rearrange("p (c t) -> p c t", c=2),
                    in1=S1[:, hb_ * 512:(hb_ + 1) * 512].rearrange("p (c t) -> p c t", c=2), op=ALU.mult),
                    reads=[b_bank[2 + hb_], b_S1], writes=[b_ht] if hb_ == 0 else (), pwrites=() if hb_ == 0 else [b_ht])
            for cg in range(4):
                wt, b_wt = w_get()
                for sb_ in range(2):
                    i4 = sis[sb_]
                    bk = 4 + (2 * cg + sb_) % 4
                    for kk in range(4):
                        pe_mm(banks[bk][:], ht[:, kk, sb_ * 128:(sb_ + 1) * 128], wt[:, kk, :], kk == 0, kk == 3,
                              [b_wt, b_ht], b_bank[bk], kk == 0)
                    wcol = si[i4][:, 1:2].bitcast(F32)
                    ye = yev.next()
                    wr_ = dict(writes=[b_Y[i4]] if cg == 0 else (), pwrites=() if cg == 0 else [b_Y[i4]])
                    if ye == "act":
                        S.op("act", lambda h, i4=i4, cg=cg, bk=bk, wcol=wcol: h.activation(
                            out=Y[i4][:, cg * 512:(cg + 1) * 512], in_=banks[bk][:], func=AF.Copy, scale=wcol),
                            reads=[b_bank[bk], b_si[i4]], **wr_)
                    else:
                        S.op("dve", lambda h, i4=i4, cg=cg, bk=bk, wcol=wcol: h.tensor_scalar(
                            out=Y[i4][:, cg * 512:(cg + 1) * 512], in0=banks[bk][:], scalar1=wcol, scalar2=None, op0=ALU.mult),
                            reads=[b_bank[bk], b_si[i4]], **wr_)
            for sb_ in range(2):
                i4 = sis[sb_]
                r0 = e * CAP + sb_ * 128
                S.op("pool", lambda h, i4=i4: h.indirect_dma_start(
                    out=YS, out_offset=bass.IndirectOffsetOnAxis(ap=si[i4][:, 2:3], axis=0), in_=Y[i4][:], in_offset=None),
                    reads=[b_Y[i4], b_si[i4]], pwrites=[b_YS], dma=True)

    def phase5():
        gfb = sb("gfb", [128, D], F32); b_gfb = Buf("gfb")
        S.op("sp", lambda h: h.dma_start(out=gfb[:], in_=gf[0, :].partition_broadcast(128)), writes=[b_gfb], dma=True)
        NB5 = 4
        hb = [sb(f"hb{i}", [128, D], F32) for i in range(NB5)]; b_hb = [Buf(f"hb{i}") for i in range(NB5)]
        yg = [sb(f"yg{i}", [128, D], BF16) for i in range(2 * NB5)]; b_yg = [Buf(f"yg{i}") for i in range(2 * NB5)]
        ob = [sb(f"ob{i}", [128, D], F32) for i in range(NB5)]; b_ob = [Buf(f"ob{i}") for i in range(NB5)]
        junk = sb("junk5", [128, D], BF16)
        st = [sb(f"st5{i}", [128, 4], F32) for i in range(NB5)]
        b_ssq = [Buf(f"ssq5{i}") for i in range(NB5)]; b_ms = [Buf(f"ms5{i}") for i in range(NB5)]; b_rstd = [Buf(f"rstd5{i}") for i in range(NB5)]
        nb = sum(1 for _ in cfg["tiles"]) * 4
        blocks = []
        for o in cfg["tiles"]:
            for tb in range(4):
                blocks.append(own_row(o) + tb * 128)
        def stage_a(bi):
            r0 = blocks[bi]
            i = bi % NB5
            S.op("sp", lambda h: h.dma_start(out=hb[i][:], in_=HS[r0:r0 + 128, :]), reads=[b_HS], writes=[b_hb[i]], dma=True)
            for kx in range(2):
                j = i * 2 + kx
                S.op("sp", lambda h, j=j, kx=kx: h.dma_start(out=yg[j][:], in_=YS[kx * YT_STRIDE + r0:kx * YT_STRIDE + r0 + 128, :]),
                     reads=[b_YS], writes=[b_yg[j]], dma=True)
            S.op("dve", lambda h: h.tensor_tensor(out=hb[i][:], in0=hb[i][:], in1=yg[i * 2][:], op=ALU.add),
                 reads=[b_hb[i], b_yg[i * 2]], writes=[b_hb[i]])
            S.op("dve", lambda h: h.tensor_tensor(out=hb[i][:], in0=hb[i][:], in1=yg[i * 2 + 1][:], op=ALU.add),
                 reads=[b_hb[i], b_yg[i * 2 + 1]], writes=[b_hb[i]])
            S.op("act", lambda h: h.activation(out=junk[:], in_=hb[i][:], func=AF.Square, accum_out=st[i][:, 0:1]),
                 reads=[b_hb[i]], writes=[b_ssq[i]])

        def stage_b(bi):
            r0 = blocks[bi]
            i = bi % NB5
            S.op("dve", lambda h: h.tensor_scalar(out=st[i][:, 1:2], in0=st[i][:, 0:1], scalar1=1.0 / D, scalar2=EPS,
                                                  op0=ALU.mult, op1=ALU.add), reads=[b_ssq[i]], writes=[b_ms[i]])
            S.op("pool", lambda h: h.tensor_tensor(out=st[i][:, 2:3], in0=st[i][:, 1:2], in1=neghalf[:], op=ALU.pow),
                 reads=[b_ms[i], b_const], writes=[b_rstd[i]])
            S.op("dve", lambda h: h.scalar_tensor_tensor(out=ob[i][:], in0=hb[i][:], scalar=st[i][:, 2:3], in1=gfb[:],
                                                         op0=ALU.mult, op1=ALU.mult),
                 reads=[b_hb[i], b_rstd[i], b_gfb], writes=[b_ob[i]])
            S.op("sp", lambda h: h.dma_start(out=yout[r0:r0 + 128, :], in_=ob[i][:]), reads=[b_ob[i]], pwrites=[b_yout], dma=True)

        LAG = 2
        for bi in range(min(LAG, len(blocks))):
            stage_a(bi)
        for bi in range(len(blocks)):
            if bi + LAG < len(blocks):
                stage_a(bi + LAG)
            stage_b(bi)

    if 1 in cfg["phases"]:
        run_phase(phase1, 8)
    if 2 in cfg["phases"]:
        run_phase(phase2)
    if 3 in cfg["phases"]:
        run_phase(phase3)
    if 4 in cfg["phases"]:
        run_phase(phase4, 16)
    if 5 in cfg["phases"]:
        run_phase(phase5)
    S.barrier()
    emit_program(nc, S, es)
    es.close()
    return nc


def _bf16(a):
    return np.ascontiguousarray(a.astype(ml_dtypes.bfloat16))


def position_tables():
    k = np.arange(128)[:, None].astype(np.float64)
    sl_a = np.power(2.0, -8.0 * (np.arange(16) + 1) / 16)
    sl_b = np.power(2.0, -8.0 * (np.arange(12) + 1) / 12)
    eA = np.zeros((128, 4, 3, 4, 128))
    q = np.arange(128)[None, :].astype(np.float64)
    for kvh in range(4):
        for s in range(3):
            rel = (128 * s - 128 + k) - q
            for g in range(4):
                e = np.exp(-sl_a[kvh * 4 + g] * np.abs(rel))
                eA[:, kvh, s, g, :] = np.where(np.abs(rel) <= 128, e, 0.0)
    eB01 = np.zeros((128, 2, 4, 2, 128))
    for gi, r in ((0, 1), (1, 4)):
        for hh in range(4):
            for s in range(2):
                rel = (k + 128 * s - 64) - q
                e = np.exp(-sl_b[gi * 4 + hh] * r * np.abs(rel))
                eB01[:, gi, hh, s, :] = np.where(np.abs(rel) <= 64, e, 0.0)
    eB2 = np.zeros((128, 4, 2, 32))
    q32 = np.arange(32)[None, :].astype(np.float64)
    for hh in range(4):
        for s in range(2):
            rel = (k + 128 * s - 64) - q32
            e = np.exp(-sl_b[8 + hh] * 16 * np.abs(rel))
            e = np.where(np.abs(rel) <= 64, e, 0.0)
            if s == 1:
                e = np.where(k < 32, e, 0.0)
            eB2[:, hh, s, :] = e
    eB = np.concatenate([eB01.reshape(128, -1), eB2.reshape(128, -1)], axis=1)
    return _bf16(eA.reshape(128, -1)), _bf16(eB)


def valid_mask_table(valid_ext, tiles):
    p = np.arange(128)
    out = np.zeros((len(tiles), 128, 51), np.float32)
    for ti, o in enumerate(tiles):
        cols = []
        for j in range(6):
            cols.append(valid_ext[o - 128 + 128 * j + p])
        for j in range(5):
            cols.append(valid_ext[o - 64 + 128 * j + p])
        for ph in range(4):
            for s in range(2):
                cols.append(valid_ext[o - 256 + ph + 512 * s + 4 * p])
        for ph in range(16):
            for s in range(2):
                tok = o - 1024 + ph + 2048 * s + 16 * p
                if s == 0:
                    cols.append(valid_ext[tok])
                else:
                    cols.append(np.where(p < 32, valid_ext[np.minimum(tok, len(valid_ext) - 1)], 0.0))
        out[ti] = np.stack(cols, axis=1)
    full = np.repeat(out[:, :, :, None], 128, axis=3)
    return _bf16(full.reshape(len(tiles) * 128, 51 * 128))


def make_shared_inputs(inp):
    f = lambda a: np.ascontiguousarray(np.asarray(a, dtype=np.float32))
    eA, eB = position_tables()
    w_rg = f(inp["w_router_group"])[0]
    w_re = f(inp["w_router_expert"])[0]
    wrm = np.concatenate([w_rg, w_re.transpose(1, 0, 2).reshape(D, 64)], axis=1)
    wrm = wrm.reshape(KC, 128, 72).transpose(1, 0, 2).reshape(128, KC * 72)
    brm = np.concatenate([f(inp["b_router_group"])[0], f(inp["b_router_expert"])[0].reshape(64)])[None, :]
    return dict(
        w_in=f(inp["w_in"])[0], w_gate=f(inp["w_gate"])[0], w_pa=f(inp["w_proj_a"])[0], w_pb=f(inp["w_proj_b"])[0],
        w_out=f(inp["w_out"])[0],
        w1=f(inp["w_expert_gate"])[0].reshape(N_EXP * D, D_FF), w3=f(inp["w_expert_up"])[0].reshape(N_EXP * D, D_FF),
        w2=f(inp["w_expert_down"])[0].reshape(N_EXP * D_FF, D),
        g1=f(inp["norm1"]).reshape(1, D), g2=f(inp["norm2"]).reshape(1, D), gf=f(inp["norm_final"]).reshape(1, D),
        bgT=np.ascontiguousarray(f(inp["b_gate"])[0].reshape(32, 128).T), wr=np.ascontiguousarray(wrm), br=f(brm),
        sink=f(inp["attn_sink"]).reshape(1, 16), eA=eA, eB=eB,
    )


def make_core_inputs(inp, c, tiles):
    xp = np.asarray(inp["x_prompt"], dtype=np.float32)[0]
    xs = np.asarray(inp["x_sample"], dtype=np.float32)[c // 2]
    xin = np.zeros((EXT, D), np.float32)
    valid = np.zeros((EXT,), np.float32)
    p0 = 1024 * c - HALO
    lo, hi = max(p0, 0), min(p0 + P_EXT, xp.shape[0])
    xin[lo - p0:hi - p0] = xp[lo:hi]
    valid[lo - p0:hi - p0] = 1.0
    seq = xs if c % 2 == 0 else xs[::-1]
    n = S_OWN + HALO
    xin[P_EXT + HALO:P_EXT + HALO + n] = seq[0:n]
    valid[P_EXT + HALO:P_EXT + HALO + n] = 1.0
    return dict(xin=xin, vmask=valid_mask_table(valid, tiles))


_NC_CACHE = {}


def kernel(**inputs):
    cfg = default_cfg()
    if "nc" not in _NC_CACHE:
        _NC_CACHE["nc"] = build(cfg)
    nc = _NC_CACHE["nc"]
    shared = make_shared_inputs(inputs)
    in_maps = []
    for c in range(NCORES):
        m = dict(shared)
        m.update(make_core_inputs(inputs, c, cfg["tiles"]))
        in_maps.append(m)
    res = run_bass_kernel_spmd(nc, in_maps, core_ids=list(range(NCORES)))
    yp = np.zeros((1, 8192, D), np.float32)
    ys = np.zeros((4, 4096, D), np.float32)
    for c in range(NCORES):
        y = np.asarray(res.results[c]["yout"])
        yp[0, 1024 * c:1024 * c + 1024] = y[:P_OWN]
        ys[c // 2, 2048 * (c % 2):2048 * (c % 2) + 2048] = y[P_OWN:] if c % 2 == 0 else y[P_OWN:][::-1]
    return (yp, ys)
```

```python
import math
import numpy as np
import ml_dtypes
import concourse.bass as bass
import concourse.mybir as mybir
from concourse.bass_utils import run_bass_kernel_spmd

F32 = mybir.dt.float32
BF16 = mybir.dt.bfloat16
I32 = mybir.dt.int32
AF = mybir.ActivationFunctionType
ALU = mybir.AluOpType
AX = mybir.AxisListType

D = 2048
KC = D // 128
HEAD = 128
A_HEADS, A_KV, A_GROUP = 16, 4, 4
B_HPG = 4
DIL = ((128, 1), (512, 4), (2048, 16))
IN_COLS = 7680
N_EXP = 64
D_FF = 512
EPS = 1e-6
SCALE = HEAD ** -0.5
NCORES = 8
T = 512
HALO = 1024
P_OWN, S_OWN = 1024, 2048
P_EXT, S_EXT = P_OWN + 2 * HALO, S_OWN + 2 * HALO
EXT = P_EXT + S_EXT
OWN = P_OWN + S_OWN
CAP = 256
NSLOT = N_EXP * CAP
YT_STRIDE = OWN + 128
BIG_IDX = 1 << 24
SEM_ROLL = 20000


class Buf:
    __slots__ = ("name", "writers", "readers")

    def __init__(self, name):
        self.name = name
        self.writers = []
        self.readers = []


class Op:
    __slots__ = ("eng", "fn", "deps", "signal", "event", "is_dma", "idx")

    def __init__(self, eng, fn, is_dma):
        self.eng = eng
        self.fn = fn
        self.deps = []
        self.signal = False
        self.event = None
        self.is_dma = is_dma
        self.idx = -1


ENGS = ("pe", "act", "dve", "pool", "sp")


class Sched:
    def __init__(self):
        self.streams = {e: [] for e in ENGS}
        self.nops = 0

    def op(self, eng, fn, reads=(), writes=(), pwrites=(), dma=False):
        o = Op(eng, fn, dma)
        o.idx = self.nops
        self.nops += 1
        deps = []
        for b in reads:
            deps.extend(b.writers)
        for b in writes:
            deps.extend(b.writers)
            deps.extend(b.readers)
        for b in pwrites:
            deps.extend(b.readers)
        for b in reads:
            b.readers.append(o)
        for b in writes:
            b.writers = [o]
            b.readers = []
        for b in pwrites:
            if b.readers:
                b.writers = [o]
                b.readers = []
            else:
                b.writers.append(o)
        latest = {}
        out = []
        seen = set()
        for d in deps:
            if d is o or id(d) in seen:
                continue
            seen.add(id(d))
            if d.is_dma:
                out.append(d)
            else:
                if d.eng == eng and eng == "pe":
                    continue
                cur = latest.get(d.eng)
                if cur is None or d.idx > cur.idx:
                    latest[d.eng] = d
        out.extend(latest.values())
        o.deps = out
        self.streams[eng].append(o)
        return o

    def barrier(self):
        deps = []
        for e in ENGS:
            st = self.streams[e]
            if not st:
                continue
            last_c = None
            for o in reversed(st):
                if o.fn is None:
                    break
                if o.is_dma:
                    deps.append(o)
                elif last_c is None:
                    last_c = o
            if last_c is not None:
                deps.append(last_c)
        for e in ENGS:
            o = Op(e, None, False)
            o.idx = self.nops
            self.nops += 1
            o.deps = [d for d in deps]
            self.streams[e].append(o)


def emit_program(nc, sched, extra_ctx):
    for e in ENGS:
        for o in sched.streams[e]:
            for d in o.deps:
                d.signal = True
            if o.is_dma:
                o.signal = True
    n_dma_sems = {"sp": 16, "act": 4, "pool": 12, "dve": 2, "pe": 2}
    n_eng_sems = {}
    for e in ENGS:
        cnt = sum(1 for o in sched.streams[e] if o.signal and not o.is_dma)
        n_eng_sems[e] = max(1, (cnt + SEM_ROLL - 1) // SEM_ROLL)
    from contextlib import ExitStack
    with ExitStack() as st:
        eng_sems = {e: [st.enter_context(nc.semaphore(f"s_{e}_{i}")) for i in range(n_eng_sems[e])] for e in ENGS}
        dma_sems = {e: [st.enter_context(nc.semaphore(f"d_{e}_{i}")) for i in range(n_dma_sems[e])] for e in ENGS}
        for e in ENGS:
            cnt = 0
            dcnt = 0
            dma_last = [None] * n_dma_sems[e]
            dma_uses = [0] * n_dma_sems[e]
            for o in sched.streams[e]:
                if o.is_dma:
                    k = dcnt % n_dma_sems[e]
                    dcnt += 1
                    if dma_last[k] is not None:
                        o.deps.append(dma_last[k])
                    dma_uses[k] += 1
                    o.event = (dma_sems[e][k], 16 * dma_uses[k], 16)
                    dma_last[k] = o
                elif o.signal:
                    si = cnt // SEM_ROLL
                    o.event = (eng_sems[e][si], cnt % SEM_ROLL + 1, 1)
                    cnt += 1
        block = st.enter_context(nc.Block())

        def make_section(e):
            ops = sched.streams[e]

            def section(h):
                waited = {}
                for o in ops:
                    need = {}
                    for d in o.deps:
                        sem, val, _ = d.event
                        key = id(sem)
                        if key not in need or need[key][1] < val:
                            need[key] = (sem, val)
                    for key, (sem, val) in need.items():
                        if waited.get(key, 0) >= val:
                            continue
                        h.wait_ge(sem, val)
                        waited[key] = val
                    if o.fn is None:
                        continue
                    ins = o.fn(h)
                    if o.signal:
                        sem, val, inc = o.event
                        ins.then_inc(sem, inc)
            return section

        block.tensor(make_section("pe"))
        block.scalar(make_section("act"))
        block.vector(make_section("dve"))
        block.gpsimd(make_section("pool"))
        block.sync(make_section("sp"))


KV_GROUPS = (4, 5, 9, 12, 10, 13, 11, 14)
KVS_COLS = 4096


def default_cfg():
    chunks = []
    for seg0, own0, own1, ext in ((0, HALO, HALO + P_OWN, P_EXT), (P_EXT, HALO, HALO + S_OWN, S_EXT)):
        for c in range(0, ext, T):
            if seg0 > 0 and c < HALO:
                continue
            near = (c + T > own0 - 256) and (c < own1 + 256)
            chunks.append((seg0 + c, list(range(8)) if near else [6, 7], own0 <= c < own1))
    tiles = [HALO, HALO + T] + [P_EXT + HALO + i * T for i in range(4)]
    return dict(chunks=chunks, tiles=tiles, phases=(1, 2, 3, 4, 5), experts=list(range(N_EXP)), debug=False)


def own_row(o):
    return o - HALO if o < P_EXT else P_OWN + (o - P_EXT - HALO)


def build(cfg):
    from contextlib import ExitStack
    nc = bass.Bass("TRN2", target_bir_lowering=False)
    S = Sched()
    es = ExitStack()
    dbg = cfg["debug"]

    def din(name, shape, dt=F32):
        return nc.dram_tensor(name, list(shape), dt, kind="ExternalInput").ap()

    def dscratch(name, shape, dt):
        kind = "ExternalOutput" if dbg else "Internal"
        return nc.dram_tensor(name, list(shape), dt, kind=kind).ap()

    cur = [es]

    def sb(name, shape, dt):
        return cur[0].enter_context(nc.sbuf_tensor(name, list(shape), dt))

    class Rot:
        def __init__(self, items):
            self.items = list(items)
            self.i = 0

        def next(self):
            it = self.items[self.i % len(self.items)]
            self.i += 1
            return it

    def run_phase(fn, nbf=6):
        with ExitStack() as pes:
            cur[0] = pes
            ring["n"] = nbf
            ring["bf"] = [sb(f"wbf_{fn.__name__}_{i}", [128, 4, 512], BF16) for i in range(nbf)]
            ring["b"] = [Buf(f"wbf{i}") for i in range(nbf)]
            wcount[0] = 0
            fn()
            assert not wq and not wissued
            S.barrier()
        cur[0] = es

    def ps(name, shape, dt):
        return es.enter_context(nc.psum_tensor(name, list(shape), dt))

    xin = din("xin", [EXT, D])
    w_in = din("w_in", [D, IN_COLS])
    w_gate = din("w_gate", [D, 2 * D])
    w_pa = din("w_pa", [D, D])
    w_pb = din("w_pb", [512, D])
    w_out = din("w_out", [D, D])
    w1 = din("w1", [N_EXP * D, D_FF])
    w3 = din("w3", [N_EXP * D, D_FF])
    w2 = din("w2", [N_EXP * D_FF, D])
    g1 = din("g1", [1, D])
    g2 = din("g2", [1, D])
    gf = din("gf", [1, D])
    bgT = din("bgT", [128, 32])
    wr = din("wr", [128, KC * 72])
    br = din("br", [1, 72])
    sink = din("sink", [1, 16])
    eA = din("eA", [128, 4 * 3 * 512], BF16)
    eB = din("eB", [128, 2 * 4 * 2 * 128 + 4 * 2 * 32], BF16)
    vmask = din("vmask", [len(cfg["tiles"]) * 128, 51 * 128], BF16)
    yout = nc.dram_tensor("yout", [OWN, D], F32, kind="ExternalOutput").ap()

    XNT = dscratch("XNT", [KC * 128, EXT], BF16)
    KVS = dscratch("KVS", [EXT, KVS_COLS], BF16)
    OTS = dscratch("OTS", [20 * 128, OWN], BF16)
    HS = dscratch("HS", [OWN, D], F32)
    HN = dscratch("HN", [OWN + 128, D], BF16)
    SLOTI = dscratch("SLOTI", [NSLOT, 4], I32)
    YS = dscratch("YS", [2 * YT_STRIDE + NSLOT, D], BF16)
    b_XNT, b_KVS, b_OTS, b_HS, b_HN, b_SLOTI, b_YS, b_yout = (Buf(n) for n in "XNT KVS OTS HS HN SLOTI YS yout".split())

    ident = sb("ident", [128, 128], BF16)
    identf = sb("identf", [128, 128], F32)
    ones_bf = sb("ones_bf", [128, 128], BF16)
    neghalf = sb("neghalf", [128, 1], F32)
    b_const = Buf("const")

    S.op("pool", lambda h: h.memset(identf[:], 0.0), writes=[b_const])
    S.op("pool", lambda h: h.memset(neghalf[:], -0.5), pwrites=[b_const])
    S.op("pool", lambda h: h.memset(ones_bf[:], 1.0), pwrites=[b_const])
    b_identf = Buf("identf")
    S.op("pool", lambda h: h.affine_select(out=identf[:], in_=identf[:], pattern=[[1, 128]], compare_op=ALU.not_equal,
                                           fill=1.0, base=0, channel_multiplier=-1), reads=[b_const], writes=[b_identf])
    S.op("pool", lambda h: h.tensor_copy(out=ident[:], in_=identf[:]), reads=[b_identf], writes=[b_const])

    banks = [ps(f"bank{i}", [128, 512], F32) for i in range(8)]
    b_bank = [Buf(f"bank{i}") for i in range(8)]

    ring = {"bf": [], "b": [], "n": 0}
    wq = []
    wissued = []
    wcount = [0]

    def w_issue():
        src = wq.pop(0)
        i = wcount[0]
        wcount[0] += 1
        b = i % ring["n"]
        t_, b_ = ring["bf"][b], ring["b"][b]
        S.op("pool", lambda h: h.dma_start(out=t_[:], in_=src.rearrange("(kk p) c -> p kk c", p=128)),
             writes=[b_], dma=True)
        wissued.append((t_, b_))

    def w_push(srcs):
        wq.extend(srcs)

    def w_get():
        depth = ring["n"] - 2
        while wq and len(wissued) < depth + 1:
            w_issue()
        return wissued.pop(0)

    def piece(w, rg, cg):
        return w[rg * 512:(rg + 1) * 512, cg * 512:(cg + 1) * 512]

    def dma_split(eng, out_ap, in_ap, nsplit, **kw):
        n1 = out_ap.shape[1]
        step = n1 // nsplit
        first_w = kw.pop("writes", ())
        pw = kw.pop("pwrites", ())
        for i in range(nsplit):
            o_, i_ = out_ap[:, i * step:(i + 1) * step], in_ap[:, i * step:(i + 1) * step]
            if i == 0:
                S.op(eng, lambda h, o_=o_, i_=i_: h.dma_start(out=o_, in_=i_), writes=first_w, pwrites=pw, dma=True, **kw)
            else:
                S.op(eng, lambda h, o_=o_, i_=i_: h.dma_start(out=o_, in_=i_), pwrites=list(first_w) + list(pw), dma=True, **kw)

    evac_rr = [0]

    def evac_copy(out_ap, in_ap, reads, writes=(), pwrites=()):
        e = ("act", "dve")[evac_rr[0] % 2]
        evac_rr[0] += 1
        if e == "act":
            S.op("act", lambda h: h.copy(out=out_ap, in_=in_ap), reads=reads, writes=writes, pwrites=pwrites)
        else:
            S.op("dve", lambda h: h.tensor_copy(out=out_ap, in_=in_ap), reads=reads, writes=writes, pwrites=pwrites)

    def phase1():
        g1b = sb("g1b", [128, D], F32)
        b_g1b = Buf("g1b")
        S.op("sp", lambda h: h.dma_start(out=g1b[:], in_=g1[0, :].partition_broadcast(128)), writes=[b_g1b], dma=True)
        xst = [sb(f"xst{i}", [128, D], F32) for i in range(4)]
        b_xst = [Buf(f"xst{i}") for i in range(4)]
        junk = sb("junk1", [128, D], BF16)
        xnb = [sb(f"xnb{i}", [128, D], BF16) for i in range(8)]
        b_xnb = [Buf(f"xnb{i}") for i in range(8)]
        stat = [sb(f"stat{i}", [128, 4], F32) for i in range(4)]
        b_stat = [Buf(f"stat{i}") for i in range(4)]
        b_ms = [Buf(f"ms{i}") for i in range(4)]
        b_rstd = [Buf(f"rstd{i}") for i in range(4)]
        xnT = [sb(f"xnT{i}", [128, KC, T], BF16) for i in range(2)]
        b_xnT = [Buf(f"xnT{i}") for i in range(2)]
        kvo = [sb(f"kvo{i}", [128, 512], BF16) for i in range(4)]
        b_kvo = [Buf(f"kvo{i}") for i in range(4)]
        nkvo = 0
        w_push([piece(w_in, kg, KV_GROUPS[cg]) for (_c0, cgs_, _o) in cfg["chunks"] for cg in cgs_ for kg in range(4)])

        def norm_elem(ci):
            c0 = cfg["chunks"][ci][0]
            for tb in range(4):
                i = (ci * 4 + tb) % 4
                xi = (ci % 2) * 4 + tb
                r0 = c0 + tb * 128
                S.op("sp", lambda h, i=i, r0=r0: h.dma_start(out=xst[i][:], in_=xin[r0:r0 + 128, :]),
                     writes=[b_xst[i]], dma=True)
                S.op("act", lambda h, i=i: h.activation(out=junk[:], in_=xst[i][:], func=AF.Square,
                                                        accum_out=stat[i][:, 0:1]),
                     reads=[b_xst[i]], writes=[b_stat[i]])
                S.op("dve", lambda h, i=i: h.tensor_scalar(out=stat[i][:, 1:2], in0=stat[i][:, 0:1], scalar1=1.0 / D,
                                                          scalar2=EPS, op0=ALU.mult, op1=ALU.add),
                     reads=[b_stat[i]], writes=[b_ms[i]])
                S.op("pool", lambda h, i=i: h.tensor_tensor(out=stat[i][:, 2:3], in0=stat[i][:, 1:2], in1=neghalf[:], op=ALU.pow),
                     reads=[b_const, b_ms[i]], writes=[b_rstd[i]])
                S.op("dve", lambda h, i=i, xi=xi: h.scalar_tensor_tensor(out=xnb[xi][:], in0=xst[i][:], scalar=stat[i][:, 2:3],
                                                                        in1=g1b[:], op0=ALU.mult, op1=ALU.mult),
                     reads=[b_xst[i], b_rstd[i], b_g1b], writes=[b_xnb[xi]])

        if cfg.get("zero_left", True):
            zkv = sb("zkv", [128, KVS_COLS], BF16); b_zkv = Buf("zkv")
            S.op("pool", lambda h: h.memset(zkv[:], 0.0), writes=[b_zkv])
            for zb in range(HALO // 128):
                S.op("sp", lambda h, zb=zb: h.dma_start(out=KVS[P_EXT + zb * 128:P_EXT + (zb + 1) * 128, :], in_=zkv[:]),
                     reads=[b_zkv], pwrites=[b_KVS], dma=True)
        norm_elem(0)
        for ci, (c0, cgs, is_own) in enumerate(cfg["chunks"]):
            xt, b_xt = xnT[ci % 2], b_xnT[ci % 2]
            for tb in range(4):
                xi = (ci % 2) * 4 + tb
                for half in range(2):
                    bk = (2 * tb + half) % 8
                    tpv = banks[bk][:].bitcast(BF16)
                    for k8 in range(8):
                        k = half * 8 + k8
                        S.op("pe", lambda h, xi=xi, k=k, k8=k8, tpv=tpv: h.transpose(
                            out=tpv[:, k8 * 128:(k8 + 1) * 128], in_=xnb[xi][:, k * 128:(k + 1) * 128], identity=ident[:]),
                            reads=[b_xnb[xi], b_const], writes=[b_bank[bk]] if k8 == 0 else (), pwrites=() if k8 == 0 else [b_bank[bk]])
                    evac_copy(xt[:, half * 8:(half + 1) * 8, tb * 128:(tb + 1) * 128],
                              tpv.rearrange("p (k t) -> p k t", k=8), reads=[b_bank[bk]], pwrites=[b_xt])
            if ci + 1 < len(cfg["chunks"]):
                norm_elem(ci + 1)
            if is_own:
                dma_split("sp", XNT.rearrange("(k p) t -> p k t", p=128)[:, :, c0:c0 + T], xt[:], 4,
                          reads=[b_xt], pwrites=[b_XNT])
            for gi_, cg in enumerate(cgs):
                bset = (gi_ % 2) * 4
                for kg in range(4):
                    wt, b_wt = w_get()
                    for tb in range(4):
                        for kk in range(4):
                            first = (kg == 0 and kk == 0)
                            last = (kg == 3 and kk == 3)
                            S.op("pe", lambda h, tb=tb, kk=kk, kg=kg, wt=wt, xt=xt, first=first, last=last, bset=bset:
                                 h.matmul(banks[bset + tb][:], xt[:, kg * 4 + kk, tb * 128:(tb + 1) * 128], wt[:, kk, :],
                                          start=first, stop=last),
                                 reads=[b_xt, b_wt], writes=[b_bank[bset + tb]] if first else (),
                                 pwrites=() if first else [b_bank[bset + tb]])
                for tb in range(4):
                    j = nkvo % 4
                    nkvo += 1
                    evac_copy(kvo[j][:], banks[bset + tb][:], reads=[b_bank[bset + tb]], writes=[b_kvo[j]])
                    r0 = c0 + tb * 128
                    S.op("sp", lambda h, j=j, r0=r0, cg=cg: h.dma_start(out=KVS[r0:r0 + 128, cg * 512:(cg + 1) * 512], in_=kvo[j][:]),
                         reads=[b_kvo[j]], pwrites=[b_KVS], dma=True)


    def pe_mm(out_ap, lhsT, rhs, start, stop, reads, bbuf, first):
        S.op("pe", lambda h: h.matmul(out_ap, lhsT, rhs, start=start, stop=stop),
             reads=reads, writes=[bbuf] if first else (), pwrites=() if first else [bbuf])

    def phase2():
        eA_sb = sb("eA_sb", [128, 4 * 3 * 512], BF16)
        eB_sb = sb("eB_sb", [128, 2304], BF16)
        b_tab = Buf("tab")
        S.op("sp", lambda h: h.dma_start(out=eA_sb[:], in_=eA), writes=[b_tab], dma=True)
        S.op("sp", lambda h: h.dma_start(out=eB_sb[:], in_=eB), pwrites=[b_tab], dma=True)
        sink_sb = sb("sink_sb", [1, 16], F32)
        sexp = sb("sexp", [1, 16], F32)
        sinkrow = sb("sinkrow", [1, 16 * 128], BF16)
        b_sink, b_sexp, b_sinkrow = Buf("sink"), Buf("sexp"), Buf("sinkrow")
        S.op("sp", lambda h: h.dma_start(out=sink_sb[:], in_=sink), writes=[b_sink], dma=True)
        S.op("act", lambda h: h.activation(out=sexp[:], in_=sink_sb[:], func=AF.Exp), reads=[b_sink], writes=[b_sexp])
        S.op("dve", lambda h: h.tensor_copy(out=sinkrow[:].rearrange("p (h q) -> p h q", q=128),
                                            in_=sexp[:].unsqueeze(2).broadcast_to([1, 16, 128])),
             reads=[b_sexp], writes=[b_sinkrow])
        xT = sb("xT2", [128, KC, T], BF16); b_xT = Buf("xT2")
        vm2 = [sb(f"vm{i}", [128, 51, 128], BF16) for i in range(2)]; b_vm2 = [Buf(f"vm{i}") for i in range(2)]
        OT = sb("OT", [128, 20, T], BF16); b_OT = Buf("OT")
        QA = [sb(f"QA{i}", [128, 4, T], BF16) for i in range(2)]; b_QA = [Buf(f"QA{i}") for i in range(2)]
        QB = sb("QB", [128, 12, T], BF16); b_QB = Buf("QB")
        KnA = [sb(f"KnA{i}", [128, 6, 128], BF16) for i in range(2)]; b_KnA = [Buf(f"KnA{i}") for i in range(2)]
        VA = [sb(f"VA{i}", [128, 6, 128], BF16) for i in range(2)]; b_VA = [Buf(f"VA{i}") for i in range(2)]
        KTA = [sb(f"KTA{i}", [128, 768], BF16) for i in range(2)]; b_KTA = [Buf(f"KTA{i}") for i in range(2)]
        Kn1 = sb("Kn1", [128, 5, 128], BF16)
        Kn2 = sb("Kn2", [128, 4, 2, 128], BF16)
        Kn3a = sb("Kn3a", [128, 16, 128], BF16)
        Kn3b = sb("Kn3b", [32, 16, 128], BF16)
        V1 = [sb(f"V1_{i}", [128, 5, 128], BF16) for i in range(2)]
        V2 = [sb(f"V2_{i}", [128, 4, 2, 128], BF16) for i in range(2)]
        V3a = [sb(f"V3a_{i}", [128, 16, 128], BF16) for i in range(2)]
        V3b = [sb(f"V3b_{i}", [32, 16, 128], BF16) for i in range(2)]
        b_KnB = Buf("KnB")
        b_VB = [Buf(f"VB{i}") for i in range(2)]
        KT1 = [sb(f"KT1_{i}", [128, 5 * 128], BF16) for i in range(2)]
        KT2 = [sb(f"KT2_{i}", [128, 8 * 128], BF16) for i in range(2)]
        KT3 = [sb(f"KT3_{i}", [128, 16, 160], BF16) for i in range(2)]
        b_KTB = [Buf(f"KTB{i}") for i in range(2)]
        NP = 8
        Pb = [sb(f"P{i}", [128, 512], BF16) for i in range(NP)]
        b_P = [Buf(f"P{i}") for i in range(NP)]
        prot = Rot(range(NP))
        Rb = [sb(f"R{i}", [128, 512], F32) for i in range(2)]
        b_R = [Buf(f"R{i}") for i in range(2)]
        rrot = Rot(range(2))
        srot = Rot(range(4))
        orot = Rot((4, 5))
        lrot = Rot((6, 7))
        mulrot = Rot(("dve",))

        def load_rows(dst_t, src, bbuf, first, nsplit=1):
            four = len(dst_t.shape) == 4
            n1 = dst_t.shape[2] if four else dst_t.shape[1]
            step = n1 // nsplit
            for i in range(nsplit):
                if four:
                    d_, s_ = dst_t[:, :, i, :], src[:, :, i, :]
                else:
                    d_, s_ = dst_t[:, i * step:(i + 1) * step], src[:, i * step:(i + 1) * step]
                f_ = first and i == 0
                S.op("sp", lambda h, d_=d_, s_=s_: h.dma_start(out=d_, in_=s_), reads=[b_KVS],
                     writes=[bbuf] if f_ else (), pwrites=() if f_ else [bbuf], dma=True)

        def kv_src(kind, o, col):
            cs = slice(col, col + 128)
            if kind == "A":
                return KVS[o - 128:o - 128 + 768, cs].rearrange("(j p) d -> p j d", p=128)
            if kind == "1":
                return KVS[o - 64:o - 64 + 640, cs].rearrange("(j p) d -> p j d", p=128)
            if kind == "2":
                return KVS[o - 256:o - 256 + 1024, cs].rearrange("(s p ph) d -> p ph s d", s=2, p=128, ph=4)
            if kind == "3a":
                return KVS[o - 1024:o - 1024 + 2048, cs].rearrange("(p ph) d -> p ph d", ph=16)
            if kind == "3b":
                return KVS[o + 1024:o + 1024 + 512, cs].rearrange("(p ph) d -> p ph d", ph=16)

        def softmax_slot(sbk, nk, tab_ap):
            pi = prot.next()
            S.op("act", lambda h: h.activation(out=Pb[pi][0:nk, :], in_=banks[sbk][0:nk, :], func=AF.Exp, scale=SCALE),
                 reads=[b_bank[sbk]], writes=[b_P[pi]])
            me = mulrot.next()
            S.op(me, lambda h: h.tensor_tensor(out=Pb[pi][0:nk, :], in0=Pb[pi][0:nk, :], in1=tab_ap, op=ALU.mult),
                 reads=[b_tab, b_P[pi]], writes=[b_P[pi]])
            return pi

        def finish_unit(ob, lb, out_ap, split=None):
            ri = rrot.next()
            S.op("dve", lambda h: h.reciprocal(out=Rb[ri][:], in_=banks[lb][:]), reads=[b_bank[lb]], writes=[b_R[ri]])
            i0, i1 = banks[ob][:], Rb[ri][:]
            if split is not None:
                i0 = i0.rearrange("p (g q) -> p g q", g=split)
                i1 = i1.rearrange("p (g q) -> p g q", g=split)
            S.op("dve", lambda h: h.tensor_tensor(out=out_ap, in0=i0, in1=i1, op=ALU.mult),
                 reads=[b_bank[ob], b_R[ri]], pwrites=[b_OT])

        def qproj(cgs, dst, b_dst):
            for i, cg in enumerate(cgs):
                bks = [srot.next() for _ in range(4)]
                for kg in range(4):
                    wt, b_wt = w_get()
                    for cc in range(4):
                        for kk in range(4):
                            first = (kg == 0 and kk == 0)
                            pe_mm(banks[bks[cc]][:], wt[:, kk, cc * 128:(cc + 1) * 128], xT[:, kg * 4 + kk, :],
                                  first, (kg == 3 and kk == 3), [b_wt, b_xT], b_bank[bks[cc]], first)
                for cc in range(4):
                    evac_copy(dst[:, 4 * i + cc, :], banks[bks[cc]][:], reads=[b_bank[bks[cc]]], pwrites=[b_dst])

        for _ in cfg["tiles"]:
            w_push([piece(w_in, kg, kvh) for kvh in range(4) for kg in range(4)])
            w_push([piece(w_in, kg, 6 + gi) for gi in range(3) for kg in range(4)])
        class Step:
            def __init__(self):
                self.prep = None
                self.s1 = None
                self.s2 = None

        def tile_steps(ti, o):
            steps = []
            vm, b_vm = vm2[ti % 2], b_vm2[ti % 2]

            def tile_prep():
                dma_split("sp", xT[:], XNT.rearrange("(k p) t -> p k t", p=128)[:, :, o:o + T], 4, reads=[b_XNT], writes=[b_xT])
                S.op("sp", lambda h: h.dma_start(out=vm[:].rearrange("p s q -> p (s q)"), in_=vmask[ti * 128:(ti + 1) * 128, :]),
                     writes=[b_vm], dma=True)

            def a_prep(kvh):
                par = kvh % 2
                load_rows(KnA[par], kv_src("A", o, 0 * 512 + kvh * 128), b_KnA[par], True, 2)
                load_rows(VA[par], kv_src("A", o, 1 * 512 + kvh * 128), b_VA[par], True, 2)
                qproj([kvh], QA[par], b_QA[par])
                bk = srot.next()
                tpv = banks[bk][:].bitcast(BF16)
                for j in range(6):
                    S.op("pe", lambda h, j=j: h.transpose(out=tpv[:, j * 128:(j + 1) * 128], in_=KnA[par][:, j, :], identity=ident[:]),
                         reads=[b_KnA[par], b_const], writes=[b_bank[bk]] if j == 0 else (), pwrites=() if j == 0 else [b_bank[bk]])
                evac_copy(KTA[par][:], tpv[:, 0:768], reads=[b_bank[bk]], writes=[b_KTA[par]])

            def a_step(kvh, qb):
                par = kvh % 2
                st_ = Step()
                state = {}
                if qb == 0:
                    if kvh == 0:
                        st_.prep = lambda: (tile_prep(), a_prep(kvh))
                    else:
                        st_.prep = lambda: a_prep(kvh)

                def s1():
                    pis = []
                    for s_ in range(3):
                        j = qb + s_
                        sbk = srot.next()
                        pe_mm(banks[sbk][:], KTA[par][:, j * 128:(j + 1) * 128], QA[par][:, :, qb * 128:(qb + 1) * 128],
                              True, True, [b_KTA[par], b_QA[par]], b_bank[sbk], True)
                        pis.append(softmax_slot(sbk, 128, eA_sb[:, (kvh * 3 + s_) * 512:(kvh * 3 + s_ + 1) * 512]))
                    state["pis"] = pis

                def s2():
                    pis = state["pis"]
                    ob, lb = orot.next(), lrot.next()
                    for s_ in range(3):
                        j = qb + s_
                        pe_mm(banks[ob][:], VA[par][:, j, :], Pb[pis[s_]][:], s_ == 0, s_ == 2, [b_VA[par], b_P[pis[s_]]], b_bank[ob], s_ == 0)
                    for s_ in range(3):
                        j = qb + s_
                        pe_mm(banks[lb][:], vm[:, j, :], Pb[pis[s_]][:], s_ == 0, False, [b_vm, b_P[pis[s_]]], b_bank[lb], s_ == 0)
                    pe_mm(banks[lb][:], ones_bf[0:1, :], sinkrow[0:1, kvh * 512:(kvh + 1) * 512], False, True,
                          [b_const, b_sinkrow], b_bank[lb], False)
                    finish_unit(ob, lb, OT[:, kvh * 4:(kvh + 1) * 4, qb * 128:(qb + 1) * 128], split=4)
                st_.s1, st_.s2 = s1, s2
                return st_

            for kvh in range(4):
                for qb in range(4):
                    steps.append(a_step(kvh, qb))

            def b_prep(hh):
                par = hh % 2
                c = hh * 128
                load_rows(Kn1, kv_src("1", o, 2 * 512 + c), b_KnB, True)
                load_rows(Kn2, kv_src("2", o, 4 * 512 + c), b_KnB, False, 2)
                load_rows(Kn3a, kv_src("3a", o, 6 * 512 + c), b_KnB, False, 4)
                load_rows(Kn3b, kv_src("3b", o, 6 * 512 + c), b_KnB, False)
                load_rows(V1[par], kv_src("1", o, 3 * 512 + c), b_VB[par], True)
                load_rows(V2[par], kv_src("2", o, 5 * 512 + c), b_VB[par], False, 2)
                load_rows(V3a[par], kv_src("3a", o, 7 * 512 + c), b_VB[par], False, 4)
                load_rows(V3b[par], kv_src("3b", o, 7 * 512 + c), b_VB[par], False)
                groups = [
                    ([(Kn1[:, j, :], 128) for j in range(5)], 128, KT1[par][:, 0:640], None),
                    ([(Kn2[:, ph, s_, :], 128) for ph in range(4) for s_ in range(2)], 128, KT2[par][:, :], None),
                    ([(Kn3a[:, ph, :], 128) for ph in range(8)], 128, KT3[par][:, 0:8, 0:128], 8),
                    ([(Kn3a[:, ph, :], 128) for ph in range(8, 16)], 128, KT3[par][:, 8:16, 0:128], 8),
                    ([(Kn3b[:, ph, :], 32) for ph in range(16)], 32, KT3[par][:, :, 128:160], 16),
                ]
                for gidx, (jobs, w_, dst, nsplit) in enumerate(groups):
                    bk = srot.next()
                    tpv = banks[bk][:].bitcast(BF16)
                    for n_, (src, npart) in enumerate(jobs):
                        S.op("pe", lambda h, n_=n_, src=src, npart=npart, tpv=tpv, w_=w_: h.transpose(
                            out=tpv[:, n_ * w_:(n_ + 1) * w_], in_=src, identity=ident[0:npart, 0:npart]),
                            reads=[b_KnB, b_const], writes=[b_bank[bk]] if n_ == 0 else (), pwrites=() if n_ == 0 else [b_bank[bk]])
                    srcv = tpv[:, 0:len(jobs) * w_]
                    if nsplit is not None:
                        srcv = srcv.rearrange("p (n w) -> p n w", n=nsplit)
                    evac_copy(dst, srcv, reads=[b_bank[bk]], writes=[b_KTB[par]] if gidx == 0 else (),
                              pwrites=() if gidx == 0 else [b_KTB[par]])

            def b_step(hh, nslot, shared):
                par = hh % 2
                gi, s_ = nslot // 2, nslot % 2
                st_ = Step()
                state = {}
                if nslot == 0:
                    if hh == 0:
                        st_.prep = lambda: (qproj([6, 7, 8], QB, b_QB), b_prep(hh))
                    else:
                        st_.prep = lambda: b_prep(hh)
                nsub, nq = (4, 128) if gi < 2 else (16, 32)
                nk = 32 if (gi == 2 and s_ == 1) else 128
                qv = QB[:, gi * 4 + hh, :]

                def s1():
                    sbk = srot.next()
                    for sp in range(nsub):
                        if gi == 0:
                            lhsT = KT1[par][:, (sp + s_) * 128:(sp + s_ + 1) * 128]
                            rhs = qv[:, sp * 128:(sp + 1) * 128]
                        elif gi == 1:
                            lhsT = KT2[par][:, (sp * 2 + s_) * 128:(sp * 2 + s_ + 1) * 128]
                            rhs = qv.rearrange("p (u r) -> p r u", r=4)[:, sp, :]
                        else:
                            lhsT = KT3[par][:, sp, s_ * 128:s_ * 128 + nk]
                            rhs = qv.rearrange("p (u r) -> p r u", r=16)[:, sp, :]
                        pe_mm(banks[sbk][0:nk, sp * nq:(sp + 1) * nq], lhsT, rhs, sp == 0, sp == nsub - 1,
                              [b_KTB[par], b_QB], b_bank[sbk], sp == 0)
                    if gi < 2:
                        tab = eB_sb[:, ((gi * 4 + hh) * 2 + s_) * 128:((gi * 4 + hh) * 2 + s_ + 1) * 128]
                    else:
                        tab = eB_sb[0:nk, 2048 + (hh * 2 + s_) * 32:2048 + (hh * 2 + s_ + 1) * 32]
                    tab_b = tab.unsqueeze(1).broadcast_to([nk, nsub, nq])
                    pi = prot.next()
                    S.op("act", lambda h: h.activation(out=Pb[pi][0:nk, :], in_=banks[sbk][0:nk, :], func=AF.Exp, scale=SCALE),
                         reads=[b_bank[sbk]], writes=[b_P[pi]])
                    S.op("dve", lambda h: h.tensor_tensor(
                        out=Pb[pi][0:nk, :].rearrange("p (s q) -> p s q", s=nsub),
                        in0=Pb[pi][0:nk, :].rearrange("p (s q) -> p s q", s=nsub), in1=tab_b, op=ALU.mult),
                        reads=[b_tab, b_P[pi]], writes=[b_P[pi]])
                    state["pi"] = pi

                def s2():
                    pi = state["pi"]
                    if nslot == 0:
                        shared["ob"], shared["lb"] = orot.next(), lrot.next()
                    ob, lb = shared["ob"], shared["lb"]
                    for which, bkx in ((0, ob), (1, lb)):
                        for sp in range(nsub):
                            if gi == 0:
                                vv = V1[par][:, sp + s_, :]
                                sc = 6 + sp + s_
                                out_ap = banks[bkx][:, sp * 128:(sp + 1) * 128]
                            elif gi == 1:
                                vv = V2[par][:, sp, s_, :]
                                sc = 11 + sp * 2 + s_
                                out_ap = banks[bkx][:].rearrange("p (u r) -> p r u", r=4)[:, sp, :]
                            else:
                                vv = (V3a[par] if s_ == 0 else V3b[par])[0:nk, sp, :]
                                sc = 19 + sp * 2 + s_
                                out_ap = banks[bkx][:].rearrange("p (u r) -> p r u", r=16)[:, sp, :]
                            lhsT = vv if which == 0 else vm[0:nk, sc, :]
                            very_first = (nslot == 0 and sp == 0)
                            very_last = (nslot == 5 and sp == nsub - 1)
                            pe_mm(out_ap, lhsT, Pb[pi][0:nk, sp * nq:(sp + 1) * nq], very_first, very_last,
                                  [b_VB[par] if which == 0 else b_vm, b_P[pi]], b_bank[bkx], very_first)
                    if nslot == 5:
                        finish_unit(ob, lb, OT[:, 16 + hh, :])
                        if hh == 3:
                            ro = own_row(o)
                            dma_split("sp", OTS.rearrange("(n p) t -> p n t", p=128)[:, :, ro:ro + T], OT[:], 5,
                                      reads=[b_OT], pwrites=[b_OTS])
                st_.s1, st_.s2 = s1, s2
                return st_

            for hh in range(4):
                shared = {}
                for nslot in range(6):
                    steps.append(b_step(hh, nslot, shared))
            return steps

        steps = []
        for ti, o in enumerate(cfg["tiles"]):
            steps.extend(tile_steps(ti, o))
        for i, st_ in enumerate(steps):
            if i == 0:
                if st_.prep:
                    st_.prep()
                st_.s1()
            if i + 1 < len(steps):
                nx = steps[i + 1]
                if nx.prep:
                    nx.prep()
                nx.s1()
            st_.s2()


    NBLK = OWN // 128
    SLOTS = sb("SLOTS", [128, NBLK * 2], I32); b_SLOTS = Buf("SLOTS")
    Asum = sb("Asum", [128, 64], BF16); b_Asum = Buf("Asum")
    Umat = sb("Umat", [128, 128], BF16)
    ECi = sb("ECi", [128, 64], I32)
    ECf = sb("ECf", [128, 64], F32)
    tokid = sb("tokid", [128, NBLK], I32)
    tokid2 = sb("tokid2", [128, NBLK], I32)
    b_c3 = Buf("c3")

    def phase3():
        S.op("pool", lambda h: h.memset(Asum[:], 0.0), writes=[b_Asum])
        S.op("pool", lambda h: h.memset(Umat[:], 1.0), writes=[b_c3])
        b_U = Buf("U")
        S.op("pool", lambda h: h.affine_select(out=Umat[:], in_=Umat[:], pattern=[[1, 128]], compare_op=ALU.is_gt,
                                               fill=0.0, base=0, channel_multiplier=-1), reads=[b_c3], writes=[b_U])
        S.op("pool", lambda h: h.iota(ECi[:], pattern=[[CAP, 64]], base=0, channel_multiplier=0), pwrites=[b_c3])
        S.op("pool", lambda h: h.iota(tokid[:], pattern=[[128, NBLK]], base=0, channel_multiplier=1), pwrites=[b_c3])
        S.op("pool", lambda h: h.iota(tokid2[:], pattern=[[128, NBLK]], base=YT_STRIDE, channel_multiplier=1), pwrites=[b_c3])
        b_EC = Buf("EC")
        S.op("pool", lambda h: h.tensor_copy(out=ECf[:], in_=ECi[:]), reads=[b_c3], writes=[b_EC])
        zt = sb("zt", [128, D], BF16); b_zt = Buf("zt")
        S.op("pool", lambda h: h.memset(zt[:], 0.0), writes=[b_zt])
        S.op("sp", lambda h: h.dma_start(out=HN[OWN:OWN + 128, :], in_=zt[:]), reads=[b_zt], pwrites=[b_HN], dma=True)
        initI = sb("initI", [128, 128, 4], I32); b_initI = Buf("initI")
        S.op("pool", lambda h: h.memset(initI[:], 0), writes=[b_initI])
        b_initI2 = Buf("initI2")
        S.op("pool", lambda h: h.memset(initI[:, :, 0:1], OWN), reads=[b_initI], writes=[b_initI2])
        S.op("pool", lambda h: h.iota(initI[:, :, 2:3], pattern=[[1, 128], [1, 1]], base=2 * YT_STRIDE, channel_multiplier=128),
             reads=[b_initI], pwrites=[b_initI2])
        S.op("sp", lambda h: h.dma_start(out=SLOTI.rearrange("(p j) c -> p j c", p=128), in_=initI[:]),
             reads=[b_initI, b_initI2], writes=[b_SLOTI], dma=True)
        g2b = sb("g2b", [128, D], F32); b_g2b = Buf("g2b")
        S.op("sp", lambda h: h.dma_start(out=g2b[:], in_=g2[0, :].partition_broadcast(128)), writes=[b_g2b], dma=True)
        brb = sb("brb", [128, 72], F32)
        wr_sb = sb("wr_sb", [128, KC, 72], F32)
        bg_sb = sb("bg_sb", [128, 32], F32)
        b_rc = Buf("rconst")
        S.op("sp", lambda h: h.dma_start(out=brb[:], in_=br[0, :].partition_broadcast(128)), writes=[b_rc], dma=True)
        S.op("sp", lambda h: h.dma_start(out=wr_sb[:].rearrange("p k c -> p (k c)"), in_=wr), pwrites=[b_rc], dma=True)
        S.op("sp", lambda h: h.dma_start(out=bg_sb[:], in_=bgT), pwrites=[b_rc], dma=True)

        xT = sb("xT3", [128, KC, T], BF16); b_xT = Buf("xT3")
        OTt = sb("OTt", [128, 20, T], BF16); b_OTt = Buf("OTt")
        GA = sb("GA", [128, 4, T], F32); b_GA = Buf("GA")
        GB = sb("GB", [128, 4, T], F32); b_GB = Buf("GB")
        MT = sb("MT", [128, KC, T], BF16); b_MT = Buf("MT")
        Hh = sb("Hh", [128, 4, D], F32); b_Hh = [Buf(f"Hh{i}") for i in range(4)]
        xres = [sb(f"xres{i}", [128, 512], F32) for i in range(4)]
        b_xres = [Buf(f"xres{i}") for i in range(4)]
        xrot = Rot(range(4))
        junk = sb("junk3", [128, D], BF16)
        hnf = sb("hnf", [128, D], F32); b_hnf = Buf("hnf")
        hnb = sb("hnb", [128, D], BF16); b_hnb = Buf("hnb")
        hlo = sb("hlo", [128, D], BF16); b_hlo = Buf("hlo")
        hiT = sb("hiT", [128, KC, 128], BF16); b_hiT = Buf("hiT")
        loT = sb("loT", [128, KC, 128], BF16); b_loT = Buf("loT")
        wr_hi = sb("wr_hi", [128, KC, 72], BF16)
        wr_lo = sb("wr_lo", [128, KC, 72], BF16)
        b_wrs = Buf("wrs")
        S.op("act", lambda h: h.copy(out=wr_hi[:], in_=wr_sb[:]), reads=[b_rc], writes=[b_wrs])
        S.op("dve", lambda h: h.tensor_tensor(out=wr_lo[:], in0=wr_sb[:], in1=wr_hi[:], op=ALU.subtract), reads=[b_rc, b_wrs], pwrites=[b_wrs])
        st = sb("st3", [128, 8], F32)
        b_ssq, b_ms, b_rstd = Buf("ssq3"), Buf("ms3"), Buf("rstd3")
        LG = sb("LG", [128, 72], F32); b_LG = Buf("LG")
        rt = sb("rt", [128, 16], F32)
        ohg = sb("ohg", [128, 8], F32)
        eg = sb("eg", [128, 8], F32)
        tmp64 = sb("tmp64", [128, 64], F32)
        lsel = sb("lsel", [128, 8], F32)
        msk = sb("msk", [128, 8], F32)
        oh1 = sb("oh1", [128, 8], F32)
        oh2 = sb("oh2", [128, 8], F32)
        A1 = sb("A1", [128, 64], F32)
        A2 = sb("A2", [128, 64], F32)
        Abf = sb("Abf", [128, 64], BF16)
        rk = sb("rk", [128, 64], F32)
        junk64 = sb("junk64", [128, 64], F32)
        sl_f = sb("sl_f", [128, 2], F32)
        sl_i = sb("sl_i", [128, 2], I32)
        info = [sb(f"info{i}", [128, 4], I32) for i in range(2)]
        bR = {n: Buf("r_" + n) for n in "mg ohg negmg se pg lsel m1 oh1 negm1 msk m2 oh2 e2 den rden w1 w2 A1 A2 Abf rk slf sli info0 info1 tmp64 A2j".split()}

        def dve(fn, reads, writes, pw=()):
            S.op("dve", fn, reads=[bR[r] if isinstance(r, str) else r for r in reads],
                 writes=[bR[w] if isinstance(w, str) else w for w in writes],
                 pwrites=[bR[w] if isinstance(w, str) else w for w in pw])

        def proj_ws(w, cg, nkg, rhs_fn, rhs_reads, bset):
            for kg in range(nkg):
                wt, b_wt = w_get()
                for cc in range(4):
                    for kk in range(4):
                        first = (kg == 0 and kk == 0)
                        pe_mm(banks[bset + cc][:], wt[:, kk, cc * 128:(cc + 1) * 128], rhs_fn(kg * 4 + kk),
                              first, (kg == nkg - 1 and kk == 3), [b_wt] + rhs_reads, b_bank[bset + cc], first)

        for _ in cfg["tiles"]:
            for fg in range(4):
                w_push([piece(w_gate, kg, fg) for kg in range(4)] + [piece(w_gate, kg, 4 + fg) for kg in range(4)]
                       + [piece(w_pa, kg, fg) for kg in range(4)] + [piece(w_pb, 0, fg)])
            for cg in range(4):
                w_push([piece(w_out, kg, cg) for kg in range(4)])
        def tail1(tb, ro, blk):
            r0 = ro + tb * 128
            S.op("sp", lambda h, tb=tb, r0=r0: h.dma_start(out=HS[r0:r0 + 128, :], in_=Hh[:, tb, :]),
                 reads=[b_Hh[tb]], pwrites=[b_HS], dma=True)
            S.op("act", lambda h, tb=tb: h.activation(out=junk[:], in_=Hh[:, tb, :], func=AF.Square, accum_out=st[:, 0:1]),
                 reads=[b_Hh[tb]], writes=[b_ssq])
            S.op("dve", lambda h: h.tensor_scalar(out=st[:, 1:2], in0=st[:, 0:1], scalar1=1.0 / D, scalar2=EPS,
                                                  op0=ALU.mult, op1=ALU.add), reads=[b_ssq], writes=[b_ms])
            S.op("pool", lambda h: h.tensor_tensor(out=st[:, 2:3], in0=st[:, 1:2], in1=neghalf[:], op=ALU.pow),
                 reads=[b_ms, b_const], writes=[b_rstd])
            S.op("dve", lambda h, tb=tb: h.scalar_tensor_tensor(out=hnf[:], in0=Hh[:, tb, :], scalar=st[:, 2:3], in1=g2b[:],
                                                               op0=ALU.mult, op1=ALU.mult),
                 reads=[b_Hh[tb], b_rstd, b_g2b], writes=[b_hnf])
            S.op("act", lambda h: h.copy(out=hnb[:], in_=hnf[:]), reads=[b_hnf], writes=[b_hnb])
            S.op("dve", lambda h: h.tensor_tensor(out=hlo[:], in0=hnf[:], in1=hnb[:], op=ALU.subtract),
                 reads=[b_hnf, b_hnb], writes=[b_hlo])
            S.op("sp", lambda h, r0=r0: h.dma_start(out=HN[r0:r0 + 128, :], in_=hnb[:]), reads=[b_hnb], pwrites=[b_HN], dma=True)

        def tail2(tb, ro, blk):
            for which, (src, b_src, dstT, b_dstT) in enumerate(((hnb, b_hnb, hiT, b_hiT), (hlo, b_hlo, loT, b_loT))):
                for half in range(2):
                    bk = which * 2 + half
                    tpv = banks[bk][:].bitcast(BF16)
                    for k8 in range(8):
                        k = half * 8 + k8
                        S.op("pe", lambda h, k=k, k8=k8, tpv=tpv, src=src: h.transpose(
                            out=tpv[:, k8 * 128:(k8 + 1) * 128], in_=src[:, k * 128:(k + 1) * 128], identity=ident[:]),
                            reads=[b_src, b_const], writes=[b_bank[bk]] if k8 == 0 else (), pwrites=() if k8 == 0 else [b_bank[bk]])
                    evac_copy(dstT[:, half * 8:(half + 1) * 8, :], tpv.rearrange("p (k t) -> p k t", k=8),
                              reads=[b_bank[bk]], writes=[b_dstT] if half == 0 else (), pwrites=() if half == 0 else [b_dstT])
            lb_ = 4
            terms = [(hiT, b_hiT, wr_hi), (loT, b_loT, wr_hi), (hiT, b_hiT, wr_lo)]
            nmm = 0
            for (aT, b_aT, wmat) in terms:
                for k in range(KC):
                    pe_mm(banks[lb_][:, 0:72], aT[:, k, :], wmat[:, k, :], nmm == 0, nmm == 3 * KC - 1, [b_aT, b_wrs], b_bank[lb_], nmm == 0)
                    nmm += 1
            dve(lambda h: h.tensor_tensor(out=LG[:], in0=banks[lb_][:, 0:72], in1=brb[:], op=ALU.add), [b_bank[lb_], b_rc], [b_LG])
            dve(lambda h: h.tensor_reduce(out=rt[:, 0:1], in_=LG[:, 0:8], axis=AX.X, op=ALU.max), [b_LG], ["mg"])
            dve(lambda h: h.tensor_scalar(out=ohg[:], in0=LG[:, 0:8], scalar1=rt[:, 0:1], scalar2=None, op0=ALU.is_equal), [b_LG, "mg"], ["ohg"])
            dve(lambda h: h.tensor_scalar(out=rt[:, 1:2], in0=rt[:, 0:1], scalar1=-1.0, scalar2=None, op0=ALU.mult), ["mg"], ["negmg"])
            S.op("act", lambda h: h.activation(out=eg[:], in_=LG[:, 0:8], func=AF.Exp, bias=rt[:, 1:2], accum_out=rt[:, 2:3]),
                 reads=[b_LG, bR["negmg"]], writes=[bR["se"]])
            dve(lambda h: h.reciprocal(out=rt[:, 3:4], in_=rt[:, 2:3]), ["se"], ["pg"])
            dve(lambda h: h.tensor_tensor(out=tmp64[:].rearrange("p (g j) -> p g j", g=8),
                                          in0=LG[:, 8:72].rearrange("p (g j) -> p g j", g=8),
                                          in1=ohg[:].unsqueeze(2).broadcast_to([128, 8, 8]), op=ALU.mult), [b_LG, "ohg"], ["tmp64"])
            dve(lambda h: h.tensor_reduce(out=lsel[:], in_=tmp64[:].rearrange("p (g j) -> p j g", g=8), axis=AX.X, op=ALU.add),
                ["tmp64"], ["lsel"])
            dve(lambda h: h.tensor_reduce(out=rt[:, 4:5], in_=lsel[:], axis=AX.X, op=ALU.max), ["lsel"], ["m1"])
            dve(lambda h: h.tensor_scalar(out=oh1[:], in0=lsel[:], scalar1=rt[:, 4:5], scalar2=None, op0=ALU.is_equal), ["lsel", "m1"], ["oh1"])
            dve(lambda h: h.tensor_scalar(out=rt[:, 5:6], in0=rt[:, 4:5], scalar1=-1.0, scalar2=None, op0=ALU.mult), ["m1"], ["negm1"])
            dve(lambda h: h.scalar_tensor_tensor(out=msk[:], in0=oh1[:], scalar=-1e30, in1=lsel[:], op0=ALU.mult, op1=ALU.add),
                ["oh1", "lsel"], ["msk"])
            dve(lambda h: h.tensor_reduce(out=rt[:, 6:7], in_=msk[:], axis=AX.X, op=ALU.max), ["msk"], ["m2"])
            dve(lambda h: h.tensor_scalar(out=oh2[:], in0=msk[:], scalar1=rt[:, 6:7], scalar2=None, op0=ALU.is_equal), ["msk", "m2"], ["oh2"])
            S.op("act", lambda h: h.activation(out=rt[:, 7:8], in_=rt[:, 6:7], func=AF.Exp, bias=rt[:, 5:6]),
                 reads=[bR["m2"], bR["negm1"]], writes=[bR["e2"]])
            dve(lambda h: h.tensor_scalar(out=rt[:, 8:9], in0=rt[:, 7:8], scalar1=1.0, scalar2=None, op0=ALU.add), ["e2"], ["den"])
            dve(lambda h: h.reciprocal(out=rt[:, 9:10], in_=rt[:, 8:9]), ["den"], ["rden"])
            dve(lambda h: h.tensor_tensor(out=rt[:, 10:11], in0=rt[:, 9:10], in1=rt[:, 3:4], op=ALU.mult), ["rden", "pg"], ["w1"])
            dve(lambda h: h.tensor_tensor(out=rt[:, 11:12], in0=rt[:, 10:11], in1=rt[:, 7:8], op=ALU.mult), ["w1", "e2"], ["w2"])
            dve(lambda h: h.tensor_tensor(out=A1[:].rearrange("p (g j) -> p g j", g=8),
                                          in0=ohg[:].unsqueeze(2).broadcast_to([128, 8, 8]),
                                          in1=oh1[:].unsqueeze(1).broadcast_to([128, 8, 8]), op=ALU.mult), ["ohg", "oh1"], ["A1"])
            dve(lambda h: h.tensor_tensor(out=A2[:].rearrange("p (g j) -> p g j", g=8),
                                          in0=ohg[:].unsqueeze(2).broadcast_to([128, 8, 8]),
                                          in1=oh2[:].unsqueeze(1).broadcast_to([128, 8, 8]), op=ALU.mult), ["ohg", "oh2"], ["A2"])
            dve(lambda h: h.tensor_tensor(out=Abf[:], in0=A1[:], in1=A2[:], op=ALU.add), ["A1", "A2"], ["Abf"])

        def tail3(tb, ro, blk):
            rb_ = 5
            pe_mm(banks[rb_][:, 0:64], Umat[:], Abf[:], True, False, [b_U, bR["Abf"]], b_bank[rb_], True)
            pe_mm(banks[rb_][:, 0:64], ones_bf[:], Asum[:], False, True, [b_const, b_Asum], b_bank[rb_], False)
            dve(lambda h: h.tensor_tensor(out=rk[:], in0=banks[rb_][:, 0:64], in1=ECf[:], op=ALU.add), [b_bank[rb_], b_EC], ["rk"])
            S.op("pool", lambda h: h.tensor_tensor(out=Asum[:], in0=Asum[:], in1=Abf[:], op=ALU.add),
                 reads=[bR["Abf"], b_Asum], writes=[b_Asum])
            dve(lambda h: h.tensor_tensor(out=tmp64[:], in0=rk[:], in1=A1[:], op=ALU.mult), ["rk", "A1"], ["tmp64"])
            dve(lambda h: h.tensor_reduce(out=sl_f[:, 0:1], in_=tmp64[:], axis=AX.X, op=ALU.add), ["tmp64"], [], ["slf"])
            dve(lambda h: h.tensor_tensor(out=junk64[:], in0=rk[:], in1=A2[:], op=ALU.mult), ["rk", "A2"], ["A2j"])
            dve(lambda h: h.tensor_reduce(out=sl_f[:, 1:2], in_=junk64[:], axis=AX.X, op=ALU.add), ["A2j"], [], ["slf"])
            dve(lambda h: h.tensor_copy(out=sl_i[:], in_=sl_f[:]), ["slf"], ["sli"])
            dve(lambda h, blk=blk: h.tensor_copy(out=SLOTS[:, blk * 2:blk * 2 + 2], in_=sl_i[:]), ["sli"], [], [b_SLOTS])
            for kx in range(2):
                nm = f"info{kx}"
                dve(lambda h, kx=kx, blk=blk: h.tensor_copy(out=info[kx][:, 0:1], in_=tokid[:, blk:blk + 1]), [b_c3], [nm])
                dve(lambda h, kx=kx, blk=blk: h.tensor_copy(out=info[kx][:, 2:3], in_=(tokid if kx == 0 else tokid2)[:, blk:blk + 1]),
                    [b_c3, nm], [nm])
                dve(lambda h, kx=kx: h.tensor_copy(out=info[kx][:, 1:2].bitcast(F32), in_=rt[:, 10 + kx:11 + kx]),
                    ["w1", "w2", nm], [nm])
                S.op("pool", lambda h, kx=kx: h.indirect_dma_start(
                    out=SLOTI, out_offset=bass.IndirectOffsetOnAxis(ap=sl_i[:, kx:kx + 1], axis=0),
                    in_=info[kx][:], in_offset=None),
                    reads=[bR[nm], bR["sli"], b_SLOTI], pwrites=[b_SLOTI], dma=True)

        pending = []
        done = set()

        def run_slot(slot):
            if not pending:
                return
            for part, fn_, b in ((3, tail3, slot - 2), (2, tail2, slot - 1), (1, tail1, slot)):
                if 0 <= b < 4:
                    key = (pending[b][1], b, part)
                    if key in done:
                        continue
                    done.add(key)
                    fn_(*pending[b])

        blk = 0
        for ti, o in enumerate(cfg["tiles"]):
            ro = own_row(o)
            dma_split("sp", xT[:], XNT.rearrange("(k p) t -> p k t", p=128)[:, :, o:o + T], 4, reads=[b_XNT], writes=[b_xT])
            dma_split("sp", OTt[:], OTS.rearrange("(n p) t -> p n t", p=128)[:, :, ro:ro + T], 5, reads=[b_OTS], writes=[b_OTt])
            for fg in range(4):
                proj_ws(w_gate, fg, 4, lambda k: xT[:, k, :], [b_xT], 0)
                for cc in range(4):
                    c_ = fg * 4 + cc
                    S.op("act", lambda h, cc=cc, c_=c_: h.activation(out=GA[:, cc, :], in_=banks[cc][:], func=AF.Sigmoid,
                                                                     bias=bg_sb[:, c_:c_ + 1]),
                         reads=[b_bank[cc], b_rc], writes=[b_GA] if cc == 0 else (), pwrites=() if cc == 0 else [b_GA])
                proj_ws(w_gate, 4 + fg, 4, lambda k: xT[:, k, :], [b_xT], 4)
                for cc in range(4):
                    c_ = 16 + fg * 4 + cc
                    S.op("act", lambda h, cc=cc, c_=c_: h.activation(out=GB[:, cc, :], in_=banks[4 + cc][:], func=AF.Sigmoid,
                                                                     bias=bg_sb[:, c_:c_ + 1]),
                         reads=[b_bank[4 + cc], b_rc], writes=[b_GB] if cc == 0 else (), pwrites=() if cc == 0 else [b_GB])
                proj_ws(w_pa, fg, 4, lambda k: OTt[:, k, :], [b_OTt], 0)
                for cc in range(4):
                    S.op("dve", lambda h, cc=cc: h.tensor_tensor(out=GA[:, cc, :], in0=banks[cc][:], in1=GA[:, cc, :], op=ALU.mult),
                         reads=[b_bank[cc], b_GA], pwrites=[b_GA])
                proj_ws(w_pb, fg, 1, lambda k: OTt[:, 16 + k, :], [b_OTt], 4)
                for cc in range(4):
                    S.op("dve", lambda h, cc=cc: h.tensor_tensor(out=GB[:, cc, :], in0=banks[4 + cc][:], in1=GB[:, cc, :], op=ALU.mult),
                         reads=[b_bank[4 + cc], b_GB], pwrites=[b_GB])
                S.op("pool", lambda h, fg=fg: h.tensor_tensor(out=MT[:, fg * 4:(fg + 1) * 4, :], in0=GA[:], in1=GB[:], op=ALU.add),
                     reads=[b_GA, b_GB], writes=[b_MT] if fg == 0 else (), pwrites=() if fg == 0 else [b_MT])
                run_slot(fg)
            for cg in range(4):
                bset = (cg % 2) * 4
                for kg in range(4):
                    wt, b_wt = w_get()
                    for tb in range(4):
                        for kk in range(4):
                            first = (kg == 0 and kk == 0)
                            pe_mm(banks[bset + tb][:], MT[:, kg * 4 + kk, tb * 128:(tb + 1) * 128], wt[:, kk, :],
                                  first, (kg == 3 and kk == 3), [b_wt, b_MT], b_bank[bset + tb], first)
                for tb in range(4):
                    xi = xrot.next()
                    r0 = o + tb * 128
                    S.op("sp", lambda h, xi=xi, r0=r0, cg=cg: h.dma_start(out=xres[xi][:], in_=xin[r0:r0 + 128, cg * 512:(cg + 1) * 512]),
                         writes=[b_xres[xi]], dma=True)
                    S.op("dve", lambda h, xi=xi, tb=tb, cg=cg, bset=bset: h.tensor_tensor(
                        out=Hh[:, tb, cg * 512:(cg + 1) * 512], in0=banks[bset + tb][:], in1=xres[xi][:], op=ALU.add),
                        reads=[b_bank[bset + tb], b_xres[xi]], writes=[b_Hh[tb]] if cg == 0 else (), pwrites=() if cg == 0 else [b_Hh[tb]])
                run_slot(4 + cg)
            pending = [(tb, ro, blk + tb) for tb in range(4)]
            blk += 4
        for slot in range(8):
            run_slot(slot)

    def phase4():
        si = [sb(f"si{i}", [128, 4], I32) for i in range(4)]; b_si = [Buf(f"si{i}") for i in range(4)]
        X = [sb(f"X{i}", [128, D], BF16) for i in range(4)]; b_X = [Buf(f"X{i}") for i in range(4)]
        XT = [sb(f"XT{i}", [128, KC, 256], BF16) for i in range(2)]; b_XTe = [Buf(f"XT{i}") for i in range(2)]
        S1 = sb("S1", [128, 1024], F32); b_S1 = Buf("S1")
        HT = [sb(f"HT{i}", [128, 4, 256], BF16) for i in range(2)]; b_HT = [Buf(f"HT{i}") for i in range(2)]
        Y = [sb(f"Y{i}", [128, D], BF16) for i in range(4)]; b_Y = [Buf(f"Y{i}") for i in range(4)]
        yev = Rot(("act", "dve"))
        for e in cfg["experts"]:
            w_push([piece(w1, e * 4 + kg, 0) for kg in range(4)] + [piece(w3, e * 4 + kg, 0) for kg in range(4)]
                   + [piece(w2, e, cg) for cg in range(4)])
        for ei, e in enumerate(cfg["experts"]):
            xt, b_xt = XT[ei % 2], b_XTe[ei % 2]
            sis = []
            for sb_ in range(2):
                i4 = (ei % 2) * 2 + sb_
                sis.append(i4)
                r0 = e * CAP + sb_ * 128
                S.op("sp", lambda h, i4=i4, r0=r0: h.dma_start(out=si[i4][:], in_=SLOTI[r0:r0 + 128, :]),
                     reads=[b_SLOTI], writes=[b_si[i4]], dma=True)
                S.op("pool", lambda h, i4=i4: h.indirect_dma_start(
                    out=X[i4][:], out_offset=None, in_=HN, in_offset=bass.IndirectOffsetOnAxis(ap=si[i4][:, 0:1], axis=0)),
                    reads=[b_si[i4], b_HN], writes=[b_X[i4]], dma=True)
                for half in range(2):
                    bk = 4 + (2 * sb_ + half) % 4
                    tpv = banks[bk][:].bitcast(BF16)
                    for k8 in range(8):
                        k = half * 8 + k8
                        S.op("pe", lambda h, i4=i4, k=k, k8=k8, tpv=tpv: h.transpose(
                            out=tpv[:, k8 * 128:(k8 + 1) * 128], in_=X[i4][:, k * 128:(k + 1) * 128], identity=ident[:]),
                            reads=[b_X[i4], b_const], writes=[b_bank[bk]] if k8 == 0 else (), pwrites=() if k8 == 0 else [b_bank[bk]])
                    first_w = (sb_ == 0 and half == 0)
                    evac_copy(xt[:, half * 8:(half + 1) * 8, sb_ * 128:(sb_ + 1) * 128], tpv.rearrange("p (k t) -> p k t", k=8),
                              reads=[b_bank[bk]], writes=[b_xt] if first_w else (), pwrites=() if first_w else [b_xt])
            for which in range(2):
                for kg in range(4):
                    wt, b_wt = w_get()
                    for cc in range(4):
                        bk = which * 2 + cc // 2
                        for kk in range(4):
                            vfirst = (kg == 0 and kk == 0 and cc % 2 == 0)
                            vlast = (kg == 3 and kk == 3 and cc % 2 == 1)
                            pe_mm(banks[bk][:, (cc % 2) * 256:(cc % 2 + 1) * 256], wt[:, kk, cc * 128:(cc + 1) * 128],
                                  xt[:, kg * 4 + kk, :], vfirst, vlast, [b_wt, b_xt], b_bank[bk], vfirst)
            ht, b_ht = HT[ei % 2], b_HT[ei % 2]
            for hb_ in range(2):
                S.op("act", lambda h, hb_=hb_: h.activation(out=S1[:, hb_ * 512:(hb_ + 1) * 512], in_=banks[hb_][:], func=AF.Silu),
                     reads=[b_bank[hb_]], writes=[b_S1] if hb_ == 0 else (), pwrites=() if hb_ == 0 else [b_S1])
            for hb_ in range(2):
                S.op("dve", lambda h, hb_=hb_, ht=ht: h.tensor_tensor(
                    out=ht[:, hb_ * 2:(hb_ + 1) * 2, :], in0=banks[2 + hb_][:].rearrange("p (c t) -> p c t", c=2),
                    in1=S1[:, hb_ * 512:(hb_ + 1) * 512].rearrange("p (c t) -> p c t", c=2), op=ALU.mult),
                    reads=[b_bank[2 + hb_], b_S1], writes=[b_ht] if hb_ == 0 else (), pwrites=() if hb_ == 0 else [b_ht])
            for cg in range(4):
                wt, b_wt = w_get()
                for sb_ in range(2):
                    i4 = sis[sb_]
                    bk = 4 + (2 * cg + sb_) % 4
                    for kk in range(4):
                        pe_mm(banks[bk][:], ht[:, kk, sb_ * 128:(sb_ + 1) * 128], wt[:, kk, :], kk == 0, kk == 3,
                              [b_wt, b_ht], b_bank[bk], kk == 0)
                    wcol = si[i4][:, 1:2].bitcast(F32)
                    ye = yev.next()
                    wr_ = dict(writes=[b_Y[i4]] if cg == 0 else (), pwrites=() if cg == 0 else [b_Y[i4]])
                    if ye == "act":
                        S.op("act", lambda h, i4=i4, cg=cg, bk=bk, wcol=wcol: h.activation(
                            out=Y[i4][:, cg * 512:(cg + 1) * 512], in_=banks[bk][:], func=AF.Copy, scale=wcol),
                            reads=[b_bank[bk], b_si[i4]], **wr_)
                    else:
                        S.op("dve", lambda h, i4=i4, cg=cg, bk=bk, wcol=wcol: h.tensor_scalar(
                            out=Y[i4][:, cg * 512:(cg + 1) * 512], in0=banks[bk][:], scalar1=wcol, scalar2=None, op0=ALU.mult),
                            reads=[b_bank[bk], b_si[i4]], **wr_)
            for sb_ in range(2):
                i4 = sis[sb_]
                r0 = e * CAP + sb_ * 128
                S.op("pool", lambda h, i4=i4: h.indirect_dma_start(
                    out=YS, out_offset=bass.IndirectOffsetOnAxis(ap=si[i4][:, 2:3], axis=0), in_=Y[i4][:], in_offset=None),
                    reads=[b_Y[i4], b_si[i4]], pwrites=[b_YS], dma=True)

    def phase5():
        gfb = sb("gfb", [128, D], F32); b_gfb = Buf("gfb")
        S.op("sp", lambda h: h.dma_start(out=gfb[:], in_=gf[0, :].partition_broadcast(128)), writes=[b_gfb], dma=True)
        NB5 = 4
        hb = [sb(f"hb{i}", [128, D], F32) for i in range(NB5)]; b_hb = [Buf(f"hb{i}") for i in range(NB5)]
        yg = [sb(f"yg{i}", [128, D], BF16) for i in range(2 * NB5)]; b_yg = [Buf(f"yg{i}") for i in range(2 * NB5)]
        ob = [sb(f"ob{i}", [128, D], F32) for i in range(NB5)]; b_ob = [Buf(f"ob{i}") for i in range(NB5)]
        junk = sb("junk5", [128, D], BF16)
        st = [sb(f"st5{i}", [128, 4], F32) for i in range(NB5)]
        b_ssq = [Buf(f"ssq5{i}") for i in range(NB5)]; b_ms = [Buf(f"ms5{i}") for i in range(NB5)]; b_rstd = [Buf(f"rstd5{i}") for i in range(NB5)]
        nb = sum(1 for _ in cfg["tiles"]) * 4
        blocks = []
        for o in cfg["tiles"]:
            for tb in range(4):
                blocks.append(own_row(o) + tb * 128)
        def stage_a(bi):
            r0 = blocks[bi]
            i = bi % NB5
            S.op("sp", lambda h: h.dma_start(out=hb[i][:], in_=HS[r0:r0 + 128, :]), reads=[b_HS], writes=[b_hb[i]], dma=True)
            for kx in range(2):
                j = i * 2 + kx
                S.op("sp", lambda h, j=j, kx=kx: h.dma_start(out=yg[j][:], in_=YS[kx * YT_STRIDE + r0:kx * YT_STRIDE + r0 + 128, :]),
                     reads=[b_YS], writes=[b_yg[j]], dma=True)
            S.op("dve", lambda h: h.tensor_tensor(out=hb[i][:], in0=hb[i][:], in1=yg[i * 2][:], op=ALU.add),
                 reads=[b_hb[i], b_yg[i * 2]], writes=[b_hb[i]])
            S.op("dve", lambda h: h.tensor_tensor(out=hb[i][:], in0=hb[i][:], in1=yg[i * 2 + 1][:], op=ALU.add),
                 reads=[b_hb[i], b_yg[i * 2 + 1]], writes=[b_hb[i]])
            S.op("act", lambda h: h.activation(out=junk[:], in_=hb[i][:], func=AF.Square, accum_out=st[i][:, 0:1]),
                 reads=[b_hb[i]], writes=[b_ssq[i]])

        def stage_b(bi):
            r0 = blocks[bi]
            i = bi % NB5
            S.op("dve", lambda h: h.tensor_scalar(out=st[i][:, 1:2], in0=st[i][:, 0:1], scalar1=1.0 / D, scalar2=EPS,
                                                  op0=ALU.mult, op1=ALU.add), reads=[b_ssq[i]], writes=[b_ms[i]])
            S.op("pool", lambda h: h.tensor_tensor(out=st[i][:, 2:3], in0=st[i][:, 1:2], in1=neghalf[:], op=ALU.pow),
                 reads=[b_ms[i], b_const], writes=[b_rstd[i]])
            S.op("dve", lambda h: h.scalar_tensor_tensor(out=ob[i][:], in0=hb[i][:], scalar=st[i][:, 2:3], in1=gfb[:],
                                                         op0=ALU.mult, op1=ALU.mult),
                 reads=[b_hb[i], b_rstd[i], b_gfb], writes=[b_ob[i]])
            S.op("pool", lambda h: h.dma_start(out=yout[r0:r0 + 128, :], in_=ob[i][:]), reads=[b_ob[i]], pwrites=[b_yout], dma=True)

        LAG = 3
        for bi in range(min(LAG, len(blocks))):
            stage_a(bi)
        for bi in range(len(blocks)):
            if bi + LAG < len(blocks):
                stage_a(bi + LAG)
            stage_b(bi)

    if 1 in cfg["phases"]:
        run_phase(phase1, 8)
    if 2 in cfg["phases"]:
        run_phase(phase2)
    if 3 in cfg["phases"]:
        run_phase(phase3)
    if 4 in cfg["phases"]:
        run_phase(phase4, 16)
    if 5 in cfg["phases"]:
        run_phase(phase5)
    S.barrier()
    emit_program(nc, S, es)
    es.close()
    return nc


def _bf16(a):
    return np.ascontiguousarray(a.astype(ml_dtypes.bfloat16))


def position_tables():
    k = np.arange(128)[:, None].astype(np.float64)
    sl_a = np.power(2.0, -8.0 * (np.arange(16) + 1) / 16)
    sl_b = np.power(2.0, -8.0 * (np.arange(12) + 1) / 12)
    eA = np.zeros((128, 4, 3, 4, 128))
    q = np.arange(128)[None, :].astype(np.float64)
    for kvh in range(4):
        for s in range(3):
            rel = (128 * s - 128 + k) - q
            for g in range(4):
                e = np.exp(-sl_a[kvh * 4 + g] * np.abs(rel))
                eA[:, kvh, s, g, :] = np.where(np.abs(rel) <= 128, e, 0.0)
    eB01 = np.zeros((128, 2, 4, 2, 128))
    for gi, r in ((0, 1), (1, 4)):
        for hh in range(4):
            for s in range(2):
                rel = (k + 128 * s - 64) - q
                e = np.exp(-sl_b[gi * 4 + hh] * r * np.abs(rel))
                eB01[:, gi, hh, s, :] = np.where(np.abs(rel) <= 64, e, 0.0)
    eB2 = np.zeros((128, 4, 2, 32))
    q32 = np.arange(32)[None, :].astype(np.float64)
    for hh in range(4):
        for s in range(2):
            rel = (k + 128 * s - 64) - q32
            e = np.exp(-sl_b[8 + hh] * 16 * np.abs(rel))
            e = np.where(np.abs(rel) <= 64, e, 0.0)
            if s == 1:
                e = np.where(k < 32, e, 0.0)
            eB2[:, hh, s, :] = e
    eB = np.concatenate([eB01.reshape(128, -1), eB2.reshape(128, -1)], axis=1)
    return _bf16(eA.reshape(128, -1)), _bf16(eB)


def valid_mask_table(valid_ext, tiles):
    p = np.arange(128)
    out = np.zeros((len(tiles), 128, 51), np.float32)
    for ti, o in enumerate(tiles):
        cols = []
        for j in range(6):
            cols.append(valid_ext[o - 128 + 128 * j + p])
        for j in range(5):
            cols.append(valid_ext[o - 64 + 128 * j + p])
        for ph in range(4):
            for s in range(2):
                cols.append(valid_ext[o - 256 + ph + 512 * s + 4 * p])
        for ph in range(16):
            for s in range(2):
                tok = o - 1024 + ph + 2048 * s + 16 * p
                if s == 0:
                    cols.append(valid_ext[tok])
                else:
                    cols.append(np.where(p < 32, valid_ext[np.minimum(tok, len(valid_ext) - 1)], 0.0))
        out[ti] = np.stack(cols, axis=1)
    full = np.repeat(out[:, :, :, None], 128, axis=3)
    return _bf16(full.reshape(len(tiles) * 128, 51 * 128))


def make_shared_inputs(inp):
    f = lambda a: np.ascontiguousarray(np.asarray(a, dtype=np.float32))
    eA, eB = position_tables()
    w_rg = f(inp["w_router_group"])[0]
    w_re = f(inp["w_router_expert"])[0]
    wrm = np.concatenate([w_rg, w_re.transpose(1, 0, 2).reshape(D, 64)], axis=1)
    wrm = wrm.reshape(KC, 128, 72).transpose(1, 0, 2).reshape(128, KC * 72)
    brm = np.concatenate([f(inp["b_router_group"])[0], f(inp["b_router_expert"])[0].reshape(64)])[None, :]
    return dict(
        w_in=f(inp["w_in"])[0], w_gate=f(inp["w_gate"])[0], w_pa=f(inp["w_proj_a"])[0], w_pb=f(inp["w_proj_b"])[0],
        w_out=f(inp["w_out"])[0],
        w1=f(inp["w_expert_gate"])[0].reshape(N_EXP * D, D_FF), w3=f(inp["w_expert_up"])[0].reshape(N_EXP * D, D_FF),
        w2=f(inp["w_expert_down"])[0].reshape(N_EXP * D_FF, D),
        g1=f(inp["norm1"]).reshape(1, D), g2=f(inp["norm2"]).reshape(1, D), gf=f(inp["norm_final"]).reshape(1, D),
        bgT=np.ascontiguousarray(f(inp["b_gate"])[0].reshape(32, 128).T), wr=np.ascontiguousarray(wrm), br=f(brm),
        sink=f(inp["attn_sink"]).reshape(1, 16), eA=eA, eB=eB,
    )


def make_core_inputs(inp, c, tiles):
    xp = np.asarray(inp["x_prompt"], dtype=np.float32)[0]
    xs = np.asarray(inp["x_sample"], dtype=np.float32)[c // 2]
    xin = np.zeros((EXT, D), np.float32)
    valid = np.zeros((EXT,), np.float32)
    p0 = 1024 * c - HALO
    lo, hi = max(p0, 0), min(p0 + P_EXT, xp.shape[0])
    xin[lo - p0:hi - p0] = xp[lo:hi]
    valid[lo - p0:hi - p0] = 1.0
    seq = xs if c % 2 == 0 else xs[::-1]
    n = S_OWN + HALO
    xin[P_EXT + HALO:P_EXT + HALO + n] = seq[0:n]
    valid[P_EXT + HALO:P_EXT + HALO + n] = 1.0
    return dict(xin=xin, vmask=valid_mask_table(valid, tiles))


_NC_CACHE = {}


def kernel(**inputs):
    cfg = default_cfg()
    if "nc" not in _NC_CACHE:
        _NC_CACHE["nc"] = build(cfg)
    nc = _NC_CACHE["nc"]
    shared = make_shared_inputs(inputs)
    in_maps = []
    for c in range(NCORES):
        m = dict(shared)
        m.update(make_core_inputs(inputs, c, cfg["tiles"]))
        in_maps.append(m)
    res = run_bass_kernel_spmd(nc, in_maps, core_ids=list(range(NCORES)))
    yp = np.zeros((1, 8192, D), np.float32)
    ys = np.zeros((4, 4096, D), np.float32)
    for c in range(NCORES):
        y = np.asarray(res.results[c]["yout"])
        yp[0, 1024 * c:1024 * c + 1024] = y[:P_OWN]
        ys[c // 2, 2048 * (c % 2):2048 * (c % 2) + 2048] = y[P_OWN:] if c % 2 == 0 else y[P_OWN:][::-1]
    return (yp, ys)
```

```python
import math
import numpy as np
import ml_dtypes
import concourse.bass as bass
import concourse.mybir as mybir
from concourse.bass_utils import run_bass_kernel_spmd

F32 = mybir.dt.float32
BF16 = mybir.dt.bfloat16
I32 = mybir.dt.int32
AF = mybir.ActivationFunctionType
ALU = mybir.AluOpType
AX = mybir.AxisListType

D = 2048
KC = D // 128
HEAD = 128
A_HEADS, A_KV, A_GROUP = 16, 4, 4
B_HPG = 4
DIL = ((128, 1), (512, 4), (2048, 16))
IN_COLS = 7680
N_EXP = 64
D_FF = 512
EPS = 1e-6
SCALE = HEAD ** -0.5
NCORES = 8
T = 512
HALO = 1024
P_OWN, S_OWN = 1024, 2048
P_EXT, S_EXT = P_OWN + 2 * HALO, S_OWN + 2 * HALO
EXT = P_EXT + S_EXT
OWN = P_OWN + S_OWN
CAP = 256
NSLOT = N_EXP * CAP
YT_STRIDE = OWN + 128
BIG_IDX = 1 << 24
SEM_ROLL = 20000


class Buf:
    __slots__ = ("name", "writers", "readers")

    def __init__(self, name):
        self.name = name
        self.writers = []
        self.readers = []


class Op:
    __slots__ = ("eng", "fn", "deps", "signal", "event", "is_dma", "idx")

    def __init__(self, eng, fn, is_dma):
        self.eng = eng
        self.fn = fn
        self.deps = []
        self.signal = False
        self.event = None
        self.is_dma = is_dma
        self.idx = -1


ENGS = ("pe", "act", "dve", "pool", "sp")


class Sched:
    def __init__(self):
        self.streams = {e: [] for e in ENGS}
        self.nops = 0

    def op(self, eng, fn, reads=(), writes=(), pwrites=(), dma=False):
        o = Op(eng, fn, dma)
        o.idx = self.nops
        self.nops += 1
        deps = []
        for b in reads:
            deps.extend(b.writers)
        for b in writes:
            deps.extend(b.writers)
            deps.extend(b.readers)
        for b in pwrites:
            deps.extend(b.readers)
        for b in reads:
            b.readers.append(o)
        for b in writes:
            b.writers = [o]
            b.readers = []
        for b in pwrites:
            if b.readers:
                b.writers = [o]
                b.readers = []
            else:
                b.writers.append(o)
        latest = {}
        out = []
        seen = set()
        for d in deps:
            if d is o or id(d) in seen:
                continue
            seen.add(id(d))
            if d.is_dma:
                out.append(d)
            else:
                if d.eng == eng and eng == "pe":
                    continue
                cur = latest.get(d.eng)
                if cur is None or d.idx > cur.idx:
                    latest[d.eng] = d
        out.extend(latest.values())
        o.deps = out
        self.streams[eng].append(o)
        return o

    def barrier(self):
        deps = []
        for e in ENGS:
            st = self.streams[e]
            if not st:
                continue
            last_c = None
            for o in reversed(st):
                if o.fn is None:
                    break
                if o.is_dma:
                    deps.append(o)
                elif last_c is None:
                    last_c = o
            if last_c is not None:
                deps.append(last_c)
        for e in ENGS:
            o = Op(e, None, False)
            o.idx = self.nops
            self.nops += 1
            o.deps = [d for d in deps]
            self.streams[e].append(o)


def emit_program(nc, sched, extra_ctx):
    for e in ENGS:
        for o in sched.streams[e]:
            for d in o.deps:
                d.signal = True
            if o.is_dma:
                o.signal = True
    n_dma_sems = {"sp": 16, "act": 4, "pool": 12, "dve": 2, "pe": 2}
    n_eng_sems = {}
    for e in ENGS:
        cnt = sum(1 for o in sched.streams[e] if o.signal and not o.is_dma)
        n_eng_sems[e] = max(1, (cnt + SEM_ROLL - 1) // SEM_ROLL)
    from contextlib import ExitStack
    with ExitStack() as st:
        eng_sems = {e: [st.enter_context(nc.semaphore(f"s_{e}_{i}")) for i in range(n_eng_sems[e])] for e in ENGS}
        dma_sems = {e: [st.enter_context(nc.semaphore(f"d_{e}_{i}")) for i in range(n_dma_sems[e])] for e in ENGS}
        for e in ENGS:
            cnt = 0
            dcnt = 0
            dma_last = [None] * n_dma_sems[e]
            dma_uses = [0] * n_dma_sems[e]
            for o in sched.streams[e]:
                if o.is_dma:
                    k = dcnt % n_dma_sems[e]
                    dcnt += 1
                    if dma_last[k] is not None:
                        o.deps.append(dma_last[k])
                    dma_uses[k] += 1
                    o.event = (dma_sems[e][k], 16 * dma_uses[k], 16)
                    dma_last[k] = o
                elif o.signal:
                    si = cnt // SEM_ROLL
                    o.event = (eng_sems[e][si], cnt % SEM_ROLL + 1, 1)
                    cnt += 1
        block = st.enter_context(nc.Block())

        def make_section(e):
            ops = sched.streams[e]

            def section(h):
                waited = {}
                for o in ops:
                    need = {}
                    for d in o.deps:
                        sem, val, _ = d.event
                        key = id(sem)
                        if key not in need or need[key][1] < val:
                            need[key] = (sem, val)
                    for key, (sem, val) in need.items():
                        if waited.get(key, 0) >= val:
                            continue
                        h.wait_ge(sem, val)
                        waited[key] = val
                    if o.fn is None:
                        continue
                    ins = o.fn(h)
                    if o.signal:
                        sem, val, inc = o.event
                        ins.then_inc(sem, inc)
            return section

        block.tensor(make_section("pe"))
        block.scalar(make_section("act"))
        block.vector(make_section("dve"))
        block.gpsimd(make_section("pool"))
        block.sync(make_section("sp"))


KV_GROUPS = (4, 5, 9, 12, 10, 13, 11, 14)
KVS_COLS = 4096


def default_cfg():
    chunks = []
    for seg0, own0, own1, ext in ((0, HALO, HALO + P_OWN, P_EXT), (P_EXT, HALO, HALO + S_OWN, S_EXT)):
        for c in range(0, ext, T):
            if seg0 > 0 and c < HALO:
                continue
            near = (c + T > own0 - 256) and (c < own1 + 256)
            chunks.append((seg0 + c, list(range(8)) if near else [6, 7], own0 <= c < own1))
    tiles = [HALO, HALO + T] + [P_EXT + HALO + i * T for i in range(4)]
    return dict(chunks=chunks, tiles=tiles, phases=(1, 2, 3, 4, 5), experts=list(range(N_EXP)), debug=False)


def own_row(o):
    return o - HALO if o < P_EXT else P_OWN + (o - P_EXT - HALO)


def build(cfg):
    from contextlib import ExitStack
    nc = bass.Bass("TRN2", target_bir_lowering=False)
    S = Sched()
    es = ExitStack()
    dbg = cfg["debug"]

    def din(name, shape, dt=F32):
        return nc.dram_tensor(name, list(shape), dt, kind="ExternalInput").ap()

    def dscratch(name, shape, dt):
        kind = "ExternalOutput" if dbg else "Internal"
        return nc.dram_tensor(name, list(shape), dt, kind=kind).ap()

    cur = [es]

    def sb(name, shape, dt):
        return cur[0].enter_context(nc.sbuf_tensor(name, list(shape), dt))

    class Rot:
        def __init__(self, items):
            self.items = list(items)
            self.i = 0

        def next(self):
            it = self.items[self.i % len(self.items)]
            self.i += 1
            return it

    def run_phase(fn, nbf=6):
        with ExitStack() as pes:
            cur[0] = pes
            ring["n"] = nbf
            ring["bf"] = [sb(f"wbf_{fn.__name__}_{i}", [128, 4, 512], BF16) for i in range(nbf)]
            ring["b"] = [Buf(f"wbf{i}") for i in range(nbf)]
            wcount[0] = 0
            fn()
            assert not wq and not wissued
            S.barrier()
        cur[0] = es

    def ps(name, shape, dt):
        return es.enter_context(nc.psum_tensor(name, list(shape), dt))

    xin = din("xin", [EXT, D])
    w_in = din("w_in", [D, IN_COLS])
    w_gate = din("w_gate", [D, 2 * D])
    w_pa = din("w_pa", [D, D])
    w_pb = din("w_pb", [512, D])
    w_out = din("w_out", [D, D])
    w1 = din("w1", [N_EXP * D, D_FF])
    w3 = din("w3", [N_EXP * D, D_FF])
    w2 = din("w2", [N_EXP * D_FF, D])
    g1 = din("g1", [1, D])
    g2 = din("g2", [1, D])
    gf = din("gf", [1, D])
    bgT = din("bgT", [128, 32])
    wr = din("wr", [128, KC * 72])
    br = din("br", [1, 72])
    sink = din("sink", [1, 16])
    eA = din("eA", [128, 4 * 3 * 512], BF16)
    eB = din("eB", [128, 2 * 4 * 2 * 128 + 4 * 2 * 32], BF16)
    vmask = din("vmask", [len(cfg["tiles"]) * 128, 51 * 128], BF16)
    yout = nc.dram_tensor("yout", [OWN, D], F32, kind="ExternalOutput").ap()

    XNT = dscratch("XNT", [KC * 128, EXT], BF16)
    KVS = dscratch("KVS", [EXT, KVS_COLS], BF16)
    OTS = dscratch("OTS", [20 * 128, OWN], BF16)
    HS = dscratch("HS", [OWN, D], F32)
    HN = dscratch("HN", [OWN + 128, D], BF16)
    SLOTI = dscratch("SLOTI", [NSLOT, 4], I32)
    YS = dscratch("YS", [2 * YT_STRIDE + NSLOT, D], BF16)
    b_XNT, b_KVS, b_OTS, b_HS, b_HN, b_SLOTI, b_YS, b_yout = (Buf(n) for n in "XNT KVS OTS HS HN SLOTI YS yout".split())

    ident = sb("ident", [128, 128], BF16)
    identf = sb("identf", [128, 128], F32)
    ones_bf = sb("ones_bf", [128, 128], BF16)
    neghalf = sb("neghalf", [128, 1], F32)
    b_const = Buf("const")

    S.op("pool", lambda h: h.memset(identf[:], 0.0), writes=[b_const])
    S.op("pool", lambda h: h.memset(neghalf[:], -0.5), pwrites=[b_const])
    S.op("pool", lambda h: h.memset(ones_bf[:], 1.0), pwrites=[b_const])
    b_identf = Buf("identf")
    S.op("pool", lambda h: h.affine_select(out=identf[:], in_=identf[:], pattern=[[1, 128]], compare_op=ALU.not_equal,
                                           fill=1.0, base=0, channel_multiplier=-1), reads=[b_const], writes=[b_identf])
    S.op("pool", lambda h: h.tensor_copy(out=ident[:], in_=identf[:]), reads=[b_identf], writes=[b_const])

    banks = [ps(f"bank{i}", [128, 512], F32) for i in range(8)]
    b_bank = [Buf(f"bank{i}") for i in range(8)]

    ring = {"bf": [], "b": [], "n": 0}
    wq = []
    wissued = []
    wcount = [0]

    def w_issue():
        src = wq.pop(0)
        i = wcount[0]
        wcount[0] += 1
        b = i % ring["n"]
        t_, b_ = ring["bf"][b], ring["b"][b]
        S.op("pool", lambda h: h.dma_start(out=t_[:], in_=src.rearrange("(kk p) c -> p kk c", p=128)),
             writes=[b_], dma=True)
        wissued.append((t_, b_))

    def w_push(srcs):
        wq.extend(srcs)

    def w_get():
        depth = ring["n"] - 2
        while wq and len(wissued) < depth + 1:
            w_issue()
        return wissued.pop(0)

    def piece(w, rg, cg):
        return w[rg * 512:(rg + 1) * 512, cg * 512:(cg + 1) * 512]

    def dma_split(eng, out_ap, in_ap, nsplit, **kw):
        n1 = out_ap.shape[1]
        step = n1 // nsplit
        first_w = kw.pop("writes", ())
        pw = kw.pop("pwrites", ())
        for i in range(nsplit):
            o_, i_ = out_ap[:, i * step:(i + 1) * step], in_ap[:, i * step:(i + 1) * step]
            if i == 0:
                S.op(eng, lambda h, o_=o_, i_=i_: h.dma_start(out=o_, in_=i_), writes=first_w, pwrites=pw, dma=True, **kw)
            else:
                S.op(eng, lambda h, o_=o_, i_=i_: h.dma_start(out=o_, in_=i_), pwrites=list(first_w) + list(pw), dma=True, **kw)

    evac_rr = [0]

    def evac_copy(out_ap, in_ap, reads, writes=(), pwrites=(), eng=None):
        e = eng or ("act", "dve")[evac_rr[0] % 2]
        if eng is None:
            evac_rr[0] += 1
        if e == "act":
            S.op("act", lambda h: h.copy(out=out_ap, in_=in_ap), reads=reads, writes=writes, pwrites=pwrites)
        else:
            S.op("dve", lambda h: h.tensor_copy(out=out_ap, in_=in_ap), reads=reads, writes=writes, pwrites=pwrites)

    def phase1():
        g1b = sb("g1b", [128, D], F32)
        b_g1b = Buf("g1b")
        S.op("sp", lambda h: h.dma_start(out=g1b[:], in_=g1[0, :].partition_broadcast(128)), writes=[b_g1b], dma=True)
        xst = [sb(f"xst{i}", [128, D], F32) for i in range(4)]
        b_xst = [Buf(f"xst{i}") for i in range(4)]
        junk = sb("junk1", [128, D], BF16)
        xnb = [sb(f"xnb{i}", [128, D], BF16) for i in range(8)]
        b_xnb = [Buf(f"xnb{i}") for i in range(8)]
        stat = [sb(f"stat{i}", [128, 4], F32) for i in range(4)]
        b_stat = [Buf(f"stat{i}") for i in range(4)]
        b_ms = [Buf(f"ms{i}") for i in range(4)]
        b_rstd = [Buf(f"rstd{i}") for i in range(4)]
        xnT = [sb(f"xnT{i}", [128, KC, T], BF16) for i in range(2)]
        b_xnT = [Buf(f"xnT{i}") for i in range(2)]
        kvo = [sb(f"kvo{i}", [128, 512], BF16) for i in range(4)]
        b_kvo = [Buf(f"kvo{i}") for i in range(4)]
        nkvo = 0
        w_push([piece(w_in, kg, KV_GROUPS[cg]) for (_c0, cgs_, _o) in cfg["chunks"] for cg in cgs_ for kg in range(4)])

        def norm_elem(ci):
            c0 = cfg["chunks"][ci][0]
            for tb in range(4):
                i = (ci * 4 + tb) % 4
                xi = (ci % 2) * 4 + tb
                r0 = c0 + tb * 128
                S.op("sp", lambda h, i=i, r0=r0: h.dma_start(out=xst[i][:], in_=xin[r0:r0 + 128, :]),
                     writes=[b_xst[i]], dma=True)
                S.op("act", lambda h, i=i: h.activation(out=junk[:], in_=xst[i][:], func=AF.Square,
                                                        accum_out=stat[i][:, 0:1]),
                     reads=[b_xst[i]], writes=[b_stat[i]])
                S.op("dve", lambda h, i=i: h.tensor_scalar(out=stat[i][:, 1:2], in0=stat[i][:, 0:1], scalar1=1.0 / D,
                                                          scalar2=EPS, op0=ALU.mult, op1=ALU.add),
                     reads=[b_stat[i]], writes=[b_ms[i]])
                S.op("pool", lambda h, i=i: h.tensor_tensor(out=stat[i][:, 2:3], in0=stat[i][:, 1:2], in1=neghalf[:], op=ALU.pow),
                     reads=[b_const, b_ms[i]], writes=[b_rstd[i]])
                S.op("dve", lambda h, i=i, xi=xi: h.scalar_tensor_tensor(out=xnb[xi][:], in0=xst[i][:], scalar=stat[i][:, 2:3],
                                                                        in1=g1b[:], op0=ALU.mult, op1=ALU.mult),
                     reads=[b_xst[i], b_rstd[i], b_g1b], writes=[b_xnb[xi]])

        if cfg.get("zero_left", True):
            zkv = sb("zkv", [128, KVS_COLS], BF16); b_zkv = Buf("zkv")
            S.op("pool", lambda h: h.memset(zkv[:], 0.0), writes=[b_zkv])
            for zb in range(HALO // 128):
                S.op("sp", lambda h, zb=zb: h.dma_start(out=KVS[P_EXT + zb * 128:P_EXT + (zb + 1) * 128, :], in_=zkv[:]),
                     reads=[b_zkv], pwrites=[b_KVS], dma=True)
        norm_elem(0)
        for ci, (c0, cgs, is_own) in enumerate(cfg["chunks"]):
            xt, b_xt = xnT[ci % 2], b_xnT[ci % 2]
            for tb in range(4):
                xi = (ci % 2) * 4 + tb
                for half in range(2):
                    bk = (2 * tb + half) % 8
                    tpv = banks[bk][:].bitcast(BF16)
                    for k8 in range(8):
                        k = half * 8 + k8
                        S.op("pe", lambda h, xi=xi, k=k, k8=k8, tpv=tpv: h.transpose(
                            out=tpv[:, k8 * 128:(k8 + 1) * 128], in_=xnb[xi][:, k * 128:(k + 1) * 128], identity=ident[:]),
                            reads=[b_xnb[xi], b_const], writes=[b_bank[bk]] if k8 == 0 else (), pwrites=() if k8 == 0 else [b_bank[bk]])
                    evac_copy(xt[:, half * 8:(half + 1) * 8, tb * 128:(tb + 1) * 128],
                              tpv.rearrange("p (k t) -> p k t", k=8), reads=[b_bank[bk]], pwrites=[b_xt])
            if ci + 1 < len(cfg["chunks"]):
                norm_elem(ci + 1)
            if is_own:
                dma_split("sp", XNT.rearrange("(k p) t -> p k t", p=128)[:, :, c0:c0 + T], xt[:], 4,
                          reads=[b_xt], pwrites=[b_XNT])
            for gi_, cg in enumerate(cgs):
                bset = (gi_ % 2) * 4
                for kg in range(4):
                    wt, b_wt = w_get()
                    for tb in range(4):
                        for kk in range(4):
                            first = (kg == 0 and kk == 0)
                            last = (kg == 3 and kk == 3)
                            S.op("pe", lambda h, tb=tb, kk=kk, kg=kg, wt=wt, xt=xt, first=first, last=last, bset=bset:
                                 h.matmul(banks[bset + tb][:], xt[:, kg * 4 + kk, tb * 128:(tb + 1) * 128], wt[:, kk, :],
                                          start=first, stop=last),
                                 reads=[b_xt, b_wt], writes=[b_bank[bset + tb]] if first else (),
                                 pwrites=() if first else [b_bank[bset + tb]])
                for tb in range(4):
                    j = nkvo % 4
                    nkvo += 1
                    evac_copy(kvo[j][:], banks[bset + tb][:], reads=[b_bank[bset + tb]], writes=[b_kvo[j]])
                    r0 = c0 + tb * 128
                    S.op("sp", lambda h, j=j, r0=r0, cg=cg: h.dma_start(out=KVS[r0:r0 + 128, cg * 512:(cg + 1) * 512], in_=kvo[j][:]),
                         reads=[b_kvo[j]], pwrites=[b_KVS], dma=True)


    def pe_mm(out_ap, lhsT, rhs, start, stop, reads, bbuf, first):
        S.op("pe", lambda h: h.matmul(out_ap, lhsT, rhs, start=start, stop=stop),
             reads=reads, writes=[bbuf] if first else (), pwrites=() if first else [bbuf])

    def phase2():
        eA_sb = sb("eA_sb", [128, 4 * 3 * 512], BF16)
        eB_sb = sb("eB_sb", [128, 2304], BF16)
        b_tab = Buf("tab")
        S.op("sp", lambda h: h.dma_start(out=eA_sb[:], in_=eA), writes=[b_tab], dma=True)
        S.op("sp", lambda h: h.dma_start(out=eB_sb[:], in_=eB), pwrites=[b_tab], dma=True)
        sink_sb = sb("sink_sb", [1, 16], F32)
        sexp = sb("sexp", [1, 16], F32)
        sinkrow = sb("sinkrow", [1, 16 * 128], BF16)
        b_sink, b_sexp, b_sinkrow = Buf("sink"), Buf("sexp"), Buf("sinkrow")
        S.op("sp", lambda h: h.dma_start(out=sink_sb[:], in_=sink), writes=[b_sink], dma=True)
        S.op("act", lambda h: h.activation(out=sexp[:], in_=sink_sb[:], func=AF.Exp), reads=[b_sink], writes=[b_sexp])
        S.op("dve", lambda h: h.tensor_copy(out=sinkrow[:].rearrange("p (h q) -> p h q", q=128),
                                            in_=sexp[:].unsqueeze(2).broadcast_to([1, 16, 128])),
             reads=[b_sexp], writes=[b_sinkrow])
        xT = sb("xT2", [128, KC, T], BF16); b_xT = Buf("xT2")
        vm2 = [sb(f"vm{i}", [128, 51, 128], BF16) for i in range(2)]; b_vm2 = [Buf(f"vm{i}") for i in range(2)]
        OT = sb("OT", [128, 20, T], BF16); b_OT = Buf("OT")
        QA = [sb(f"QA{i}", [128, 4, T], BF16) for i in range(2)]; b_QA = [Buf(f"QA{i}") for i in range(2)]
        QB = sb("QB", [128, 12, T], BF16); b_QB = Buf("QB")
        KnA = [sb(f"KnA{i}", [128, 6, 128], BF16) for i in range(2)]; b_KnA = [Buf(f"KnA{i}") for i in range(2)]
        VA = [sb(f"VA{i}", [128, 6, 128], BF16) for i in range(2)]; b_VA = [Buf(f"VA{i}") for i in range(2)]
        KTA = [sb(f"KTA{i}", [128, 768], BF16) for i in range(2)]; b_KTA = [Buf(f"KTA{i}") for i in range(2)]
        Kn1 = sb("Kn1", [128, 5, 128], BF16)
        Kn2 = sb("Kn2", [128, 4, 2, 128], BF16)
        Kn3a = sb("Kn3a", [128, 16, 128], BF16)
        Kn3b = sb("Kn3b", [32, 16, 128], BF16)
        V1 = [sb(f"V1_{i}", [128, 5, 128], BF16) for i in range(2)]
        V2 = [sb(f"V2_{i}", [128, 4, 2, 128], BF16) for i in range(2)]
        V3a = [sb(f"V3a_{i}", [128, 16, 128], BF16) for i in range(2)]
        V3b = [sb(f"V3b_{i}", [32, 16, 128], BF16) for i in range(2)]
        b_KnB = Buf("KnB")
        b_VB = [Buf(f"VB{i}") for i in range(2)]
        KT1 = [sb(f"KT1_{i}", [128, 5 * 128], BF16) for i in range(2)]
        KT2 = [sb(f"KT2_{i}", [128, 8 * 128], BF16) for i in range(2)]
        KT3 = [sb(f"KT3_{i}", [128, 16, 160], BF16) for i in range(2)]
        b_KTB = [Buf(f"KTB{i}") for i in range(2)]
        NP = 8
        Pb = [sb(f"P{i}", [128, 512], BF16) for i in range(NP)]
        b_P = [Buf(f"P{i}") for i in range(NP)]
        prot = Rot(range(NP))
        Rb = [sb(f"R{i}", [128, 512], F32) for i in range(2)]
        b_R = [Buf(f"R{i}") for i in range(2)]
        rrot = Rot(range(2))
        srot = Rot(range(4))
        orot = Rot((4, 5))
        lrot = Rot((6, 7))
        mulrot = Rot(("dve",))

        def load_rows(dst_t, src, bbuf, first, nsplit=1):
            four = len(dst_t.shape) == 4
            n1 = dst_t.shape[2] if four else dst_t.shape[1]
            step = n1 // nsplit
            for i in range(nsplit):
                if four:
                    d_, s_ = dst_t[:, :, i, :], src[:, :, i, :]
                else:
                    d_, s_ = dst_t[:, i * step:(i + 1) * step], src[:, i * step:(i + 1) * step]
                f_ = first and i == 0
                S.op("sp", lambda h, d_=d_, s_=s_: h.dma_start(out=d_, in_=s_), reads=[b_KVS],
                     writes=[bbuf] if f_ else (), pwrites=() if f_ else [bbuf], dma=True)

        def kv_src(kind, o, col):
            cs = slice(col, col + 128)
            if kind == "A":
                return KVS[o - 128:o - 128 + 768, cs].rearrange("(j p) d -> p j d", p=128)
            if kind == "1":
                return KVS[o - 64:o - 64 + 640, cs].rearrange("(j p) d -> p j d", p=128)
            if kind == "2":
                return KVS[o - 256:o - 256 + 1024, cs].rearrange("(s p ph) d -> p ph s d", s=2, p=128, ph=4)
            if kind == "3a":
                return KVS[o - 1024:o - 1024 + 2048, cs].rearrange("(p ph) d -> p ph d", ph=16)
            if kind == "3b":
                return KVS[o + 1024:o + 1024 + 512, cs].rearrange("(p ph) d -> p ph d", ph=16)

        def softmax_slot(sbk, nk, tab_ap):
            pi = prot.next()
            S.op("act", lambda h: h.activation(out=Pb[pi][0:nk, :], in_=banks[sbk][0:nk, :], func=AF.Exp, scale=SCALE),
                 reads=[b_bank[sbk]], writes=[b_P[pi]])
            me = mulrot.next()
            S.op(me, lambda h: h.tensor_tensor(out=Pb[pi][0:nk, :], in0=Pb[pi][0:nk, :], in1=tab_ap, op=ALU.mult),
                 reads=[b_tab, b_P[pi]], writes=[b_P[pi]])
            return pi

        def finish_unit(ob, lb, out_ap, split=None):
            ri = rrot.next()
            S.op("dve", lambda h: h.reciprocal(out=Rb[ri][:], in_=banks[lb][:]), reads=[b_bank[lb]], writes=[b_R[ri]])
            i0, i1 = banks[ob][:], Rb[ri][:]
            if split is not None:
                i0 = i0.rearrange("p (g q) -> p g q", g=split)
                i1 = i1.rearrange("p (g q) -> p g q", g=split)
            S.op("dve", lambda h: h.tensor_tensor(out=out_ap, in0=i0, in1=i1, op=ALU.mult),
                 reads=[b_bank[ob], b_R[ri]], pwrites=[b_OT])

        def qproj(cgs, dst, b_dst):
            for i, cg in enumerate(cgs):
                bks = [srot.next() for _ in range(4)]
                for kg in range(4):
                    wt, b_wt = w_get()
                    for cc in range(4):
                        for kk in range(4):
                            first = (kg == 0 and kk == 0)
                            pe_mm(banks[bks[cc]][:], wt[:, kk, cc * 128:(cc + 1) * 128], xT[:, kg * 4 + kk, :],
                                  first, (kg == 3 and kk == 3), [b_wt, b_xT], b_bank[bks[cc]], first)
                for cc in range(4):
                    evac_copy(dst[:, 4 * i + cc, :], banks[bks[cc]][:], reads=[b_bank[bks[cc]]], pwrites=[b_dst])

        for _ in cfg["tiles"]:
            w_push([piece(w_in, kg, kvh) for kvh in range(4) for kg in range(4)])
            w_push([piece(w_in, kg, 6 + gi) for gi in range(3) for kg in range(4)])
        class Step:
            def __init__(self):
                self.prep = None
                self.s1 = None
                self.s2 = None

        def tile_steps(ti, o, next_tile_prep):
            steps = []
            next_tile = next_tile_prep
            vm, b_vm = vm2[ti % 2], b_vm2[ti % 2]

            def tile_prep():
                dma_split("sp", xT[:], XNT.rearrange("(k p) t -> p k t", p=128)[:, :, o:o + T], 4, reads=[b_XNT], writes=[b_xT])
                S.op("sp", lambda h: h.dma_start(out=vm[:].rearrange("p s q -> p (s q)"), in_=vmask[ti * 128:(ti + 1) * 128, :]),
                     writes=[b_vm], dma=True)

            def a_prep(kvh):
                par = kvh % 2
                load_rows(KnA[par], kv_src("A", o, 0 * 512 + kvh * 128), b_KnA[par], True, 2)
                load_rows(VA[par], kv_src("A", o, 1 * 512 + kvh * 128), b_VA[par], True, 2)
                qproj([kvh], QA[par], b_QA[par])
                bk = srot.next()
                tpv = banks[bk][:].bitcast(BF16)
                for j in range(6):
                    S.op("pe", lambda h, j=j: h.transpose(out=tpv[:, j * 128:(j + 1) * 128], in_=KnA[par][:, j, :], identity=ident[:]),
                         reads=[b_KnA[par], b_const], writes=[b_bank[bk]] if j == 0 else (), pwrites=() if j == 0 else [b_bank[bk]])
                evac_copy(KTA[par][:], tpv[:, 0:768], reads=[b_bank[bk]], writes=[b_KTA[par]])

            def a_step(kvh, qb):
                par = kvh % 2
                st_ = Step()
                state = {}
                if qb == 0 and kvh == 0 and ti == 0:
                    st_.prep = lambda: (tile_prep(), a_prep(0))
                if qb == 1 and kvh < 3:
                    st_.prep = lambda: a_prep(kvh + 1)
                if qb == 1 and kvh == 3:
                    st_.prep = lambda: (qproj([6, 7, 8], QB, b_QB), b_prep(0))

                def s1():
                    pis = []
                    for s_ in range(3):
                        j = qb + s_
                        sbk = srot.next()
                        pe_mm(banks[sbk][:], KTA[par][:, j * 128:(j + 1) * 128], QA[par][:, :, qb * 128:(qb + 1) * 128],
                              True, True, [b_KTA[par], b_QA[par]], b_bank[sbk], True)
                        pis.append(softmax_slot(sbk, 128, eA_sb[:, (kvh * 3 + s_) * 512:(kvh * 3 + s_ + 1) * 512]))
                    state["pis"] = pis

                def s2():
                    pis = state["pis"]
                    ob, lb = orot.next(), lrot.next()
                    for s_ in range(3):
                        j = qb + s_
                        pe_mm(banks[ob][:], VA[par][:, j, :], Pb[pis[s_]][:], s_ == 0, s_ == 2, [b_VA[par], b_P[pis[s_]]], b_bank[ob], s_ == 0)
                    for s_ in range(3):
                        j = qb + s_
                        pe_mm(banks[lb][:], vm[:, j, :], Pb[pis[s_]][:], s_ == 0, False, [b_vm, b_P[pis[s_]]], b_bank[lb], s_ == 0)
                    pe_mm(banks[lb][:], ones_bf[0:1, :], sinkrow[0:1, kvh * 512:(kvh + 1) * 512], False, True,
                          [b_const, b_sinkrow], b_bank[lb], False)
                    finish_unit(ob, lb, OT[:, kvh * 4:(kvh + 1) * 4, qb * 128:(qb + 1) * 128], split=4)
                st_.s1, st_.s2 = s1, s2
                return st_

            for kvh in range(4):
                for qb in range(4):
                    steps.append(a_step(kvh, qb))
            tile_entry[ti] = lambda: (tile_prep(), a_prep(0))

            def b_prep(hh):
                par = hh % 2
                c = hh * 128
                load_rows(Kn1, kv_src("1", o, 2 * 512 + c), b_KnB, True)
                load_rows(Kn2, kv_src("2", o, 4 * 512 + c), b_KnB, False, 2)
                load_rows(Kn3a, kv_src("3a", o, 6 * 512 + c), b_KnB, False, 4)
                load_rows(Kn3b, kv_src("3b", o, 6 * 512 + c), b_KnB, False)
                load_rows(V1[par], kv_src("1", o, 3 * 512 + c), b_VB[par], True)
                load_rows(V2[par], kv_src("2", o, 5 * 512 + c), b_VB[par], False, 2)
                load_rows(V3a[par], kv_src("3a", o, 7 * 512 + c), b_VB[par], False, 4)
                load_rows(V3b[par], kv_src("3b", o, 7 * 512 + c), b_VB[par], False)
                groups = [
                    ([(Kn1[:, j, :], 128) for j in range(5)], 128, KT1[par][:, 0:640], None),
                    ([(Kn2[:, ph, s_, :], 128) for ph in range(4) for s_ in range(2)], 128, KT2[par][:, :], None),
                    ([(Kn3a[:, ph, :], 128) for ph in range(8)], 128, KT3[par][:, 0:8, 0:128], 8),
                    ([(Kn3a[:, ph, :], 128) for ph in range(8, 16)], 128, KT3[par][:, 8:16, 0:128], 8),
                    ([(Kn3b[:, ph, :], 32) for ph in range(16)], 32, KT3[par][:, :, 128:160], 16),
                ]
                for gidx, (jobs, w_, dst, nsplit) in enumerate(groups):
                    bk = srot.next()
                    tpv = banks[bk][:].bitcast(BF16)
                    for n_, (src, npart) in enumerate(jobs):
                        S.op("pe", lambda h, n_=n_, src=src, npart=npart, tpv=tpv, w_=w_: h.transpose(
                            out=tpv[:, n_ * w_:(n_ + 1) * w_], in_=src, identity=ident[0:npart, 0:npart]),
                            reads=[b_KnB, b_const], writes=[b_bank[bk]] if n_ == 0 else (), pwrites=() if n_ == 0 else [b_bank[bk]])
                    srcv = tpv[:, 0:len(jobs) * w_]
                    if nsplit is not None:
                        srcv = srcv.rearrange("p (n w) -> p n w", n=nsplit)
                    evac_copy(dst, srcv, reads=[b_bank[bk]], writes=[b_KTB[par]] if gidx == 0 else (),
                              pwrites=() if gidx == 0 else [b_KTB[par]])

            def b_step(hh, nslot, shared):
                par = hh % 2
                gi, s_ = nslot // 2, nslot % 2
                st_ = Step()
                state = {}
                if nslot == 3 and hh < 3:
                    st_.prep = lambda: b_prep(hh + 1)
                if nslot == 2 and hh == 3 and next_tile is not None:
                    st_.prep = next_tile
                nsub, nq = (4, 128) if gi < 2 else (16, 32)
                nk = 32 if (gi == 2 and s_ == 1) else 128
                qv = QB[:, gi * 4 + hh, :]

                def s1():
                    sbk = srot.next()
                    for sp in range(nsub):
                        if gi == 0:
                            lhsT = KT1[par][:, (sp + s_) * 128:(sp + s_ + 1) * 128]
                            rhs = qv[:, sp * 128:(sp + 1) * 128]
                        elif gi == 1:
                            lhsT = KT2[par][:, (sp * 2 + s_) * 128:(sp * 2 + s_ + 1) * 128]
                            rhs = qv.rearrange("p (u r) -> p r u", r=4)[:, sp, :]
                        else:
                            lhsT = KT3[par][:, sp, s_ * 128:s_ * 128 + nk]
                            rhs = qv.rearrange("p (u r) -> p r u", r=16)[:, sp, :]
                        pe_mm(banks[sbk][0:nk, sp * nq:(sp + 1) * nq], lhsT, rhs, sp == 0, sp == nsub - 1,
                              [b_KTB[par], b_QB], b_bank[sbk], sp == 0)
                    if gi < 2:
                        tab = eB_sb[:, ((gi * 4 + hh) * 2 + s_) * 128:((gi * 4 + hh) * 2 + s_ + 1) * 128]
                    else:
                        tab = eB_sb[0:nk, 2048 + (hh * 2 + s_) * 32:2048 + (hh * 2 + s_ + 1) * 32]
                    tab_b = tab.unsqueeze(1).broadcast_to([nk, nsub, nq])
                    pi = prot.next()
                    S.op("act", lambda h: h.activation(out=Pb[pi][0:nk, :], in_=banks[sbk][0:nk, :], func=AF.Exp, scale=SCALE),
                         reads=[b_bank[sbk]], writes=[b_P[pi]])
                    S.op("dve", lambda h: h.tensor_tensor(
                        out=Pb[pi][0:nk, :].rearrange("p (s q) -> p s q", s=nsub),
                        in0=Pb[pi][0:nk, :].rearrange("p (s q) -> p s q", s=nsub), in1=tab_b, op=ALU.mult),
                        reads=[b_tab, b_P[pi]], writes=[b_P[pi]])
                    state["pi"] = pi

                def s2():
                    pi = state["pi"]
                    if nslot == 0:
                        shared["ob"], shared["lb"] = orot.next(), lrot.next()
                    ob, lb = shared["ob"], shared["lb"]
                    for which, bkx in ((0, ob), (1, lb)):
                        for sp in range(nsub):
                            if gi == 0:
                                vv = V1[par][:, sp + s_, :]
                                sc = 6 + sp + s_
                                out_ap = banks[bkx][:, sp * 128:(sp + 1) * 128]
                            elif gi == 1:
                                vv = V2[par][:, sp, s_, :]
                                sc = 11 + sp * 2 + s_
                                out_ap = banks[bkx][:].rearrange("p (u r) -> p r u", r=4)[:, sp, :]
                            else:
                                vv = (V3a[par] if s_ == 0 else V3b[par])[0:nk, sp, :]
                                sc = 19 + sp * 2 + s_
                                out_ap = banks[bkx][:].rearrange("p (u r) -> p r u", r=16)[:, sp, :]
                            lhsT = vv if which == 0 else vm[0:nk, sc, :]
                            very_first = (nslot == 0 and sp == 0)
                            very_last = (nslot == 5 and sp == nsub - 1)
                            pe_mm(out_ap, lhsT, Pb[pi][0:nk, sp * nq:(sp + 1) * nq], very_first, very_last,
                                  [b_VB[par] if which == 0 else b_vm, b_P[pi]], b_bank[bkx], very_first)
                    if nslot == 5:
                        finish_unit(ob, lb, OT[:, 16 + hh, :])
                        if hh == 3:
                            ro = own_row(o)
                            dma_split("sp", OTS.rearrange("(n p) t -> p n t", p=128)[:, :, ro:ro + T], OT[:], 5,
                                      reads=[b_OT], pwrites=[b_OTS])
                st_.s1, st_.s2 = s1, s2
                return st_

            for hh in range(4):
                shared = {}
                for nslot in range(6):
                    steps.append(b_step(hh, nslot, shared))
            return steps

        steps = []
        tile_entry = {}
        ntl = len(cfg["tiles"])
        per_tile = []
        for ti in reversed(range(ntl)):
            nxt = (lambda t=ti + 1: tile_entry[t]()) if ti + 1 < ntl else None
            per_tile.append(tile_steps(ti, cfg["tiles"][ti], nxt))
        for st_list in reversed(per_tile):
            steps.extend(st_list)
        for i, st_ in enumerate(steps):
            if i == 0:
                if st_.prep:
                    st_.prep()
                st_.s1()
            if i + 1 < len(steps):
                nx = steps[i + 1]
                if nx.prep:
                    nx.prep()
                nx.s1()
            st_.s2()


    NBLK = OWN // 128
    SLOTS = sb("SLOTS", [128, NBLK * 2], I32); b_SLOTS = Buf("SLOTS")
    Asum = sb("Asum", [128, 64], BF16); b_Asum = Buf("Asum")
    Umat = sb("Umat", [128, 128], BF16)
    ECi = sb("ECi", [128, 64], I32)
    ECf = sb("ECf", [128, 64], F32)
    tokid = sb("tokid", [128, NBLK], I32)
    tokid2 = sb("tokid2", [128, NBLK], I32)
    b_c3 = Buf("c3")

    def phase3():
        S.op("pool", lambda h: h.memset(Asum[:], 0.0), writes=[b_Asum])
        S.op("pool", lambda h: h.memset(Umat[:], 1.0), writes=[b_c3])
        b_U = Buf("U")
        S.op("pool", lambda h: h.affine_select(out=Umat[:], in_=Umat[:], pattern=[[1, 128]], compare_op=ALU.is_gt,
                                               fill=0.0, base=0, channel_multiplier=-1), reads=[b_c3], writes=[b_U])
        S.op("pool", lambda h: h.iota(ECi[:], pattern=[[CAP, 64]], base=0, channel_multiplier=0), pwrites=[b_c3])
        S.op("pool", lambda h: h.iota(tokid[:], pattern=[[128, NBLK]], base=0, channel_multiplier=1), pwrites=[b_c3])
        S.op("pool", lambda h: h.iota(tokid2[:], pattern=[[128, NBLK]], base=YT_STRIDE, channel_multiplier=1), pwrites=[b_c3])
        b_EC = Buf("EC")
        S.op("pool", lambda h: h.tensor_copy(out=ECf[:], in_=ECi[:]), reads=[b_c3], writes=[b_EC])
        zt = sb("zt", [128, D], BF16); b_zt = Buf("zt")
        S.op("pool", lambda h: h.memset(zt[:], 0.0), writes=[b_zt])
        S.op("sp", lambda h: h.dma_start(out=HN[OWN:OWN + 128, :], in_=zt[:]), reads=[b_zt], pwrites=[b_HN], dma=True)
        initI = sb("initI", [128, 128, 4], I32); b_initI = Buf("initI")
        S.op("pool", lambda h: h.memset(initI[:], 0), writes=[b_initI])
        b_initI2 = Buf("initI2")
        S.op("pool", lambda h: h.memset(initI[:, :, 0:1], OWN), reads=[b_initI], writes=[b_initI2])
        S.op("pool", lambda h: h.iota(initI[:, :, 2:3], pattern=[[1, 128], [1, 1]], base=2 * YT_STRIDE, channel_multiplier=128),
             reads=[b_initI], pwrites=[b_initI2])
        S.op("sp", lambda h: h.dma_start(out=SLOTI.rearrange("(p j) c -> p j c", p=128), in_=initI[:]),
             reads=[b_initI, b_initI2], writes=[b_SLOTI], dma=True)
        g2b = sb("g2b", [128, D], F32); b_g2b = Buf("g2b")
        S.op("sp", lambda h: h.dma_start(out=g2b[:], in_=g2[0, :].partition_broadcast(128)), writes=[b_g2b], dma=True)
        brb = sb("brb", [128, 72], F32)
        wr_sb = sb("wr_sb", [128, KC, 72], F32)
        bg_sb = sb("bg_sb", [128, 32], F32)
        b_rc = Buf("rconst")
        S.op("sp", lambda h: h.dma_start(out=brb[:], in_=br[0, :].partition_broadcast(128)), writes=[b_rc], dma=True)
        S.op("sp", lambda h: h.dma_start(out=wr_sb[:].rearrange("p k c -> p (k c)"), in_=wr), pwrites=[b_rc], dma=True)
        S.op("sp", lambda h: h.dma_start(out=bg_sb[:], in_=bgT), pwrites=[b_rc], dma=True)

        xT = sb("xT3", [128, KC, T], BF16); b_xT = Buf("xT3")
        OTt = sb("OTt", [128, 20, T], BF16); b_OTt = Buf("OTt")
        GA = sb("GA", [128, 4, T], F32); b_GA = Buf("GA")
        GB = sb("GB", [128, 4, T], F32); b_GB = Buf("GB")
        MT = sb("MT", [128, KC, T], BF16); b_MT = Buf("MT")
        Hh = sb("Hh", [128, 4, D], F32); b_Hh = [Buf(f"Hh{i}") for i in range(4)]
        xres = [sb(f"xres{i}", [128, 512], F32) for i in range(4)]
        b_xres = [Buf(f"xres{i}") for i in range(4)]
        xrot = Rot(range(4))
        junk = sb("junk3", [128, D], BF16)
        hnf = sb("hnf", [128, D], F32); b_hnf = Buf("hnf")
        hnb = sb("hnb", [128, D], BF16); b_hnb = Buf("hnb")
        hlo = sb("hlo", [128, D], BF16); b_hlo = Buf("hlo")
        hiT = sb("hiT", [128, KC, 128], BF16); b_hiT = Buf("hiT")
        loT = sb("loT", [128, KC, 128], BF16); b_loT = Buf("loT")
        wr_hi = sb("wr_hi", [128, KC, 72], BF16)
        wr_lo = sb("wr_lo", [128, KC, 72], BF16)
        b_wrs = Buf("wrs")
        S.op("act", lambda h: h.copy(out=wr_hi[:], in_=wr_sb[:]), reads=[b_rc], writes=[b_wrs])
        S.op("dve", lambda h: h.tensor_tensor(out=wr_lo[:], in0=wr_sb[:], in1=wr_hi[:], op=ALU.subtract), reads=[b_rc, b_wrs], pwrites=[b_wrs])
        st = sb("st3", [128, 8], F32)
        b_ssq, b_ms, b_rstd = Buf("ssq3"), Buf("ms3"), Buf("rstd3")
        LG = sb("LG", [128, 72], F32); b_LG = Buf("LG")
        rt = sb("rt", [128, 16], F32)
        ohg = sb("ohg", [128, 8], F32)
        eg = sb("eg", [128, 8], F32)
        tmp64 = sb("tmp64", [128, 64], F32)
        lsel = sb("lsel", [128, 8], F32)
        msk = sb("msk", [128, 8], F32)
        oh1 = sb("oh1", [128, 8], F32)
        oh2 = sb("oh2", [128, 8], F32)
        A1 = sb("A1", [128, 64], F32)
        A2 = sb("A2", [128, 64], F32)
        Abf = sb("Abf", [128, 64], BF16)
        rk = sb("rk", [128, 64], F32)
        junk64 = sb("junk64", [128, 64], F32)
        sl_f = sb("sl_f", [128, 2], F32)
        sl_i = sb("sl_i", [128, 2], I32)
        info = [sb(f"info{i}", [128, 4], I32) for i in range(2)]
        bR = {n: Buf("r_" + n) for n in "mg ohg negmg se pg lsel m1 oh1 negm1 msk m2 oh2 e2 den rden w1 w2 A1 A2 Abf rk slf sli info0 info1 tmp64 A2j".split()}

        def dve(fn, reads, writes, pw=()):
            S.op("dve", fn, reads=[bR[r] if isinstance(r, str) else r for r in reads],
                 writes=[bR[w] if isinstance(w, str) else w for w in writes],
                 pwrites=[bR[w] if isinstance(w, str) else w for w in pw])

        def proj_ws(w, cg, nkg, rhs_fn, rhs_reads, bset):
            for kg in range(nkg):
                wt, b_wt = w_get()
                for cc in range(4):
                    for kk in range(4):
                        first = (kg == 0 and kk == 0)
                        pe_mm(banks[bset + cc][:], wt[:, kk, cc * 128:(cc + 1) * 128], rhs_fn(kg * 4 + kk),
                              first, (kg == nkg - 1 and kk == 3), [b_wt] + rhs_reads, b_bank[bset + cc], first)

        for _ in cfg["tiles"]:
            for fg in range(4):
                w_push([piece(w_gate, kg, fg) for kg in range(4)] + [piece(w_gate, kg, 4 + fg) for kg in range(4)]
                       + [piece(w_pa, kg, fg) for kg in range(4)] + [piece(w_pb, 0, fg)])
            for cg in range(4):
                w_push([piece(w_out, kg, cg) for kg in range(4)])
        def tail1(tb, ro, blk):
            r0 = ro + tb * 128
            S.op("sp", lambda h, tb=tb, r0=r0: h.dma_start(out=HS[r0:r0 + 128, :], in_=Hh[:, tb, :]),
                 reads=[b_Hh[tb]], pwrites=[b_HS], dma=True)
            S.op("act", lambda h, tb=tb: h.activation(out=junk[:], in_=Hh[:, tb, :], func=AF.Square, accum_out=st[:, 0:1]),
                 reads=[b_Hh[tb]], writes=[b_ssq])
            S.op("dve", lambda h: h.tensor_scalar(out=st[:, 1:2], in0=st[:, 0:1], scalar1=1.0 / D, scalar2=EPS,
                                                  op0=ALU.mult, op1=ALU.add), reads=[b_ssq], writes=[b_ms])
            S.op("pool", lambda h: h.tensor_tensor(out=st[:, 2:3], in0=st[:, 1:2], in1=neghalf[:], op=ALU.pow),
                 reads=[b_ms, b_const], writes=[b_rstd])
            S.op("dve", lambda h, tb=tb: h.scalar_tensor_tensor(out=hnf[:], in0=Hh[:, tb, :], scalar=st[:, 2:3], in1=g2b[:],
                                                               op0=ALU.mult, op1=ALU.mult),
                 reads=[b_Hh[tb], b_rstd, b_g2b], writes=[b_hnf])
            S.op("act", lambda h: h.copy(out=hnb[:], in_=hnf[:]), reads=[b_hnf], writes=[b_hnb])
            S.op("dve", lambda h: h.tensor_tensor(out=hlo[:], in0=hnf[:], in1=hnb[:], op=ALU.subtract),
                 reads=[b_hnf, b_hnb], writes=[b_hlo])
            S.op("sp", lambda h, r0=r0: h.dma_start(out=HN[r0:r0 + 128, :], in_=hnb[:]), reads=[b_hnb], pwrites=[b_HN], dma=True)

        def tail2(tb, ro, blk):
            for which, (src, b_src, dstT, b_dstT) in enumerate(((hnb, b_hnb, hiT, b_hiT), (hlo, b_hlo, loT, b_loT))):
                for half in range(2):
                    bk = which * 2 + half
                    tpv = banks[bk][:].bitcast(BF16)
                    for k8 in range(8):
                        k = half * 8 + k8
                        S.op("pe", lambda h, k=k, k8=k8, tpv=tpv, src=src: h.transpose(
                            out=tpv[:, k8 * 128:(k8 + 1) * 128], in_=src[:, k * 128:(k + 1) * 128], identity=ident[:]),
                            reads=[b_src, b_const], writes=[b_bank[bk]] if k8 == 0 else (), pwrites=() if k8 == 0 else [b_bank[bk]])
                    evac_copy(dstT[:, half * 8:(half + 1) * 8, :], tpv.rearrange("p (k t) -> p k t", k=8),
                              reads=[b_bank[bk]], writes=[b_dstT] if half == 0 else (), pwrites=() if half == 0 else [b_dstT], eng="act")
            lb_ = 4
            terms = [(hiT, b_hiT, wr_hi), (loT, b_loT, wr_hi), (hiT, b_hiT, wr_lo)]
            nmm = 0
            for (aT, b_aT, wmat) in terms:
                for k in range(KC):
                    pe_mm(banks[lb_][:, 0:72], aT[:, k, :], wmat[:, k, :], nmm == 0, nmm == 3 * KC - 1, [b_aT, b_wrs], b_bank[lb_], nmm == 0)
                    nmm += 1
            dve(lambda h: h.tensor_tensor(out=LG[:], in0=banks[lb_][:, 0:72], in1=brb[:], op=ALU.add), [b_bank[lb_], b_rc], [b_LG])
            dve(lambda h: h.tensor_reduce(out=rt[:, 0:1], in_=LG[:, 0:8], axis=AX.X, op=ALU.max), [b_LG], ["mg"])
            dve(lambda h: h.tensor_scalar(out=ohg[:], in0=LG[:, 0:8], scalar1=rt[:, 0:1], scalar2=None, op0=ALU.is_equal), [b_LG, "mg"], ["ohg"])
            dve(lambda h: h.tensor_scalar(out=rt[:, 1:2], in0=rt[:, 0:1], scalar1=-1.0, scalar2=None, op0=ALU.mult), ["mg"], ["negmg"])
            S.op("act", lambda h: h.activation(out=eg[:], in_=LG[:, 0:8], func=AF.Exp, bias=rt[:, 1:2], accum_out=rt[:, 2:3]),
                 reads=[b_LG, bR["negmg"]], writes=[bR["se"]])
            dve(lambda h: h.reciprocal(out=rt[:, 3:4], in_=rt[:, 2:3]), ["se"], ["pg"])
            dve(lambda h: h.tensor_tensor(out=tmp64[:].rearrange("p (g j) -> p g j", g=8),
                                          in0=LG[:, 8:72].rearrange("p (g j) -> p g j", g=8),
                                          in1=ohg[:].unsqueeze(2).broadcast_to([128, 8, 8]), op=ALU.mult), [b_LG, "ohg"], ["tmp64"])
            dve(lambda h: h.tensor_reduce(out=lsel[:], in_=tmp64[:].rearrange("p (g j) -> p j g", g=8), axis=AX.X, op=ALU.add),
                ["tmp64"], ["lsel"])
            dve(lambda h: h.tensor_reduce(out=rt[:, 4:5], in_=lsel[:], axis=AX.X, op=ALU.max), ["lsel"], ["m1"])
            dve(lambda h: h.tensor_scalar(out=oh1[:], in0=lsel[:], scalar1=rt[:, 4:5], scalar2=None, op0=ALU.is_equal), ["lsel", "m1"], ["oh1"])
            dve(lambda h: h.tensor_scalar(out=rt[:, 5:6], in0=rt[:, 4:5], scalar1=-1.0, scalar2=None, op0=ALU.mult), ["m1"], ["negm1"])
            dve(lambda h: h.scalar_tensor_tensor(out=msk[:], in0=oh1[:], scalar=-1e30, in1=lsel[:], op0=ALU.mult, op1=ALU.add),
                ["oh1", "lsel"], ["msk"])
            dve(lambda h: h.tensor_reduce(out=rt[:, 6:7], in_=msk[:], axis=AX.X, op=ALU.max), ["msk"], ["m2"])
            dve(lambda h: h.tensor_scalar(out=oh2[:], in0=msk[:], scalar1=rt[:, 6:7], scalar2=None, op0=ALU.is_equal), ["msk", "m2"], ["oh2"])
            S.op("act", lambda h: h.activation(out=rt[:, 7:8], in_=rt[:, 6:7], func=AF.Exp, bias=rt[:, 5:6]),
                 reads=[bR["m2"], bR["negm1"]], writes=[bR["e2"]])
            dve(lambda h: h.tensor_scalar(out=rt[:, 8:9], in0=rt[:, 7:8], scalar1=1.0, scalar2=None, op0=ALU.add), ["e2"], ["den"])
            dve(lambda h: h.reciprocal(out=rt[:, 9:10], in_=rt[:, 8:9]), ["den"], ["rden"])
            dve(lambda h: h.tensor_tensor(out=rt[:, 10:11], in0=rt[:, 9:10], in1=rt[:, 3:4], op=ALU.mult), ["rden", "pg"], ["w1"])
            dve(lambda h: h.tensor_tensor(out=rt[:, 11:12], in0=rt[:, 10:11], in1=rt[:, 7:8], op=ALU.mult), ["w1", "e2"], ["w2"])
            dve(lambda h: h.tensor_tensor(out=A1[:].rearrange("p (g j) -> p g j", g=8),
                                          in0=ohg[:].unsqueeze(2).broadcast_to([128, 8, 8]),
                                          in1=oh1[:].unsqueeze(1).broadcast_to([128, 8, 8]), op=ALU.mult), ["ohg", "oh1"], ["A1"])
            dve(lambda h: h.tensor_tensor(out=A2[:].rearrange("p (g j) -> p g j", g=8),
                                          in0=ohg[:].unsqueeze(2).broadcast_to([128, 8, 8]),
                                          in1=oh2[:].unsqueeze(1).broadcast_to([128, 8, 8]), op=ALU.mult), ["ohg", "oh2"], ["A2"])
            dve(lambda h: h.tensor_tensor(out=Abf[:], in0=A1[:], in1=A2[:], op=ALU.add), ["A1", "A2"], ["Abf"])

        def tail3(tb, ro, blk):
            rb_ = 5
            pe_mm(banks[rb_][:, 0:64], Umat[:], Abf[:], True, False, [b_U, bR["Abf"]], b_bank[rb_], True)
            pe_mm(banks[rb_][:, 0:64], ones_bf[:], Asum[:], False, True, [b_const, b_Asum], b_bank[rb_], False)
            dve(lambda h: h.tensor_tensor(out=rk[:], in0=banks[rb_][:, 0:64], in1=ECf[:], op=ALU.add), [b_bank[rb_], b_EC], ["rk"])
            S.op("pool", lambda h: h.tensor_tensor(out=Asum[:], in0=Asum[:], in1=Abf[:], op=ALU.add),
                 reads=[bR["Abf"], b_Asum], writes=[b_Asum])
            dve(lambda h: h.tensor_tensor(out=tmp64[:], in0=rk[:], in1=A1[:], op=ALU.mult), ["rk", "A1"], ["tmp64"])
            dve(lambda h: h.tensor_reduce(out=sl_f[:, 0:1], in_=tmp64[:], axis=AX.X, op=ALU.add), ["tmp64"], [], ["slf"])
            dve(lambda h: h.tensor_tensor(out=junk64[:], in0=rk[:], in1=A2[:], op=ALU.mult), ["rk", "A2"], ["A2j"])
            dve(lambda h: h.tensor_reduce(out=sl_f[:, 1:2], in_=junk64[:], axis=AX.X, op=ALU.add), ["A2j"], [], ["slf"])
            dve(lambda h: h.tensor_copy(out=sl_i[:], in_=sl_f[:]), ["slf"], ["sli"])
            dve(lambda h, blk=blk: h.tensor_copy(out=SLOTS[:, blk * 2:blk * 2 + 2], in_=sl_i[:]), ["sli"], [], [b_SLOTS])
            for kx in range(2):
                nm = f"info{kx}"
                dve(lambda h, kx=kx, blk=blk: h.tensor_copy(out=info[kx][:, 0:1], in_=tokid[:, blk:blk + 1]), [b_c3], [nm])
                dve(lambda h, kx=kx, blk=blk: h.tensor_copy(out=info[kx][:, 2:3], in_=(tokid if kx == 0 else tokid2)[:, blk:blk + 1]),
                    [b_c3, nm], [nm])
                dve(lambda h, kx=kx: h.tensor_copy(out=info[kx][:, 1:2].bitcast(F32), in_=rt[:, 10 + kx:11 + kx]),
                    ["w1", "w2", nm], [nm])
                S.op("pool", lambda h, kx=kx: h.indirect_dma_start(
                    out=SLOTI, out_offset=bass.IndirectOffsetOnAxis(ap=sl_i[:, kx:kx + 1], axis=0),
                    in_=info[kx][:], in_offset=None),
                    reads=[bR[nm], bR["sli"], b_SLOTI], pwrites=[b_SLOTI], dma=True)

        pending = []
        done = set()

        def run_slot(slot):
            if not pending:
                return
            for part, fn_, b in ((3, tail3, slot - 2), (2, tail2, slot - 1), (1, tail1, slot)):
                if 0 <= b < 4:
                    key = (pending[b][1], b, part)
                    if key in done:
                        continue
                    done.add(key)
                    fn_(*pending[b])

        blk = 0
        for ti, o in enumerate(cfg["tiles"]):
            ro = own_row(o)
            dma_split("sp", xT[:], XNT.rearrange("(k p) t -> p k t", p=128)[:, :, o:o + T], 4, reads=[b_XNT], writes=[b_xT])
            dma_split("sp", OTt[:], OTS.rearrange("(n p) t -> p n t", p=128)[:, :, ro:ro + T], 5, reads=[b_OTS], writes=[b_OTt])
            for fg in range(4):
                proj_ws(w_gate, fg, 4, lambda k: xT[:, k, :], [b_xT], 0)
                for cc in range(4):
                    c_ = fg * 4 + cc
                    S.op("act", lambda h, cc=cc, c_=c_: h.activation(out=GA[:, cc, :], in_=banks[cc][:], func=AF.Sigmoid,
                                                                     bias=bg_sb[:, c_:c_ + 1]),
                         reads=[b_bank[cc], b_rc], writes=[b_GA] if cc == 0 else (), pwrites=() if cc == 0 else [b_GA])
                proj_ws(w_gate, 4 + fg, 4, lambda k: xT[:, k, :], [b_xT], 4)
                for cc in range(4):
                    c_ = 16 + fg * 4 + cc
                    S.op("act", lambda h, cc=cc, c_=c_: h.activation(out=GB[:, cc, :], in_=banks[4 + cc][:], func=AF.Sigmoid,
                                                                     bias=bg_sb[:, c_:c_ + 1]),
                         reads=[b_bank[4 + cc], b_rc], writes=[b_GB] if cc == 0 else (), pwrites=() if cc == 0 else [b_GB])
                proj_ws(w_pa, fg, 4, lambda k: OTt[:, k, :], [b_OTt], 0)
                for cc in range(4):
                    S.op("dve", lambda h, cc=cc: h.tensor_tensor(out=GA[:, cc, :], in0=banks[cc][:], in1=GA[:, cc, :], op=ALU.mult),
                         reads=[b_bank[cc], b_GA], pwrites=[b_GA])
                proj_ws(w_pb, fg, 1, lambda k: OTt[:, 16 + k, :], [b_OTt], 4)
                for cc in range(4):
                    S.op("dve", lambda h, cc=cc: h.tensor_tensor(out=GB[:, cc, :], in0=banks[4 + cc][:], in1=GB[:, cc, :], op=ALU.mult),
                         reads=[b_bank[4 + cc], b_GB], pwrites=[b_GB])
                S.op("pool", lambda h, fg=fg: h.tensor_tensor(out=MT[:, fg * 4:(fg + 1) * 4, :], in0=GA[:], in1=GB[:], op=ALU.add),
                     reads=[b_GA, b_GB], writes=[b_MT] if fg == 0 else (), pwrites=() if fg == 0 else [b_MT])
                run_slot(fg)
            for cg in range(4):
                bset = (cg % 2) * 4
                for kg in range(4):
                    wt, b_wt = w_get()
                    for tb in range(4):
                        for kk in range(4):
                            first = (kg == 0 and kk == 0)
                            pe_mm(banks[bset + tb][:], MT[:, kg * 4 + kk, tb * 128:(tb + 1) * 128], wt[:, kk, :],
                                  first, (kg == 3 and kk == 3), [b_wt, b_MT], b_bank[bset + tb], first)
                for tb in range(4):
                    xi = xrot.next()
                    r0 = o + tb * 128
                    S.op("sp", lambda h, xi=xi, r0=r0, cg=cg: h.dma_start(out=xres[xi][:], in_=xin[r0:r0 + 128, cg * 512:(cg + 1) * 512]),
                         writes=[b_xres[xi]], dma=True)
                    S.op("dve", lambda h, xi=xi, tb=tb, cg=cg, bset=bset: h.tensor_tensor(
                        out=Hh[:, tb, cg * 512:(cg + 1) * 512], in0=banks[bset + tb][:], in1=xres[xi][:], op=ALU.add),
                        reads=[b_bank[bset + tb], b_xres[xi]], writes=[b_Hh[tb]] if cg == 0 else (), pwrites=() if cg == 0 else [b_Hh[tb]])
                run_slot(4 + cg)
            pending = [(tb, ro, blk + tb) for tb in range(4)]
            blk += 4
        for slot in range(8):
            run_slot(slot)

    def phase4():
        si = [sb(f"si{i}", [128, 4], I32) for i in range(4)]; b_si = [Buf(f"si{i}") for i in range(4)]
        X = [sb(f"X{i}", [128, D], BF16) for i in range(4)]; b_X = [Buf(f"X{i}") for i in range(4)]
        XT = [sb(f"XT{i}", [128, KC, 256], BF16) for i in range(2)]; b_XTe = [Buf(f"XT{i}") for i in range(2)]
        S1 = sb("S1", [128, 1024], F32); b_S1 = Buf("S1")
        HT = [sb(f"HT{i}", [128, 4, 256], BF16) for i in range(2)]; b_HT = [Buf(f"HT{i}") for i in range(2)]
        Y = [sb(f"Y{i}", [128, D], BF16) for i in range(4)]; b_Y = [Buf(f"Y{i}") for i in range(4)]
        yev = Rot(("act", "dve"))
        for e in cfg["experts"]:
            w_push([piece(w1, e * 4 + kg, 0) for kg in range(4)] + [piece(w3, e * 4 + kg, 0) for kg in range(4)]
                   + [piece(w2, e, cg) for cg in range(4)])
        for ei, e in enumerate(cfg["experts"]):
            xt, b_xt = XT[ei % 2], b_XTe[ei % 2]
            sis = []
            for sb_ in range(2):
                i4 = (ei % 2) * 2 + sb_
                sis.append(i4)
                r0 = e * CAP + sb_ * 128
                S.op("sp", lambda h, i4=i4, r0=r0: h.dma_start(out=si[i4][:], in_=SLOTI[r0:r0 + 128, :]),
                     reads=[b_SLOTI], writes=[b_si[i4]], dma=True)
                S.op("pool", lambda h, i4=i4: h.indirect_dma_start(
                    out=X[i4][:], out_offset=None, in_=HN, in_offset=bass.IndirectOffsetOnAxis(ap=si[i4][:, 0:1], axis=0)),
                    reads=[b_si[i4], b_HN], writes=[b_X[i4]], dma=True)
                for half in range(2):
                    bk = 4 + (2 * sb_ + half) % 4
                    tpv = banks[bk][:].bitcast(BF16)
                    for k8 in range(8):
                        k = half * 8 + k8
                        S.op("pe", lambda h, i4=i4, k=k, k8=k8, tpv=tpv: h.transpose(
                            out=tpv[:, k8 * 128:(k8 + 1) * 128], in_=X[i4][:, k * 128:(k + 1) * 128], identity=ident[:]),
                            reads=[b_X[i4], b_const], writes=[b_bank[bk]] if k8 == 0 else (), pwrites=() if k8 == 0 else [b_bank[bk]])
                    first_w = (sb_ == 0 and half == 0)
                    evac_copy(xt[:, half * 8:(half + 1) * 8, sb_ * 128:(sb_ + 1) * 128], tpv.rearrange("p (k t) -> p k t", k=8),
                              reads=[b_bank[bk]], writes=[b_xt] if first_w else (), pwrites=() if first_w else [b_xt])
            for which in range(2):
                for kg in range(4):
                    wt, b_wt = w_get()
                    for cc in range(4):
                        bk = which * 2 + cc // 2
                        for kk in range(4):
                            vfirst = (kg == 0 and kk == 0 and cc % 2 == 0)
                            vlast = (kg == 3 and kk == 3 and cc % 2 == 1)
                            pe_mm(banks[bk][:, (cc % 2) * 256:(cc % 2 + 1) * 256], wt[:, kk, cc * 128:(cc + 1) * 128],
                                  xt[:, kg * 4 + kk, :], vfirst, vlast, [b_wt, b_xt], b_bank[bk], vfirst)
            ht, b_ht = HT[ei % 2], b_HT[ei % 2]
            for hb_ in range(2):
                S.op("act", lambda h, hb_=hb_: h.activation(out=S1[:, hb_ * 512:(hb_ + 1) * 512], in_=banks[hb_][:], func=AF.Silu),
                     reads=[b_bank[hb_]], writes=[b_S1] if hb_ == 0 else (), pwrites=() if hb_ == 0 else [b_S1])
            for hb_ in range(2):
                S.op("dve", lambda h, hb_=hb_, ht=ht: h.tensor_tensor(
                    out=ht[:, hb_ * 2:(hb_ + 1) * 2, :], in0=banks[2 + hb_][:].rearrange("p (c t) -> p c t", c=2),
                    in1=S1[:, hb_ * 512:(hb_ + 1) * 512].rearrange("p (c t) -> p c t", c=2), op=ALU.mult),
                    reads=[b_bank[2 + hb_], b_S1], writes=[b_ht] if hb_ == 0 else (), pwrites=() if hb_ == 0 else [b_ht])
            for cg in range(4):
                wt, b_wt = w_get()
                for sb_ in range(2):
                    i4 = sis[sb_]
                    bk = 4 + (2 * cg + sb_) % 4
                    for kk in range(4):
                        pe_mm(banks[bk][:], ht[:, kk, sb_ * 128:(sb_ + 1) * 128], wt[:, kk, :], kk == 0, kk == 3,
                              [b_wt, b_ht], b_bank[bk], kk == 0)
                    wcol = si[i4][:, 1:2].bitcast(F32)
                    ye = yev.next()
                    wr_ = dict(writes=[b_Y[i4]] if cg == 0 else (), pwrites=() if cg == 0 else [b_Y[i4]])
                    if ye == "act":
                        S.op("act", lambda h, i4=i4, cg=cg, bk=bk, wcol=wcol: h.activation(
                            out=Y[i4][:, cg * 512:(cg + 1) * 512], in_=banks[bk][:], func=AF.Copy, scale=wcol),
                            reads=[b_bank[bk], b_si[i4]], **wr_)
                    else:
                        S.op("dve", lambda h, i4=i4, cg=cg, bk=bk, wcol=wcol: h.tensor_scalar(
                            out=Y[i4][:, cg * 512:(cg + 1) * 512], in0=banks[bk][:], scalar1=wcol, scalar2=None, op0=ALU.mult),
                            reads=[b_bank[bk], b_si[i4]], **wr_)
            for sb_ in range(2):
                i4 = sis[sb_]
                r0 = e * CAP + sb_ * 128
                S.op("pool", lambda h, i4=i4: h.indirect_dma_start(
                    out=YS, out_offset=bass.IndirectOffsetOnAxis(ap=si[i4][:, 2:3], axis=0), in_=Y[i4][:], in_offset=None),
                    reads=[b_Y[i4], b_si[i4]], pwrites=[b_YS], dma=True)

    def phase5():
        gfb = sb("gfb", [128, D], F32); b_gfb = Buf("gfb")
        S.op("sp", lambda h: h.dma_start(out=gfb[:], in_=gf[0, :].partition_broadcast(128)), writes=[b_gfb], dma=True)
        NB5 = 4
        hb = [sb(f"hb{i}", [128, D], F32) for i in range(NB5)]; b_hb = [Buf(f"hb{i}") for i in range(NB5)]
        yg = [sb(f"yg{i}", [128, D], BF16) for i in range(2 * NB5)]; b_yg = [Buf(f"yg{i}") for i in range(2 * NB5)]
        ob = [sb(f"ob{i}", [128, D], F32) for i in range(NB5)]; b_ob = [Buf(f"ob{i}") for i in range(NB5)]
        junk = sb("junk5", [128, D], BF16)
        st = [sb(f"st5{i}", [128, 4], F32) for i in range(NB5)]
        b_ssq = [Buf(f"ssq5{i}") for i in range(NB5)]; b_ms = [Buf(f"ms5{i}") for i in range(NB5)]; b_rstd = [Buf(f"rstd5{i}") for i in range(NB5)]
        nb = sum(1 for _ in cfg["tiles"]) * 4
        blocks = []
        for o in cfg["tiles"]:
            for tb in range(4):
                blocks.append(own_row(o) + tb * 128)
        def stage_a(bi):
            r0 = blocks[bi]
            i = bi % NB5
            S.op("sp", lambda h: h.dma_start(out=hb[i][:], in_=HS[r0:r0 + 128, :]), reads=[b_HS], writes=[b_hb[i]], dma=True)
            for kx in range(2):
                j = i * 2 + kx
                S.op("sp", lambda h, j=j, kx=kx: h.dma_start(out=yg[j][:], in_=YS[kx * YT_STRIDE + r0:kx * YT_STRIDE + r0 + 128, :]),
                     reads=[b_YS], writes=[b_yg[j]], dma=True)
            S.op("dve", lambda h: h.tensor_tensor(out=hb[i][:], in0=hb[i][:], in1=yg[i * 2][:], op=ALU.add),
                 reads=[b_hb[i], b_yg[i * 2]], writes=[b_hb[i]])
            S.op("dve", lambda h: h.tensor_tensor(out=hb[i][:], in0=hb[i][:], in1=yg[i * 2 + 1][:], op=ALU.add),
                 reads=[b_hb[i], b_yg[i * 2 + 1]], writes=[b_hb[i]])
            S.op("act", lambda h: h.activation(out=junk[:], in_=hb[i][:], func=AF.Square, accum_out=st[i][:, 0:1]),
                 reads=[b_hb[i]], writes=[b_ssq[i]])

        def stage_b(bi):
            r0 = blocks[bi]
            i = bi % NB5
            S.op("dve", lambda h: h.tensor_scalar(out=st[i][:, 1:2], in0=st[i][:, 0:1], scalar1=1.0 / D, scalar2=EPS,
                                                  op0=ALU.mult, op1=ALU.add), reads=[b_ssq[i]], writes=[b_ms[i]])
            S.op("pool", lambda h: h.tensor_tensor(out=st[i][:, 2:3], in0=st[i][:, 1:2], in1=neghalf[:], op=ALU.pow),
                 reads=[b_ms[i], b_const], writes=[b_rstd[i]])
            S.op("dve", lambda h: h.scalar_tensor_tensor(out=ob[i][:], in0=hb[i][:], scalar=st[i][:, 2:3], in1=gfb[:],
                                                         op0=ALU.mult, op1=ALU.mult),
                 reads=[b_hb[i], b_rstd[i], b_gfb], writes=[b_ob[i]])
            S.op("pool", lambda h: h.dma_start(out=yout[r0:r0 + 128, :], in_=ob[i][:]), reads=[b_ob[i]], pwrites=[b_yout], dma=True)

        LAG = 3
        for bi in range(min(LAG, len(blocks))):
            stage_a(bi)
        for bi in range(len(blocks)):
            if bi + LAG < len(blocks):
                stage_a(bi + LAG)
            stage_b(bi)

    if 1 in cfg["phases"]:
        run_phase(phase1, 8)
    if 2 in cfg["phases"]:
        run_phase(phase2)
    if 3 in cfg["phases"]:
        run_phase(phase3)
    if 4 in cfg["phases"]:
        run_phase(phase4, 16)
    if 5 in cfg["phases"]:
        run_phase(phase5)
    S.barrier()
    emit_program(nc, S, es)
    es.close()
    return nc


def _bf16(a):
    return np.ascontiguousarray(a.astype(ml_dtypes.bfloat16))


def position_tables():
    k = np.arange(128)[:, None].astype(np.float64)
    sl_a = np.power(2.0, -8.0 * (np.arange(16) + 1) / 16)
    sl_b = np.power(2.0, -8.0 * (np.arange(12) + 1) / 12)
    eA = np.zeros((128, 4, 3, 4, 128))
    q = np.arange(128)[None, :].astype(np.float64)
    for kvh in range(4):
        for s in range(3):
            rel = (128 * s - 128 + k) - q
            for g in range(4):
                e = np.exp(-sl_a[kvh * 4 + g] * np.abs(rel))
                eA[:, kvh, s, g, :] = np.where(np.abs(rel) <= 128, e, 0.0)
    eB01 = np.zeros((128, 2, 4, 2, 128))
    for gi, r in ((0, 1), (1, 4)):
        for hh in range(4):
            for s in range(2):
                rel = (k + 128 * s - 64) - q
                e = np.exp(-sl_b[gi * 4 + hh] * r * np.abs(rel))
                eB01[:, gi, hh, s, :] = np.where(np.abs(rel) <= 64, e, 0.0)
    eB2 = np.zeros((128, 4, 2, 32))
    q32 = np.arange(32)[None, :].astype(np.float64)
    for hh in range(4):
        for s in range(2):
            rel = (k + 128 * s - 64) - q32
            e = np.exp(-sl_b[8 + hh] * 16 * np.abs(rel))
            e = np.where(np.abs(rel) <= 64, e, 0.0)
            if s == 1:
                e = np.where(k < 32, e, 0.0)
            eB2[:, hh, s, :] = e
    eB = np.concatenate([eB01.reshape(128, -1), eB2.reshape(128, -1)], axis=1)
    return _bf16(eA.reshape(128, -1)), _bf16(eB)


def valid_mask_table(valid_ext, tiles):
    p = np.arange(128)
    out = np.zeros((len(tiles), 128, 51), np.float32)
    for ti, o in enumerate(tiles):
        cols = []
        for j in range(6):
            cols.append(valid_ext[o - 128 + 128 * j + p])
        for j in range(5):
            cols.append(valid_ext[o - 64 + 128 * j + p])
        for ph in range(4):
            for s in range(2):
                cols.append(valid_ext[o - 256 + ph + 512 * s + 4 * p])
        for ph in range(16):
            for s in range(2):
                tok = o - 1024 + ph + 2048 * s + 16 * p
                if s == 0:
                    cols.append(valid_ext[tok])
                else:
                    cols.append(np.where(p < 32, valid_ext[np.minimum(tok, len(valid_ext) - 1)], 0.0))
        out[ti] = np.stack(cols, axis=1)
    full = np.repeat(out[:, :, :, None], 128, axis=3)
    return _bf16(full.reshape(len(tiles) * 128, 51 * 128))


def make_shared_inputs(inp):
    f = lambda a: np.ascontiguousarray(np.asarray(a, dtype=np.float32))
    eA, eB = position_tables()
    w_rg = f(inp["w_router_group"])[0]
    w_re = f(inp["w_router_expert"])[0]
    wrm = np.concatenate([w_rg, w_re.transpose(1, 0, 2).reshape(D, 64)], axis=1)
    wrm = wrm.reshape(KC, 128, 72).transpose(1, 0, 2).reshape(128, KC * 72)
    brm = np.concatenate([f(inp["b_router_group"])[0], f(inp["b_router_expert"])[0].reshape(64)])[None, :]
    return dict(
        w_in=f(inp["w_in"])[0], w_gate=f(inp["w_gate"])[0], w_pa=f(inp["w_proj_a"])[0], w_pb=f(inp["w_proj_b"])[0],
        w_out=f(inp["w_out"])[0],
        w1=f(inp["w_expert_gate"])[0].reshape(N_EXP * D, D_FF), w3=f(inp["w_expert_up"])[0].reshape(N_EXP * D, D_FF),
        w2=f(inp["w_expert_down"])[0].reshape(N_EXP * D_FF, D),
        g1=f(inp["norm1"]).reshape(1, D), g2=f(inp["norm2"]).reshape(1, D), gf=f(inp["norm_final"]).reshape(1, D),
        bgT=np.ascontiguousarray(f(inp["b_gate"])[0].reshape(32, 128).T), wr=np.ascontiguousarray(wrm), br=f(brm),
        sink=f(inp["attn_sink"]).reshape(1, 16), eA=eA, eB=eB,
    )


def make_core_inputs(inp, c, tiles):
    xp = np.asarray(inp["x_prompt"], dtype=np.float32)[0]
    xs = np.asarray(inp["x_sample"], dtype=np.float32)[c // 2]
    xin = np.zeros((EXT, D), np.float32)
    valid = np.zeros((EXT,), np.float32)
    p0 = 1024 * c - HALO
    lo, hi = max(p0, 0), min(p0 + P_EXT, xp.shape[0])
    xin[lo - p0:hi - p0] = xp[lo:hi]
    valid[lo - p0:hi - p0] = 1.0
    seq = xs if c % 2 == 0 else xs[::-1]
    n = S_OWN + HALO
    xin[P_EXT + HALO:P_EXT + HALO + n] = seq[0:n]
    valid[P_EXT + HALO:P_EXT + HALO + n] = 1.0
    return dict(xin=xin, vmask=valid_mask_table(valid, tiles))


_NC_CACHE = {}


def kernel(**inputs):
    cfg = default_cfg()
    if "nc" not in _NC_CACHE:
        _NC_CACHE["nc"] = build(cfg)
    nc = _NC_CACHE["nc"]
    shared = make_shared_inputs(inputs)
    in_maps = []
    for c in range(NCORES):
        m = dict(shared)
        m.update(make_core_inputs(inputs, c, cfg["tiles"]))
        in_maps.append(m)
    res = run_bass_kernel_spmd(nc, in_maps, core_ids=list(range(NCORES)))
    yp = np.zeros((1, 8192, D), np.float32)
    ys = np.zeros((4, 4096, D), np.float32)
    for c in range(NCORES):
        y = np.asarray(res.results[c]["yout"])
        yp[0, 1024 * c:1024 * c + 1024] = y[:P_OWN]
        ys[c // 2, 2048 * (c % 2):2048 * (c % 2) + 2048] = y[P_OWN:] if c % 2 == 0 else y[P_OWN:][::-1]
    return (yp, ys)
```

```python
import math
import numpy as np
import ml_dtypes
import concourse.bass as bass
import concourse.mybir as mybir
from concourse.bass_utils import run_bass_kernel_spmd

F32 = mybir.dt.float32
BF16 = mybir.dt.bfloat16
I32 = mybir.dt.int32
AF = mybir.ActivationFunctionType
ALU = mybir.AluOpType
AX = mybir.AxisListType

D = 2048
KC = D // 128
HEAD = 128
A_HEADS, A_KV, A_GROUP = 16, 4, 4
B_HPG = 4
DIL = ((128, 1), (512, 4), (2048, 16))
IN_COLS = 7680
N_EXP = 64
D_FF = 512
EPS = 1e-6
SCALE = HEAD ** -0.5
NCORES = 8
T = 512
HALO = 1024
P_OWN, S_OWN = 1024, 2048
P_EXT, S_EXT = P_OWN + 2 * HALO, S_OWN + 2 * HALO
EXT = P_EXT + S_EXT
OWN = P_OWN + S_OWN
CAP = 256
NSLOT = N_EXP * CAP
YT_STRIDE = OWN + 128
BIG_IDX = 1 << 24
SEM_ROLL = 20000


class Buf:
    __slots__ = ("name", "writers", "readers")

    def __init__(self, name):
        self.name = name
        self.writers = []
        self.readers = []


class Op:
    __slots__ = ("eng", "fn", "deps", "signal", "event", "is_dma", "idx")

    def __init__(self, eng, fn, is_dma):
        self.eng = eng
        self.fn = fn
        self.deps = []
        self.signal = False
        self.event = None
        self.is_dma = is_dma
        self.idx = -1


ENGS = ("pe", "act", "dve", "pool", "sp")


class Sched:
    def __init__(self):
        self.streams = {e: [] for e in ENGS}
        self.nops = 0

    def op(self, eng, fn, reads=(), writes=(), pwrites=(), dma=False):
        o = Op(eng, fn, dma)
        o.idx = self.nops
        self.nops += 1
        deps = []
        for b in reads:
            deps.extend(b.writers)
        for b in writes:
            deps.extend(b.writers)
            deps.extend(b.readers)
        for b in pwrites:
            deps.extend(b.readers)
        for b in reads:
            b.readers.append(o)
        for b in writes:
            b.writers = [o]
            b.readers = []
        for b in pwrites:
            if b.readers:
                b.writers = [o]
                b.readers = []
            else:
                b.writers.append(o)
        latest = {}
        out = []
        seen = set()
        for d in deps:
            if d is o or id(d) in seen:
                continue
            seen.add(id(d))
            if d.is_dma:
                out.append(d)
            else:
                if d.eng == eng and eng == "pe":
                    continue
                cur = latest.get(d.eng)
                if cur is None or d.idx > cur.idx:
                    latest[d.eng] = d
        out.extend(latest.values())
        o.deps = out
        self.streams[eng].append(o)
        return o

    def barrier(self):
        deps = []
        for e in ENGS:
            st = self.streams[e]
            if not st:
                continue
            last_c = None
            for o in reversed(st):
                if o.fn is None:
                    break
                if o.is_dma:
                    deps.append(o)
                elif last_c is None:
                    last_c = o
            if last_c is not None:
                deps.append(last_c)
        for e in ENGS:
            o = Op(e, None, False)
            o.idx = self.nops
            self.nops += 1
            o.deps = [d for d in deps]
            self.streams[e].append(o)


def emit_program(nc, sched, extra_ctx):
    for e in ENGS:
        for o in sched.streams[e]:
            for d in o.deps:
                d.signal = True
            if o.is_dma:
                o.signal = True
    n_dma_sems = {"sp": 16, "act": 4, "pool": 12, "dve": 2, "pe": 2}
    n_eng_sems = {}
    for e in ENGS:
        cnt = sum(1 for o in sched.streams[e] if o.signal and not o.is_dma)
        n_eng_sems[e] = max(1, (cnt + SEM_ROLL - 1) // SEM_ROLL)
    from contextlib import ExitStack
    with ExitStack() as st:
        eng_sems = {e: [st.enter_context(nc.semaphore(f"s_{e}_{i}")) for i in range(n_eng_sems[e])] for e in ENGS}
        dma_sems = {e: [st.enter_context(nc.semaphore(f"d_{e}_{i}")) for i in range(n_dma_sems[e])] for e in ENGS}
        for e in ENGS:
            cnt = 0
            dcnt = 0
            dma_last = [None] * n_dma_sems[e]
            dma_uses = [0] * n_dma_sems[e]
            for o in sched.streams[e]:
                if o.is_dma:
                    k = dcnt % n_dma_sems[e]
                    dcnt += 1
                    if dma_last[k] is not None:
                        o.deps.append(dma_last[k])
                    dma_uses[k] += 1
                    o.event = (dma_sems[e][k], 16 * dma_uses[k], 16)
                    dma_last[k] = o
                elif o.signal:
                    si = cnt // SEM_ROLL
                    o.event = (eng_sems[e][si], cnt % SEM_ROLL + 1, 1)
                    cnt += 1
        block = st.enter_context(nc.Block())

        def make_section(e):
            ops = sched.streams[e]

            def section(h):
                waited = {}
                for o in ops:
                    need = {}
                    for d in o.deps:
                        sem, val, _ = d.event
                        key = id(sem)
                        if key not in need or need[key][1] < val:
                            need[key] = (sem, val)
                    for key, (sem, val) in need.items():
                        if waited.get(key, 0) >= val:
                            continue
                        h.wait_ge(sem, val)
                        waited[key] = val
                    if o.fn is None:
                        continue
                    ins = o.fn(h)
                    if o.signal:
                        sem, val, inc = o.event
                        ins.then_inc(sem, inc)
            return section

        block.tensor(make_section("pe"))
        block.scalar(make_section("act"))
        block.vector(make_section("dve"))
        block.gpsimd(make_section("pool"))
        block.sync(make_section("sp"))


KV_GROUPS = (4, 5, 9, 12, 10, 13, 11, 14)
KVS_COLS = 4096


def default_cfg():
    chunks = []
    for seg0, own0, own1, ext in ((0, HALO, HALO + P_OWN, P_EXT), (P_EXT, HALO, HALO + S_OWN, S_EXT)):
        for c in range(0, ext, T):
            if seg0 > 0 and c < HALO:
                continue
            near = (c + T > own0 - 256) and (c < own1 + 256)
            chunks.append((seg0 + c, list(range(8)) if near else [6, 7], own0 <= c < own1))
    tiles = [HALO, HALO + T] + [P_EXT + HALO + i * T for i in range(4)]
    return dict(chunks=chunks, tiles=tiles, phases=(1, 2, 3, 4, 5), experts=list(range(N_EXP)), debug=False)


def own_row(o):
    return o - HALO if o < P_EXT else P_OWN + (o - P_EXT - HALO)


def build(cfg):
    from contextlib import ExitStack
    nc = bass.Bass("TRN2", target_bir_lowering=False)
    S = Sched()
    es = ExitStack()
    dbg = cfg["debug"]

    def din(name, shape, dt=F32):
        return nc.dram_tensor(name, list(shape), dt, kind="ExternalInput").ap()

    def dscratch(name, shape, dt):
        kind = "ExternalOutput" if dbg else "Internal"
        return nc.dram_tensor(name, list(shape), dt, kind=kind).ap()

    cur = [es]

    def sb(name, shape, dt):
        return cur[0].enter_context(nc.sbuf_tensor(name, list(shape), dt))

    class Rot:
        def __init__(self, items):
            self.items = list(items)
            self.i = 0

        def next(self):
            it = self.items[self.i % len(self.items)]
            self.i += 1
            return it

    def run_phase(fn, nbf=6):
        with ExitStack() as pes:
            cur[0] = pes
            ring["n"] = nbf
            ring["bf"] = [sb(f"wbf_{fn.__name__}_{i}", [128, 4, 512], BF16) for i in range(nbf)]
            ring["b"] = [Buf(f"wbf{i}") for i in range(nbf)]
            wcount[0] = 0
            fn()
            assert not wq and not wissued
            S.barrier()
        cur[0] = es

    def ps(name, shape, dt):
        return es.enter_context(nc.psum_tensor(name, list(shape), dt))

    xin = din("xin", [EXT, D])
    w_in = din("w_in", [D, IN_COLS])
    w_gate = din("w_gate", [D, 2 * D])
    w_pa = din("w_pa", [D, D])
    w_pb = din("w_pb", [512, D])
    w_out = din("w_out", [D, D])
    w1 = din("w1", [N_EXP * D, D_FF])
    w3 = din("w3", [N_EXP * D, D_FF])
    w2 = din("w2", [N_EXP * D_FF, D])
    g1 = din("g1", [1, D])
    g2 = din("g2", [1, D])
    gf = din("gf", [1, D])
    bgT = din("bgT", [128, 32])
    wr = din("wr", [128, KC * 72])
    br = din("br", [1, 72])
    sink = din("sink", [1, 16])
    eA = din("eA", [128, 4 * 3 * 512], BF16)
    eB = din("eB", [128, 2 * 4 * 2 * 128 + 4 * 2 * 32], BF16)
    vmask = din("vmask", [len(cfg["tiles"]) * 128, 51 * 128], BF16)
    yout = nc.dram_tensor("yout", [OWN, D], F32, kind="ExternalOutput").ap()

    XNT = dscratch("XNT", [KC * 128, EXT], BF16)
    KVS = dscratch("KVS", [EXT, KVS_COLS], BF16)
    OTS = dscratch("OTS", [20 * 128, OWN], BF16)
    HS = dscratch("HS", [OWN, D], F32)
    HN = dscratch("HN", [OWN + 128, D], BF16)
    SLOTI = dscratch("SLOTI", [NSLOT, 4], I32)
    YS = dscratch("YS", [2 * YT_STRIDE, D], BF16)
    b_XNT, b_KVS, b_OTS, b_HS, b_HN, b_SLOTI, b_YS, b_yout = (Buf(n) for n in "XNT KVS OTS HS HN SLOTI YS yout".split())

    ident = sb("ident", [128, 128], BF16)
    identf = sb("identf", [128, 128], F32)
    ones_bf = sb("ones_bf", [128, 128], BF16)
    neghalf = sb("neghalf", [128, 1], F32)
    b_const = Buf("const")

    S.op("pool", lambda h: h.memset(identf[:], 0.0), writes=[b_const])
    S.op("pool", lambda h: h.memset(neghalf[:], -0.5), pwrites=[b_const])
    S.op("pool", lambda h: h.memset(ones_bf[:], 1.0), pwrites=[b_const])
    b_identf = Buf("identf")
    S.op("pool", lambda h: h.affine_select(out=identf[:], in_=identf[:], pattern=[[1, 128]], compare_op=ALU.not_equal,
                                           fill=1.0, base=0, channel_multiplier=-1), reads=[b_const], writes=[b_identf])
    S.op("pool", lambda h: h.tensor_copy(out=ident[:], in_=identf[:]), reads=[b_identf], writes=[b_const])

    banks = [ps(f"bank{i}", [128, 512], F32) for i in range(8)]
    b_bank = [Buf(f"bank{i}") for i in range(8)]

    ring = {"bf": [], "b": [], "n": 0}
    wq = []
    wissued = []
    wcount = [0]

    def w_issue():
        src = wq.pop(0)
        i = wcount[0]
        wcount[0] += 1
        b = i % ring["n"]
        t_, b_ = ring["bf"][b], ring["b"][b]
        S.op("pool", lambda h: h.dma_start(out=t_[:], in_=src.rearrange("(kk p) c -> p kk c", p=128)),
             writes=[b_], dma=True)
        wissued.append((t_, b_))

    def w_push(srcs):
        wq.extend(srcs)

    def w_get():
        depth = ring["n"] - 2
        while wq and len(wissued) < depth + 1:
            w_issue()
        return wissued.pop(0)

    def piece(w, rg, cg):
        return w[rg * 512:(rg + 1) * 512, cg * 512:(cg + 1) * 512]

    def dma_split(eng, out_ap, in_ap, nsplit, **kw):
        n1 = out_ap.shape[1]
        step = n1 // nsplit
        first_w = kw.pop("writes", ())
        pw = kw.pop("pwrites", ())
        for i in range(nsplit):
            o_, i_ = out_ap[:, i * step:(i + 1) * step], in_ap[:, i * step:(i + 1) * step]
            if i == 0:
                S.op(eng, lambda h, o_=o_, i_=i_: h.dma_start(out=o_, in_=i_), writes=first_w, pwrites=pw, dma=True, **kw)
            else:
                S.op(eng, lambda h, o_=o_, i_=i_: h.dma_start(out=o_, in_=i_), pwrites=list(first_w) + list(pw), dma=True, **kw)

    evac_rr = [0]

    def evac_copy(out_ap, in_ap, reads, writes=(), pwrites=(), eng=None):
        e = eng or ("act", "dve")[evac_rr[0] % 2]
        if eng is None:
            evac_rr[0] += 1
        if e == "act":
            S.op("act", lambda h: h.copy(out=out_ap, in_=in_ap), reads=reads, writes=writes, pwrites=pwrites)
        else:
            S.op("dve", lambda h: h.tensor_copy(out=out_ap, in_=in_ap), reads=reads, writes=writes, pwrites=pwrites)

    def phase1():
        g1b = sb("g1b", [128, D], F32)
        b_g1b = Buf("g1b")
        S.op("sp", lambda h: h.dma_start(out=g1b[:], in_=g1[0, :].partition_broadcast(128)), writes=[b_g1b], dma=True)
        xst = [sb(f"xst{i}", [128, D], F32) for i in range(4)]
        b_xst = [Buf(f"xst{i}") for i in range(4)]
        junk = sb("junk1", [128, D], BF16)
        xnb = [sb(f"xnb{i}", [128, D], BF16) for i in range(8)]
        b_xnb = [Buf(f"xnb{i}") for i in range(8)]
        stat = [sb(f"stat{i}", [128, 4], F32) for i in range(4)]
        b_stat = [Buf(f"stat{i}") for i in range(4)]
        b_ms = [Buf(f"ms{i}") for i in range(4)]
        b_rstd = [Buf(f"rstd{i}") for i in range(4)]
        xnT = [sb(f"xnT{i}", [128, KC, T], BF16) for i in range(2)]
        b_xnT = [Buf(f"xnT{i}") for i in range(2)]
        kvo = [sb(f"kvo{i}", [128, 512], BF16) for i in range(4)]
        b_kvo = [Buf(f"kvo{i}") for i in range(4)]
        nkvo = 0
        w_push([piece(w_in, kg, KV_GROUPS[cg]) for (_c0, cgs_, _o) in cfg["chunks"] for cg in cgs_ for kg in range(4)])

        def norm_elem(ci):
            c0 = cfg["chunks"][ci][0]
            for tb in range(4):
                i = (ci * 4 + tb) % 4
                xi = (ci % 2) * 4 + tb
                r0 = c0 + tb * 128
                S.op("sp", lambda h, i=i, r0=r0: h.dma_start(out=xst[i][:], in_=xin[r0:r0 + 128, :]),
                     writes=[b_xst[i]], dma=True)
                S.op("act", lambda h, i=i: h.activation(out=junk[:], in_=xst[i][:], func=AF.Square,
                                                        accum_out=stat[i][:, 0:1]),
                     reads=[b_xst[i]], writes=[b_stat[i]])
                S.op("dve", lambda h, i=i: h.tensor_scalar(out=stat[i][:, 1:2], in0=stat[i][:, 0:1], scalar1=1.0 / D,
                                                          scalar2=EPS, op0=ALU.mult, op1=ALU.add),
                     reads=[b_stat[i]], writes=[b_ms[i]])
                S.op("pool", lambda h, i=i: h.tensor_tensor(out=stat[i][:, 2:3], in0=stat[i][:, 1:2], in1=neghalf[:], op=ALU.pow),
                     reads=[b_const, b_ms[i]], writes=[b_rstd[i]])
                S.op("dve", lambda h, i=i, xi=xi: h.scalar_tensor_tensor(out=xnb[xi][:], in0=xst[i][:], scalar=stat[i][:, 2:3],
                                                                        in1=g1b[:], op0=ALU.mult, op1=ALU.mult),
                     reads=[b_xst[i], b_rstd[i], b_g1b], writes=[b_xnb[xi]])

        if cfg.get("zero_left", True):
            zkv = sb("zkv", [128, KVS_COLS], BF16); b_zkv = Buf("zkv")
            S.op("pool", lambda h: h.memset(zkv[:], 0.0), writes=[b_zkv])
            for zb in range(HALO // 128):
                S.op("sp", lambda h, zb=zb: h.dma_start(out=KVS[P_EXT + zb * 128:P_EXT + (zb + 1) * 128, :], in_=zkv[:]),
                     reads=[b_zkv], pwrites=[b_KVS], dma=True)
        norm_elem(0)
        for ci, (c0, cgs, is_own) in enumerate(cfg["chunks"]):
            xt, b_xt = xnT[ci % 2], b_xnT[ci % 2]
            for tb in range(4):
                xi = (ci % 2) * 4 + tb
                for half in range(2):
                    bk = (2 * tb + half) % 8
                    tpv = banks[bk][:].bitcast(BF16)
                    for k8 in range(8):
                        k = half * 8 + k8
                        S.op("pe", lambda h, xi=xi, k=k, k8=k8, tpv=tpv: h.transpose(
                            out=tpv[:, k8 * 128:(k8 + 1) * 128], in_=xnb[xi][:, k * 128:(k + 1) * 128], identity=ident[:]),
                            reads=[b_xnb[xi], b_const], writes=[b_bank[bk]] if k8 == 0 else (), pwrites=() if k8 == 0 else [b_bank[bk]])
                    evac_copy(xt[:, half * 8:(half + 1) * 8, tb * 128:(tb + 1) * 128],
                              tpv.rearrange("p (k t) -> p k t", k=8), reads=[b_bank[bk]], pwrites=[b_xt])
            if ci + 1 < len(cfg["chunks"]):
                norm_elem(ci + 1)
            if is_own:
                dma_split("sp", XNT.rearrange("(k p) t -> p k t", p=128)[:, :, c0:c0 + T], xt[:], 4,
                          reads=[b_xt], pwrites=[b_XNT])
            for gi_, cg in enumerate(cgs):
                bset = (gi_ % 2) * 4
                for kg in range(4):
                    wt, b_wt = w_get()
                    for tb in range(4):
                        for kk in range(4):
                            first = (kg == 0 and kk == 0)
                            last = (kg == 3 and kk == 3)
                            S.op("pe", lambda h, tb=tb, kk=kk, kg=kg, wt=wt, xt=xt, first=first, last=last, bset=bset:
                                 h.matmul(banks[bset + tb][:], xt[:, kg * 4 + kk, tb * 128:(tb + 1) * 128], wt[:, kk, :],
                                          start=first, stop=last),
                                 reads=[b_xt, b_wt], writes=[b_bank[bset + tb]] if first else (),
                                 pwrites=() if first else [b_bank[bset + tb]])
                for tb in range(4):
                    j = nkvo % 4
                    nkvo += 1
                    evac_copy(kvo[j][:], banks[bset + tb][:], reads=[b_bank[bset + tb]], writes=[b_kvo[j]])
                    r0 = c0 + tb * 128
                    S.op("sp", lambda h, j=j, r0=r0, cg=cg: h.dma_start(out=KVS[r0:r0 + 128, cg * 512:(cg + 1) * 512], in_=kvo[j][:]),
                         reads=[b_kvo[j]], pwrites=[b_KVS], dma=True)


    def pe_mm(out_ap, lhsT, rhs, start, stop, reads, bbuf, first):
        S.op("pe", lambda h: h.matmul(out_ap, lhsT, rhs, start=start, stop=stop),
             reads=reads, writes=[bbuf] if first else (), pwrites=() if first else [bbuf])

    def phase2():
        eA_sb = sb("eA_sb", [128, 4 * 3 * 512], BF16)
        eB_sb = sb("eB_sb", [128, 2304], BF16)
        b_tab = Buf("tab")
        S.op("sp", lambda h: h.dma_start(out=eA_sb[:], in_=eA), writes=[b_tab], dma=True)
        S.op("sp", lambda h: h.dma_start(out=eB_sb[:], in_=eB), pwrites=[b_tab], dma=True)
        sink_sb = sb("sink_sb", [1, 16], F32)
        sexp = sb("sexp", [1, 16], F32)
        sinkrow = sb("sinkrow", [1, 16 * 128], BF16)
        b_sink, b_sexp, b_sinkrow = Buf("sink"), Buf("sexp"), Buf("sinkrow")
        S.op("sp", lambda h: h.dma_start(out=sink_sb[:], in_=sink), writes=[b_sink], dma=True)
        S.op("act", lambda h: h.activation(out=sexp[:], in_=sink_sb[:], func=AF.Exp), reads=[b_sink], writes=[b_sexp])
        S.op("dve", lambda h: h.tensor_copy(out=sinkrow[:].rearrange("p (h q) -> p h q", q=128),
                                            in_=sexp[:].unsqueeze(2).broadcast_to([1, 16, 128])),
             reads=[b_sexp], writes=[b_sinkrow])
        xT = sb("xT2", [128, KC, T], BF16); b_xT = Buf("xT2")
        vm2 = [sb(f"vm{i}", [128, 51, 128], BF16) for i in range(2)]; b_vm2 = [Buf(f"vm{i}") for i in range(2)]
        OT = sb("OT", [128, 20, T], BF16); b_OT = Buf("OT")
        QA = [sb(f"QA{i}", [128, 4, T], BF16) for i in range(2)]; b_QA = [Buf(f"QA{i}") for i in range(2)]
        QB = sb("QB", [128, 12, T], BF16); b_QB = Buf("QB")
        KnA = [sb(f"KnA{i}", [128, 6, 128], BF16) for i in range(2)]; b_KnA = [Buf(f"KnA{i}") for i in range(2)]
        VA = [sb(f"VA{i}", [128, 6, 128], BF16) for i in range(2)]; b_VA = [Buf(f"VA{i}") for i in range(2)]
        KTA = [sb(f"KTA{i}", [128, 768], BF16) for i in range(2)]; b_KTA = [Buf(f"KTA{i}") for i in range(2)]
        Kn1 = sb("Kn1", [128, 5, 128], BF16)
        Kn2 = sb("Kn2", [128, 4, 2, 128], BF16)
        Kn3a = sb("Kn3a", [128, 16, 128], BF16)
        Kn3b = sb("Kn3b", [32, 16, 128], BF16)
        V1 = [sb(f"V1_{i}", [128, 5, 128], BF16) for i in range(2)]
        V2 = [sb(f"V2_{i}", [128, 4, 2, 128], BF16) for i in range(2)]
        V3a = [sb(f"V3a_{i}", [128, 16, 128], BF16) for i in range(2)]
        V3b = [sb(f"V3b_{i}", [32, 16, 128], BF16) for i in range(2)]
        b_KnB = Buf("KnB")
        b_VB = [Buf(f"VB{i}") for i in range(2)]
        KT1 = [sb(f"KT1_{i}", [128, 5 * 128], BF16) for i in range(2)]
        KT2 = [sb(f"KT2_{i}", [128, 8 * 128], BF16) for i in range(2)]
        KT3 = [sb(f"KT3_{i}", [128, 16, 160], BF16) for i in range(2)]
        b_KTB = [Buf(f"KTB{i}") for i in range(2)]
        NP = 8
        Pb = [sb(f"P{i}", [128, 512], BF16) for i in range(NP)]
        b_P = [Buf(f"P{i}") for i in range(NP)]
        prot = Rot(range(NP))
        Rb = [sb(f"R{i}", [128, 512], F32) for i in range(2)]
        b_R = [Buf(f"R{i}") for i in range(2)]
        rrot = Rot(range(2))
        srot = Rot(range(4))
        orot = Rot((4, 5))
        lrot = Rot((6, 7))
        mulrot = Rot(("dve",))

        def load_rows(dst_t, src, bbuf, first, nsplit=1):
            four = len(dst_t.shape) == 4
            n1 = dst_t.shape[2] if four else dst_t.shape[1]
            step = n1 // nsplit
            for i in range(nsplit):
                if four:
                    d_, s_ = dst_t[:, :, i, :], src[:, :, i, :]
                else:
                    d_, s_ = dst_t[:, i * step:(i + 1) * step], src[:, i * step:(i + 1) * step]
                f_ = first and i == 0
                S.op("sp", lambda h, d_=d_, s_=s_: h.dma_start(out=d_, in_=s_), reads=[b_KVS],
                     writes=[bbuf] if f_ else (), pwrites=() if f_ else [bbuf], dma=True)

        def kv_src(kind, o, col):
            cs = slice(col, col + 128)
            if kind == "A":
                return KVS[o - 128:o - 128 + 768, cs].rearrange("(j p) d -> p j d", p=128)
            if kind == "1":
                return KVS[o - 64:o - 64 + 640, cs].rearrange("(j p) d -> p j d", p=128)
            if kind == "2":
                return KVS[o - 256:o - 256 + 1024, cs].rearrange("(s p ph) d -> p ph s d", s=2, p=128, ph=4)
            if kind == "3a":
                return KVS[o - 1024:o - 1024 + 2048, cs].rearrange("(p ph) d -> p ph d", ph=16)
            if kind == "3b":
                return KVS[o + 1024:o + 1024 + 512, cs].rearrange("(p ph) d -> p ph d", ph=16)

        def softmax_slot(sbk, nk, tab_ap):
            pi = prot.next()
            S.op("act", lambda h: h.activation(out=Pb[pi][0:nk, :], in_=banks[sbk][0:nk, :], func=AF.Exp, scale=SCALE),
                 reads=[b_bank[sbk]], writes=[b_P[pi]])
            me = mulrot.next()
            S.op(me, lambda h: h.tensor_tensor(out=Pb[pi][0:nk, :], in0=Pb[pi][0:nk, :], in1=tab_ap, op=ALU.mult),
                 reads=[b_tab, b_P[pi]], writes=[b_P[pi]])
            return pi

        def finish_unit(ob, lb, out_ap, split=None):
            ri = rrot.next()
            S.op("dve", lambda h: h.reciprocal(out=Rb[ri][:], in_=banks[lb][:]), reads=[b_bank[lb]], writes=[b_R[ri]])
            i0, i1 = banks[ob][:], Rb[ri][:]
            if split is not None:
                i0 = i0.rearrange("p (g q) -> p g q", g=split)
                i1 = i1.rearrange("p (g q) -> p g q", g=split)
            S.op("dve", lambda h: h.tensor_tensor(out=out_ap, in0=i0, in1=i1, op=ALU.mult),
                 reads=[b_bank[ob], b_R[ri]], pwrites=[b_OT])

        def qproj(cgs, dst, b_dst):
            for i, cg in enumerate(cgs):
                bks = [srot.next() for _ in range(4)]
                for kg in range(4):
                    wt, b_wt = w_get()
                    for cc in range(4):
                        for kk in range(4):
                            first = (kg == 0 and kk == 0)
                            pe_mm(banks[bks[cc]][:], wt[:, kk, cc * 128:(cc + 1) * 128], xT[:, kg * 4 + kk, :],
                                  first, (kg == 3 and kk == 3), [b_wt, b_xT], b_bank[bks[cc]], first)
                for cc in range(4):
                    evac_copy(dst[:, 4 * i + cc, :], banks[bks[cc]][:], reads=[b_bank[bks[cc]]], pwrites=[b_dst])

        for _ in cfg["tiles"]:
            w_push([piece(w_in, kg, kvh) for kvh in range(4) for kg in range(4)])
            w_push([piece(w_in, kg, 6 + gi) for gi in range(3) for kg in range(4)])
        class Step:
            def __init__(self):
                self.prep = None
                self.s1 = None
                self.s2 = None

        def tile_steps(ti, o, next_tile_prep):
            steps = []
            next_tile = next_tile_prep
            vm, b_vm = vm2[ti % 2], b_vm2[ti % 2]

            def tile_prep():
                dma_split("sp", xT[:], XNT.rearrange("(k p) t -> p k t", p=128)[:, :, o:o + T], 4, reads=[b_XNT], writes=[b_xT])
                S.op("sp", lambda h: h.dma_start(out=vm[:].rearrange("p s q -> p (s q)"), in_=vmask[ti * 128:(ti + 1) * 128, :]),
                     writes=[b_vm], dma=True)

            def a_prep(kvh):
                par = kvh % 2
                load_rows(KnA[par], kv_src("A", o, 0 * 512 + kvh * 128), b_KnA[par], True, 2)
                load_rows(VA[par], kv_src("A", o, 1 * 512 + kvh * 128), b_VA[par], True, 2)
                qproj([kvh], QA[par], b_QA[par])
                bk = srot.next()
                tpv = banks[bk][:].bitcast(BF16)
                for j in range(6):
                    S.op("pe", lambda h, j=j: h.transpose(out=tpv[:, j * 128:(j + 1) * 128], in_=KnA[par][:, j, :], identity=ident[:]),
                         reads=[b_KnA[par], b_const], writes=[b_bank[bk]] if j == 0 else (), pwrites=() if j == 0 else [b_bank[bk]])
                evac_copy(KTA[par][:], tpv[:, 0:768], reads=[b_bank[bk]], writes=[b_KTA[par]])

            def a_step(kvh, qb):
                par = kvh % 2
                st_ = Step()
                state = {}
                if qb == 0 and kvh == 0 and ti == 0:
                    st_.prep = lambda: (tile_prep(), a_prep(0))
                if qb == 1 and kvh < 3:
                    st_.prep = lambda: a_prep(kvh + 1)
                if qb == 1 and kvh == 3:
                    st_.prep = lambda: (qproj([6, 7, 8], QB, b_QB), b_prep(0))

                def s1():
                    pis = []
                    for s_ in range(3):
                        j = qb + s_
                        sbk = srot.next()
                        pe_mm(banks[sbk][:], KTA[par][:, j * 128:(j + 1) * 128], QA[par][:, :, qb * 128:(qb + 1) * 128],
                              True, True, [b_KTA[par], b_QA[par]], b_bank[sbk], True)
                        pis.append(softmax_slot(sbk, 128, eA_sb[:, (kvh * 3 + s_) * 512:(kvh * 3 + s_ + 1) * 512]))
                    state["pis"] = pis

                def s2():
                    pis = state["pis"]
                    ob, lb = orot.next(), lrot.next()
                    for s_ in range(3):
                        j = qb + s_
                        pe_mm(banks[ob][:], VA[par][:, j, :], Pb[pis[s_]][:], s_ == 0, s_ == 2, [b_VA[par], b_P[pis[s_]]], b_bank[ob], s_ == 0)
                    for s_ in range(3):
                        j = qb + s_
                        pe_mm(banks[lb][:], vm[:, j, :], Pb[pis[s_]][:], s_ == 0, False, [b_vm, b_P[pis[s_]]], b_bank[lb], s_ == 0)
                    pe_mm(banks[lb][:], ones_bf[0:1, :], sinkrow[0:1, kvh * 512:(kvh + 1) * 512], False, True,
                          [b_const, b_sinkrow], b_bank[lb], False)
                    finish_unit(ob, lb, OT[:, kvh * 4:(kvh + 1) * 4, qb * 128:(qb + 1) * 128], split=4)
                st_.s1, st_.s2 = s1, s2
                return st_

            for kvh in range(4):
                for qb in range(4):
                    steps.append(a_step(kvh, qb))
            tile_entry[ti] = lambda: (tile_prep(), a_prep(0))

            def b_prep(hh):
                par = hh % 2
                c = hh * 128
                load_rows(Kn1, kv_src("1", o, 2 * 512 + c), b_KnB, True)
                load_rows(Kn2, kv_src("2", o, 4 * 512 + c), b_KnB, False, 2)
                load_rows(Kn3a, kv_src("3a", o, 6 * 512 + c), b_KnB, False, 4)
                load_rows(Kn3b, kv_src("3b", o, 6 * 512 + c), b_KnB, False)
                load_rows(V1[par], kv_src("1", o, 3 * 512 + c), b_VB[par], True)
                load_rows(V2[par], kv_src("2", o, 5 * 512 + c), b_VB[par], False, 2)
                load_rows(V3a[par], kv_src("3a", o, 7 * 512 + c), b_VB[par], False, 4)
                load_rows(V3b[par], kv_src("3b", o, 7 * 512 + c), b_VB[par], False)
                groups = [
                    ([(Kn1[:, j, :], 128) for j in range(5)], 128, KT1[par][:, 0:640], None),
                    ([(Kn2[:, ph, s_, :], 128) for ph in range(4) for s_ in range(2)], 128, KT2[par][:, :], None),
                    ([(Kn3a[:, ph, :], 128) for ph in range(8)], 128, KT3[par][:, 0:8, 0:128], 8),
                    ([(Kn3a[:, ph, :], 128) for ph in range(8, 16)], 128, KT3[par][:, 8:16, 0:128], 8),
                    ([(Kn3b[:, ph, :], 32) for ph in range(16)], 32, KT3[par][:, :, 128:160], 16),
                ]
                for gidx, (jobs, w_, dst, nsplit) in enumerate(groups):
                    bk = srot.next()
                    tpv = banks[bk][:].bitcast(BF16)
                    for n_, (src, npart) in enumerate(jobs):
                        S.op("pe", lambda h, n_=n_, src=src, npart=npart, tpv=tpv, w_=w_: h.transpose(
                            out=tpv[:, n_ * w_:(n_ + 1) * w_], in_=src, identity=ident[0:npart, 0:npart]),
                            reads=[b_KnB, b_const], writes=[b_bank[bk]] if n_ == 0 else (), pwrites=() if n_ == 0 else [b_bank[bk]])
                    srcv = tpv[:, 0:len(jobs) * w_]
                    if nsplit is not None:
                        srcv = srcv.rearrange("p (n w) -> p n w", n=nsplit)
                    evac_copy(dst, srcv, reads=[b_bank[bk]], writes=[b_KTB[par]] if gidx == 0 else (),
                              pwrites=() if gidx == 0 else [b_KTB[par]])

            def b_step(hh, nslot, shared):
                par = hh % 2
                gi, s_ = nslot // 2, nslot % 2
                st_ = Step()
                state = {}
                if nslot == 3 and hh < 3:
                    st_.prep = lambda: b_prep(hh + 1)
                if nslot == 2 and hh == 3 and next_tile is not None:
                    st_.prep = next_tile
                nsub, nq = (4, 128) if gi < 2 else (16, 32)
                nk = 32 if (gi == 2 and s_ == 1) else 128
                qv = QB[:, gi * 4 + hh, :]

                def s1():
                    sbk = srot.next()
                    for sp in range(nsub):
                        if gi == 0:
                            lhsT = KT1[par][:, (sp + s_) * 128:(sp + s_ + 1) * 128]
                            rhs = qv[:, sp * 128:(sp + 1) * 128]
                        elif gi == 1:
                            lhsT = KT2[par][:, (sp * 2 + s_) * 128:(sp * 2 + s_ + 1) * 128]
                            rhs = qv.rearrange("p (u r) -> p r u", r=4)[:, sp, :]
                        else:
                            lhsT = KT3[par][:, sp, s_ * 128:s_ * 128 + nk]
                            rhs = qv.rearrange("p (u r) -> p r u", r=16)[:, sp, :]
                        pe_mm(banks[sbk][0:nk, sp * nq:(sp + 1) * nq], lhsT, rhs, sp == 0, sp == nsub - 1,
                              [b_KTB[par], b_QB], b_bank[sbk], sp == 0)
                    if gi < 2:
                        tab = eB_sb[:, ((gi * 4 + hh) * 2 + s_) * 128:((gi * 4 + hh) * 2 + s_ + 1) * 128]
                    else:
                        tab = eB_sb[0:nk, 2048 + (hh * 2 + s_) * 32:2048 + (hh * 2 + s_ + 1) * 32]
                    tab_b = tab.unsqueeze(1).broadcast_to([nk, nsub, nq])
                    pi = prot.next()
                    S.op("act", lambda h: h.activation(out=Pb[pi][0:nk, :], in_=banks[sbk][0:nk, :], func=AF.Exp, scale=SCALE),
                         reads=[b_bank[sbk]], writes=[b_P[pi]])
                    S.op("dve", lambda h: h.tensor_tensor(
                        out=Pb[pi][0:nk, :].rearrange("p (s q) -> p s q", s=nsub),
                        in0=Pb[pi][0:nk, :].rearrange("p (s q) -> p s q", s=nsub), in1=tab_b, op=ALU.mult),
                        reads=[b_tab, b_P[pi]], writes=[b_P[pi]])
                    state["pi"] = pi

                def s2():
                    pi = state["pi"]
                    if nslot == 0:
                        shared["ob"], shared["lb"] = orot.next(), lrot.next()
                    ob, lb = shared["ob"], shared["lb"]
                    for which, bkx in ((0, ob), (1, lb)):
                        for sp in range(nsub):
                            if gi == 0:
                                vv = V1[par][:, sp + s_, :]
                                sc = 6 + sp + s_
                                out_ap = banks[bkx][:, sp * 128:(sp + 1) * 128]
                            elif gi == 1:
                                vv = V2[par][:, sp, s_, :]
                                sc = 11 + sp * 2 + s_
                                out_ap = banks[bkx][:].rearrange("p (u r) -> p r u", r=4)[:, sp, :]
                            else:
                                vv = (V3a[par] if s_ == 0 else V3b[par])[0:nk, sp, :]
                                sc = 19 + sp * 2 + s_
                                out_ap = banks[bkx][:].rearrange("p (u r) -> p r u", r=16)[:, sp, :]
                            lhsT = vv if which == 0 else vm[0:nk, sc, :]
                            very_first = (nslot == 0 and sp == 0)
                            very_last = (nslot == 5 and sp == nsub - 1)
                            pe_mm(out_ap, lhsT, Pb[pi][0:nk, sp * nq:(sp + 1) * nq], very_first, very_last,
                                  [b_VB[par] if which == 0 else b_vm, b_P[pi]], b_bank[bkx], very_first)
                    if nslot == 5:
                        finish_unit(ob, lb, OT[:, 16 + hh, :])
                        if hh == 3:
                            ro = own_row(o)
                            dma_split("sp", OTS.rearrange("(n p) t -> p n t", p=128)[:, :, ro:ro + T], OT[:], 5,
                                      reads=[b_OT], pwrites=[b_OTS])
                st_.s1, st_.s2 = s1, s2
                return st_

            for hh in range(4):
                shared = {}
                for nslot in range(6):
                    steps.append(b_step(hh, nslot, shared))
            return steps

        steps = []
        tile_entry = {}
        ntl = len(cfg["tiles"])
        per_tile = []
        for ti in reversed(range(ntl)):
            nxt = (lambda t=ti + 1: tile_entry[t]()) if ti + 1 < ntl else None
            per_tile.append(tile_steps(ti, cfg["tiles"][ti], nxt))
        for st_list in reversed(per_tile):
            steps.extend(st_list)
        for i, st_ in enumerate(steps):
            if i == 0:
                if st_.prep:
                    st_.prep()
                st_.s1()
            if i + 1 < len(steps):
                nx = steps[i + 1]
                if nx.prep:
                    nx.prep()
                nx.s1()
            st_.s2()


    NBLK = OWN // 128
    SLOTS = sb("SLOTS", [128, NBLK * 2], I32); b_SLOTS = Buf("SLOTS")
    Asum = sb("Asum", [128, 64], BF16); b_Asum = Buf("Asum")
    Umat = sb("Umat", [128, 128], BF16)
    ECi = sb("ECi", [128, 64], I32)
    ECf = sb("ECf", [128, 64], F32)
    tokid = sb("tokid", [128, NBLK], I32)
    tokid2 = sb("tokid2", [128, NBLK], I32)
    b_c3 = Buf("c3")

    def phase3():
        S.op("pool", lambda h: h.memset(Asum[:], 0.0), writes=[b_Asum])
        S.op("pool", lambda h: h.memset(Umat[:], 1.0), writes=[b_c3])
        b_U = Buf("U")
        S.op("pool", lambda h: h.affine_select(out=Umat[:], in_=Umat[:], pattern=[[1, 128]], compare_op=ALU.is_gt,
                                               fill=0.0, base=0, channel_multiplier=-1), reads=[b_c3], writes=[b_U])
        S.op("pool", lambda h: h.iota(ECi[:], pattern=[[CAP, 64]], base=0, channel_multiplier=0), pwrites=[b_c3])
        S.op("pool", lambda h: h.iota(tokid[:], pattern=[[128, NBLK]], base=0, channel_multiplier=1), pwrites=[b_c3])
        S.op("pool", lambda h: h.iota(tokid2[:], pattern=[[128, NBLK]], base=YT_STRIDE, channel_multiplier=1), pwrites=[b_c3])
        b_EC = Buf("EC")
        S.op("pool", lambda h: h.tensor_copy(out=ECf[:], in_=ECi[:]), reads=[b_c3], writes=[b_EC])
        zt = sb("zt", [128, D], BF16); b_zt = Buf("zt")
        S.op("pool", lambda h: h.memset(zt[:], 0.0), writes=[b_zt])
        S.op("sp", lambda h: h.dma_start(out=HN[OWN:OWN + 128, :], in_=zt[:]), reads=[b_zt], pwrites=[b_HN], dma=True)
        initI = sb("initI", [128, 128, 4], I32); b_initI = Buf("initI")
        S.op("pool", lambda h: h.memset(initI[:], 0), writes=[b_initI])
        b_initI2 = Buf("initI2")
        S.op("pool", lambda h: h.memset(initI[:, :, 0:1], BIG_IDX), reads=[b_initI], writes=[b_initI2])
        S.op("pool", lambda h: h.memset(initI[:, :, 2:3], BIG_IDX), reads=[b_initI], pwrites=[b_initI2])
        S.op("sp", lambda h: h.dma_start(out=SLOTI.rearrange("(p j) c -> p j c", p=128), in_=initI[:]),
             reads=[b_initI, b_initI2], writes=[b_SLOTI], dma=True)
        g2b = sb("g2b", [128, D], F32); b_g2b = Buf("g2b")
        S.op("sp", lambda h: h.dma_start(out=g2b[:], in_=g2[0, :].partition_broadcast(128)), writes=[b_g2b], dma=True)
        brb = sb("brb", [128, 72], F32)
        wr_sb = sb("wr_sb", [128, KC, 72], F32)
        bg_sb = sb("bg_sb", [128, 32], F32)
        b_rc = Buf("rconst")
        S.op("sp", lambda h: h.dma_start(out=brb[:], in_=br[0, :].partition_broadcast(128)), writes=[b_rc], dma=True)
        S.op("sp", lambda h: h.dma_start(out=wr_sb[:].rearrange("p k c -> p (k c)"), in_=wr), pwrites=[b_rc], dma=True)
        S.op("sp", lambda h: h.dma_start(out=bg_sb[:], in_=bgT), pwrites=[b_rc], dma=True)

        xT = sb("xT3", [128, KC, T], BF16); b_xT = Buf("xT3")
        OTt = sb("OTt", [128, 20, T], BF16); b_OTt = Buf("OTt")
        GA = sb("GA", [128, 4, T], F32); b_GA = Buf("GA")
        GB = sb("GB", [128, 4, T], F32); b_GB = Buf("GB")
        MT = sb("MT", [128, KC, T], BF16); b_MT = Buf("MT")
        Hh = sb("Hh", [128, 4, D], F32); b_Hh = [Buf(f"Hh{i}") for i in range(4)]
        xres = [sb(f"xres{i}", [128, 512], F32) for i in range(4)]
        b_xres = [Buf(f"xres{i}") for i in range(4)]
        xrot = Rot(range(4))
        junk = sb("junk3", [128, D], BF16)
        hnf = sb("hnf", [128, D], F32); b_hnf = Buf("hnf")
        hnb = sb("hnb", [128, D], BF16); b_hnb = Buf("hnb")
        hlo = sb("hlo", [128, D], BF16); b_hlo = Buf("hlo")
        hiT = sb("hiT", [128, KC, 128], BF16); b_hiT = Buf("hiT")
        loT = sb("loT", [128, KC, 128], BF16); b_loT = Buf("loT")
        wr_hi = sb("wr_hi", [128, KC, 72], BF16)
        wr_lo = sb("wr_lo", [128, KC, 72], BF16)
        b_wrs = Buf("wrs")
        S.op("act", lambda h: h.copy(out=wr_hi[:], in_=wr_sb[:]), reads=[b_rc], writes=[b_wrs])
        S.op("dve", lambda h: h.tensor_tensor(out=wr_lo[:], in0=wr_sb[:], in1=wr_hi[:], op=ALU.subtract), reads=[b_rc, b_wrs], pwrites=[b_wrs])
        st = sb("st3", [128, 8], F32)
        b_ssq, b_ms, b_rstd = Buf("ssq3"), Buf("ms3"), Buf("rstd3")
        LG = sb("LG", [128, 72], F32); b_LG = Buf("LG")
        rt = sb("rt", [128, 16], F32)
        ohg = sb("ohg", [128, 8], F32)
        eg = sb("eg", [128, 8], F32)
        tmp64 = sb("tmp64", [128, 64], F32)
        lsel = sb("lsel", [128, 8], F32)
        msk = sb("msk", [128, 8], F32)
        oh1 = sb("oh1", [128, 8], F32)
        oh2 = sb("oh2", [128, 8], F32)
        A1 = sb("A1", [128, 64], F32)
        A2 = sb("A2", [128, 64], F32)
        Abf = sb("Abf", [128, 64], BF16)
        rk = sb("rk", [128, 64], F32)
        junk64 = sb("junk64", [128, 64], F32)
        sl_f = sb("sl_f", [128, 2], F32)
        sl_i = sb("sl_i", [128, 2], I32)
        info = [sb(f"info{i}", [128, 4], I32) for i in range(2)]
        bR = {n: Buf("r_" + n) for n in "mg ohg negmg se pg lsel m1 oh1 negm1 msk m2 oh2 e2 den rden w1 w2 A1 A2 Abf rk slf sli info0 info1 tmp64 A2j".split()}

        def dve(fn, reads, writes, pw=()):
            S.op("dve", fn, reads=[bR[r] if isinstance(r, str) else r for r in reads],
                 writes=[bR[w] if isinstance(w, str) else w for w in writes],
                 pwrites=[bR[w] if isinstance(w, str) else w for w in pw])

        def proj_ws(w, cg, nkg, rhs_fn, rhs_reads, bset):
            for kg in range(nkg):
                wt, b_wt = w_get()
                for cc in range(4):
                    for kk in range(4):
                        first = (kg == 0 and kk == 0)
                        pe_mm(banks[bset + cc][:], wt[:, kk, cc * 128:(cc + 1) * 128], rhs_fn(kg * 4 + kk),
                              first, (kg == nkg - 1 and kk == 3), [b_wt] + rhs_reads, b_bank[bset + cc], first)

        for _ in cfg["tiles"]:
            for fg in range(4):
                w_push([piece(w_gate, kg, fg) for kg in range(4)] + [piece(w_gate, kg, 4 + fg) for kg in range(4)]
                       + [piece(w_pa, kg, fg) for kg in range(4)] + [piece(w_pb, 0, fg)])
            for cg in range(4):
                w_push([piece(w_out, kg, cg) for kg in range(4)])
        def tail1(tb, ro, blk):
            r0 = ro + tb * 128
            S.op("sp", lambda h, tb=tb, r0=r0: h.dma_start(out=HS[r0:r0 + 128, :], in_=Hh[:, tb, :]),
                 reads=[b_Hh[tb]], pwrites=[b_HS], dma=True)
            S.op("act", lambda h, tb=tb: h.activation(out=junk[:], in_=Hh[:, tb, :], func=AF.Square, accum_out=st[:, 0:1]),
                 reads=[b_Hh[tb]], writes=[b_ssq])
            S.op("dve", lambda h: h.tensor_scalar(out=st[:, 1:2], in0=st[:, 0:1], scalar1=1.0 / D, scalar2=EPS,
                                                  op0=ALU.mult, op1=ALU.add), reads=[b_ssq], writes=[b_ms])
            S.op("pool", lambda h: h.tensor_tensor(out=st[:, 2:3], in0=st[:, 1:2], in1=neghalf[:], op=ALU.pow),
                 reads=[b_ms, b_const], writes=[b_rstd])
            S.op("dve", lambda h, tb=tb: h.scalar_tensor_tensor(out=hnf[:], in0=Hh[:, tb, :], scalar=st[:, 2:3], in1=g2b[:],
                                                               op0=ALU.mult, op1=ALU.mult),
                 reads=[b_Hh[tb], b_rstd, b_g2b], writes=[b_hnf])
            S.op("act", lambda h: h.copy(out=hnb[:], in_=hnf[:]), reads=[b_hnf], writes=[b_hnb])
            S.op("dve", lambda h: h.tensor_tensor(out=hlo[:], in0=hnf[:], in1=hnb[:], op=ALU.subtract),
                 reads=[b_hnf, b_hnb], writes=[b_hlo])
            S.op("sp", lambda h, r0=r0: h.dma_start(out=HN[r0:r0 + 128, :], in_=hnb[:]), reads=[b_hnb], pwrites=[b_HN], dma=True)

        def tail2(tb, ro, blk):
            for which, (src, b_src, dstT, b_dstT) in enumerate(((hnb, b_hnb, hiT, b_hiT), (hlo, b_hlo, loT, b_loT))):
                for half in range(2):
                    bk = which * 2 + half
                    tpv = banks[bk][:].bitcast(BF16)
                    for k8 in range(8):
                        k = half * 8 + k8
                        S.op("pe", lambda h, k=k, k8=k8, tpv=tpv, src=src: h.transpose(
                            out=tpv[:, k8 * 128:(k8 + 1) * 128], in_=src[:, k * 128:(k + 1) * 128], identity=ident[:]),
                            reads=[b_src, b_const], writes=[b_bank[bk]] if k8 == 0 else (), pwrites=() if k8 == 0 else [b_bank[bk]])
                    evac_copy(dstT[:, half * 8:(half + 1) * 8, :], tpv.rearrange("p (k t) -> p k t", k=8),
                              reads=[b_bank[bk]], writes=[b_dstT] if half == 0 else (), pwrites=() if half == 0 else [b_dstT], eng="act")
            lb_ = 4
            terms = [(hiT, b_hiT, wr_hi), (loT, b_loT, wr_hi), (hiT, b_hiT, wr_lo)]
            nmm = 0
            for (aT, b_aT, wmat) in terms:
                for k in range(KC):
                    pe_mm(banks[lb_][:, 0:72], aT[:, k, :], wmat[:, k, :], nmm == 0, nmm == 3 * KC - 1, [b_aT, b_wrs], b_bank[lb_], nmm == 0)
                    nmm += 1
            dve(lambda h: h.tensor_tensor(out=LG[:], in0=banks[lb_][:, 0:72], in1=brb[:], op=ALU.add), [b_bank[lb_], b_rc], [b_LG])
            dve(lambda h: h.tensor_reduce(out=rt[:, 0:1], in_=LG[:, 0:8], axis=AX.X, op=ALU.max), [b_LG], ["mg"])
            dve(lambda h: h.tensor_scalar(out=ohg[:], in0=LG[:, 0:8], scalar1=rt[:, 0:1], scalar2=None, op0=ALU.is_equal), [b_LG, "mg"], ["ohg"])
            dve(lambda h: h.tensor_scalar(out=rt[:, 1:2], in0=rt[:, 0:1], scalar1=-1.0, scalar2=None, op0=ALU.mult), ["mg"], ["negmg"])
            S.op("act", lambda h: h.activation(out=eg[:], in_=LG[:, 0:8], func=AF.Exp, bias=rt[:, 1:2], accum_out=rt[:, 2:3]),
                 reads=[b_LG, bR["negmg"]], writes=[bR["se"]])
            dve(lambda h: h.reciprocal(out=rt[:, 3:4], in_=rt[:, 2:3]), ["se"], ["pg"])
            dve(lambda h: h.tensor_tensor(out=tmp64[:].rearrange("p (g j) -> p g j", g=8),
                                          in0=LG[:, 8:72].rearrange("p (g j) -> p g j", g=8),
                                          in1=ohg[:].unsqueeze(2).broadcast_to([128, 8, 8]), op=ALU.mult), [b_LG, "ohg"], ["tmp64"])
            dve(lambda h: h.tensor_reduce(out=lsel[:], in_=tmp64[:].rearrange("p (g j) -> p j g", g=8), axis=AX.X, op=ALU.add),
                ["tmp64"], ["lsel"])
            dve(lambda h: h.tensor_reduce(out=rt[:, 4:5], in_=lsel[:], axis=AX.X, op=ALU.max), ["lsel"], ["m1"])
            dve(lambda h: h.tensor_scalar(out=oh1[:], in0=lsel[:], scalar1=rt[:, 4:5], scalar2=None, op0=ALU.is_equal), ["lsel", "m1"], ["oh1"])
            dve(lambda h: h.tensor_scalar(out=rt[:, 5:6], in0=rt[:, 4:5], scalar1=-1.0, scalar2=None, op0=ALU.mult), ["m1"], ["negm1"])
            dve(lambda h: h.scalar_tensor_tensor(out=msk[:], in0=oh1[:], scalar=-1e30, in1=lsel[:], op0=ALU.mult, op1=ALU.add),
                ["oh1", "lsel"], ["msk"])
            dve(lambda h: h.tensor_reduce(out=rt[:, 6:7], in_=msk[:], axis=AX.X, op=ALU.max), ["msk"], ["m2"])
            dve(lambda h: h.tensor_scalar(out=oh2[:], in0=msk[:], scalar1=rt[:, 6:7], scalar2=None, op0=ALU.is_equal), ["msk", "m2"], ["oh2"])
            S.op("act", lambda h: h.activation(out=rt[:, 7:8], in_=rt[:, 6:7], func=AF.Exp, bias=rt[:, 5:6]),
                 reads=[bR["m2"], bR["negm1"]], writes=[bR["e2"]])
            dve(lambda h: h.tensor_scalar(out=rt[:, 8:9], in0=rt[:, 7:8], scalar1=1.0, scalar2=None, op0=ALU.add), ["e2"], ["den"])
            dve(lambda h: h.reciprocal(out=rt[:, 9:10], in_=rt[:, 8:9]), ["den"], ["rden"])
            dve(lambda h: h.tensor_tensor(out=rt[:, 10:11], in0=rt[:, 9:10], in1=rt[:, 3:4], op=ALU.mult), ["rden", "pg"], ["w1"])
            dve(lambda h: h.tensor_tensor(out=rt[:, 11:12], in0=rt[:, 10:11], in1=rt[:, 7:8], op=ALU.mult), ["w1", "e2"], ["w2"])
            dve(lambda h: h.tensor_tensor(out=A1[:].rearrange("p (g j) -> p g j", g=8),
                                          in0=ohg[:].unsqueeze(2).broadcast_to([128, 8, 8]),
                                          in1=oh1[:].unsqueeze(1).broadcast_to([128, 8, 8]), op=ALU.mult), ["ohg", "oh1"], ["A1"])
            dve(lambda h: h.tensor_tensor(out=A2[:].rearrange("p (g j) -> p g j", g=8),
                                          in0=ohg[:].unsqueeze(2).broadcast_to([128, 8, 8]),
                                          in1=oh2[:].unsqueeze(1).broadcast_to([128, 8, 8]), op=ALU.mult), ["ohg", "oh2"], ["A2"])
            dve(lambda h: h.tensor_tensor(out=Abf[:], in0=A1[:], in1=A2[:], op=ALU.add), ["A1", "A2"], ["Abf"])

        def tail3(tb, ro, blk):
            rb_ = 5
            pe_mm(banks[rb_][:, 0:64], Umat[:], Abf[:], True, False, [b_U, bR["Abf"]], b_bank[rb_], True)
            pe_mm(banks[rb_][:, 0:64], ones_bf[:], Asum[:], False, True, [b_const, b_Asum], b_bank[rb_], False)
            dve(lambda h: h.tensor_tensor(out=rk[:], in0=banks[rb_][:, 0:64], in1=ECf[:], op=ALU.add), [b_bank[rb_], b_EC], ["rk"])
            S.op("pool", lambda h: h.tensor_tensor(out=Asum[:], in0=Asum[:], in1=Abf[:], op=ALU.add),
                 reads=[bR["Abf"], b_Asum], writes=[b_Asum])
            dve(lambda h: h.tensor_tensor(out=tmp64[:], in0=rk[:], in1=A1[:], op=ALU.mult), ["rk", "A1"], ["tmp64"])
            dve(lambda h: h.tensor_reduce(out=sl_f[:, 0:1], in_=tmp64[:], axis=AX.X, op=ALU.add), ["tmp64"], [], ["slf"])
            dve(lambda h: h.tensor_tensor(out=junk64[:], in0=rk[:], in1=A2[:], op=ALU.mult), ["rk", "A2"], ["A2j"])
            dve(lambda h: h.tensor_reduce(out=sl_f[:, 1:2], in_=junk64[:], axis=AX.X, op=ALU.add), ["A2j"], [], ["slf"])
            dve(lambda h: h.tensor_copy(out=sl_i[:], in_=sl_f[:]), ["slf"], ["sli"])
            dve(lambda h, blk=blk: h.tensor_copy(out=SLOTS[:, blk * 2:blk * 2 + 2], in_=sl_i[:]), ["sli"], [], [b_SLOTS])
            for kx in range(2):
                nm = f"info{kx}"
                dve(lambda h, kx=kx, blk=blk: h.tensor_copy(out=info[kx][:, 0:1], in_=tokid[:, blk:blk + 1]), [b_c3], [nm])
                dve(lambda h, kx=kx, blk=blk: h.tensor_copy(out=info[kx][:, 2:3], in_=(tokid if kx == 0 else tokid2)[:, blk:blk + 1]),
                    [b_c3, nm], [nm])
                dve(lambda h, kx=kx: h.tensor_copy(out=info[kx][:, 1:2].bitcast(F32), in_=rt[:, 10 + kx:11 + kx]),
                    ["w1", "w2", nm], [nm])
                S.op("pool", lambda h, kx=kx: h.indirect_dma_start(
                    out=SLOTI, out_offset=bass.IndirectOffsetOnAxis(ap=sl_i[:, kx:kx + 1], axis=0),
                    in_=info[kx][:], in_offset=None),
                    reads=[bR[nm], bR["sli"], b_SLOTI], pwrites=[b_SLOTI], dma=True)

        pending = []
        done = set()

        def run_slot(slot):
            if not pending:
                return
            for part, fn_, b in ((3, tail3, slot - 2), (2, tail2, slot - 1), (1, tail1, slot)):
                if 0 <= b < 4:
                    key = (pending[b][1], b, part)
                    if key in done:
                        continue
                    done.add(key)
                    fn_(*pending[b])

        blk = 0
        for ti, o in enumerate(cfg["tiles"]):
            ro = own_row(o)
            dma_split("sp", xT[:], XNT.rearrange("(k p) t -> p k t", p=128)[:, :, o:o + T], 4, reads=[b_XNT], writes=[b_xT])
            dma_split("sp", OTt[:], OTS.rearrange("(n p) t -> p n t", p=128)[:, :, ro:ro + T], 5, reads=[b_OTS], writes=[b_OTt])
            for fg in range(4):
                proj_ws(w_gate, fg, 4, lambda k: xT[:, k, :], [b_xT], 0)
                for cc in range(4):
                    c_ = fg * 4 + cc
                    S.op("act", lambda h, cc=cc, c_=c_: h.activation(out=GA[:, cc, :], in_=banks[cc][:], func=AF.Sigmoid,
                                                                     bias=bg_sb[:, c_:c_ + 1]),
                         reads=[b_bank[cc], b_rc], writes=[b_GA] if cc == 0 else (), pwrites=() if cc == 0 else [b_GA])
                proj_ws(w_gate, 4 + fg, 4, lambda k: xT[:, k, :], [b_xT], 4)
                for cc in range(4):
                    c_ = 16 + fg * 4 + cc
                    S.op("act", lambda h, cc=cc, c_=c_: h.activation(out=GB[:, cc, :], in_=banks[4 + cc][:], func=AF.Sigmoid,
                                                                     bias=bg_sb[:, c_:c_ + 1]),
                         reads=[b_bank[4 + cc], b_rc], writes=[b_GB] if cc == 0 else (), pwrites=() if cc == 0 else [b_GB])
                proj_ws(w_pa, fg, 4, lambda k: OTt[:, k, :], [b_OTt], 0)
                for cc in range(4):
                    S.op("dve", lambda h, cc=cc: h.tensor_tensor(out=GA[:, cc, :], in0=banks[cc][:], in1=GA[:, cc, :], op=ALU.mult),
                         reads=[b_bank[cc], b_GA], pwrites=[b_GA])
                proj_ws(w_pb, fg, 1, lambda k: OTt[:, 16 + k, :], [b_OTt], 4)
                for cc in range(4):
                    S.op("dve", lambda h, cc=cc: h.tensor_tensor(out=GB[:, cc, :], in0=banks[4 + cc][:], in1=GB[:, cc, :], op=ALU.mult),
                         reads=[b_bank[4 + cc], b_GB], pwrites=[b_GB])
                S.op("pool", lambda h, fg=fg: h.tensor_tensor(out=MT[:, fg * 4:(fg + 1) * 4, :], in0=GA[:], in1=GB[:], op=ALU.add),
                     reads=[b_GA, b_GB], writes=[b_MT] if fg == 0 else (), pwrites=() if fg == 0 else [b_MT])
                run_slot(fg)
            for cg in range(4):
                bset = (cg % 2) * 4
                for kg in range(4):
                    wt, b_wt = w_get()
                    for tb in range(4):
                        for kk in range(4):
                            first = (kg == 0 and kk == 0)
                            pe_mm(banks[bset + tb][:], MT[:, kg * 4 + kk, tb * 128:(tb + 1) * 128], wt[:, kk, :],
                                  first, (kg == 3 and kk == 3), [b_wt, b_MT], b_bank[bset + tb], first)
                for tb in range(4):
                    xi = xrot.next()
                    r0 = o + tb * 128
                    S.op("sp", lambda h, xi=xi, r0=r0, cg=cg: h.dma_start(out=xres[xi][:], in_=xin[r0:r0 + 128, cg * 512:(cg + 1) * 512]),
                         writes=[b_xres[xi]], dma=True)
                    S.op("dve", lambda h, xi=xi, tb=tb, cg=cg, bset=bset: h.tensor_tensor(
                        out=Hh[:, tb, cg * 512:(cg + 1) * 512], in0=banks[bset + tb][:], in1=xres[xi][:], op=ALU.add),
                        reads=[b_bank[bset + tb], b_xres[xi]], writes=[b_Hh[tb]] if cg == 0 else (), pwrites=() if cg == 0 else [b_Hh[tb]])
                run_slot(4 + cg)
            pending = [(tb, ro, blk + tb) for tb in range(4)]
            blk += 4
        for slot in range(8):
            run_slot(slot)

    def phase4():
        si = [sb(f"si{i}", [128, 4], I32) for i in range(4)]; b_si = [Buf(f"si{i}") for i in range(4)]
        X = [sb(f"X{i}", [128, D], BF16) for i in range(4)]; b_X = [Buf(f"X{i}") for i in range(4)]
        XT = [sb(f"XT{i}", [128, KC, 256], BF16) for i in range(2)]; b_XTe = [Buf(f"XT{i}") for i in range(2)]
        S1 = sb("S1", [128, 1024], F32); b_S1 = Buf("S1")
        HT = [sb(f"HT{i}", [128, 4, 256], BF16) for i in range(2)]; b_HT = [Buf(f"HT{i}") for i in range(2)]
        Y = [sb(f"Y{i}", [128, D], BF16) for i in range(4)]; b_Y = [Buf(f"Y{i}") for i in range(4)]
        yev = Rot(("act", "dve"))
        regs = {}

        def bound_reg(h):
            if "bc" not in regs:
                bc_reg = h.alloc_register("bc_reg")
                h.reg_mov(bc_reg, 2 * YT_STRIDE - 1)
                regs["bc"] = bc_reg
            return regs["bc"]
        for i4 in range(4):
            S.op("dve", lambda h, i4=i4: h.memset(X[i4][:], 0.0), writes=[b_X[i4]])
        for e in cfg["experts"]:
            w_push([piece(w1, e * 4 + kg, 0) for kg in range(4)] + [piece(w3, e * 4 + kg, 0) for kg in range(4)]
                   + [piece(w2, e, cg) for cg in range(4)])
        for ei, e in enumerate(cfg["experts"]):
            xt, b_xt = XT[ei % 2], b_XTe[ei % 2]
            sis = []
            for sb_ in range(2):
                i4 = (ei % 2) * 2 + sb_
                sis.append(i4)
                r0 = e * CAP + sb_ * 128
                S.op("sp", lambda h, i4=i4, r0=r0: h.dma_start(out=si[i4][:], in_=SLOTI[r0:r0 + 128, :]),
                     reads=[b_SLOTI], writes=[b_si[i4]], dma=True)
                S.op("pool", lambda h, i4=i4: h.indirect_dma_start(
                    out=X[i4][:], out_offset=None, in_=HN, in_offset=bass.IndirectOffsetOnAxis(ap=si[i4][:, 0:1], axis=0),
                    bounds_check=bound_reg(h), oob_is_err=False),
                    reads=[b_si[i4], b_HN], writes=[b_X[i4]], dma=True)
                for half in range(2):
                    bk = 4 + (2 * sb_ + half) % 4
                    tpv = banks[bk][:].bitcast(BF16)
                    for k8 in range(8):
                        k = half * 8 + k8
                        S.op("pe", lambda h, i4=i4, k=k, k8=k8, tpv=tpv: h.transpose(
                            out=tpv[:, k8 * 128:(k8 + 1) * 128], in_=X[i4][:, k * 128:(k + 1) * 128], identity=ident[:]),
                            reads=[b_X[i4], b_const], writes=[b_bank[bk]] if k8 == 0 else (), pwrites=() if k8 == 0 else [b_bank[bk]])
                    first_w = (sb_ == 0 and half == 0)
                    evac_copy(xt[:, half * 8:(half + 1) * 8, sb_ * 128:(sb_ + 1) * 128], tpv.rearrange("p (k t) -> p k t", k=8),
                              reads=[b_bank[bk]], writes=[b_xt] if first_w else (), pwrites=() if first_w else [b_xt])
            for which in range(2):
                for kg in range(4):
                    wt, b_wt = w_get()
                    for cc in range(4):
                        bk = which * 2 + cc // 2
                        for kk in range(4):
                            vfirst = (kg == 0 and kk == 0 and cc % 2 == 0)
                            vlast = (kg == 3 and kk == 3 and cc % 2 == 1)
                            pe_mm(banks[bk][:, (cc % 2) * 256:(cc % 2 + 1) * 256], wt[:, kk, cc * 128:(cc + 1) * 128],
                                  xt[:, kg * 4 + kk, :], vfirst, vlast, [b_wt, b_xt], b_bank[bk], vfirst)
            ht, b_ht = HT[ei % 2], b_HT[ei % 2]
            for hb_ in range(2):
                S.op("act", lambda h, hb_=hb_: h.activation(out=S1[:, hb_ * 512:(hb_ + 1) * 512], in_=banks[hb_][:], func=AF.Silu),
                     reads=[b_bank[hb_]], writes=[b_S1] if hb_ == 0 else (), pwrites=() if hb_ == 0 else [b_S1])
            for hb_ in range(2):
                S.op("dve", lambda h, hb_=hb_, ht=ht: h.tensor_tensor(
                    out=ht[:, hb_ * 2:(hb_ + 1) * 2, :], in0=banks[2 + hb_][:].rearrange("p (c t) -> p c t", c=2),
                    in1=S1[:, hb_ * 512:(hb_ + 1) * 512].rearrange("p (c t) -> p c t", c=2), op=ALU.mult),
                    reads=[b_bank[2 + hb_], b_S1], writes=[b_ht] if hb_ == 0 else (), pwrites=() if hb_ == 0 else [b_ht])
            for cg in range(4):
                wt, b_wt = w_get()
                for sb_ in range(2):
                    i4 = sis[sb_]
                    bk = 4 + (2 * cg + sb_) % 4
                    for kk in range(4):
                        pe_mm(banks[bk][:], ht[:, kk, sb_ * 128:(sb_ + 1) * 128], wt[:, kk, :], kk == 0, kk == 3,
                              [b_wt, b_ht], b_bank[bk], kk == 0)
                    wcol = si[i4][:, 1:2].bitcast(F32)
                    ye = yev.next()
                    wr_ = dict(writes=[b_Y[i4]] if cg == 0 else (), pwrites=() if cg == 0 else [b_Y[i4]])
                    if ye == "act":
                        S.op("act", lambda h, i4=i4, cg=cg, bk=bk, wcol=wcol: h.activation(
                            out=Y[i4][:, cg * 512:(cg + 1) * 512], in_=banks[bk][:], func=AF.Copy, scale=wcol),
                            reads=[b_bank[bk], b_si[i4]], **wr_)
                    else:
                        S.op("dve", lambda h, i4=i4, cg=cg, bk=bk, wcol=wcol: h.tensor_scalar(
                            out=Y[i4][:, cg * 512:(cg + 1) * 512], in0=banks[bk][:], scalar1=wcol, scalar2=None, op0=ALU.mult),
                            reads=[b_bank[bk], b_si[i4]], **wr_)
            for sb_ in range(2):
                i4 = sis[sb_]
                r0 = e * CAP + sb_ * 128
                S.op("pool", lambda h, i4=i4: h.indirect_dma_start(
                    out=YS, out_offset=bass.IndirectOffsetOnAxis(ap=si[i4][:, 2:3], axis=0), in_=Y[i4][:], in_offset=None,
                    bounds_check=bound_reg(h), oob_is_err=False),
                    reads=[b_Y[i4], b_si[i4]], pwrites=[b_YS], dma=True)

    def phase5():
        gfb = sb("gfb", [128, D], F32); b_gfb = Buf("gfb")
        S.op("sp", lambda h: h.dma_start(out=gfb[:], in_=gf[0, :].partition_broadcast(128)), writes=[b_gfb], dma=True)
        NB5 = 4
        hb = [sb(f"hb{i}", [128, D], F32) for i in range(NB5)]; b_hb = [Buf(f"hb{i}") for i in range(NB5)]
        yg = [sb(f"yg{i}", [128, D], BF16) for i in range(2 * NB5)]; b_yg = [Buf(f"yg{i}") for i in range(2 * NB5)]
        ob = [sb(f"ob{i}", [128, D], F32) for i in range(NB5)]; b_ob = [Buf(f"ob{i}") for i in range(NB5)]
        junk = sb("junk5", [128, D], BF16)
        st = [sb(f"st5{i}", [128, 4], F32) for i in range(NB5)]
        b_ssq = [Buf(f"ssq5{i}") for i in range(NB5)]; b_ms = [Buf(f"ms5{i}") for i in range(NB5)]; b_rstd = [Buf(f"rstd5{i}") for i in range(NB5)]
        nb = sum(1 for _ in cfg["tiles"]) * 4
        blocks = []
        for o in cfg["tiles"]:
            for tb in range(4):
                blocks.append(own_row(o) + tb * 128)
        def stage_a(bi):
            r0 = blocks[bi]
            i = bi % NB5
            S.op("sp", lambda h: h.dma_start(out=hb[i][:], in_=HS[r0:r0 + 128, :]), reads=[b_HS], writes=[b_hb[i]], dma=True)
            for kx in range(2):
                j = i * 2 + kx
                S.op("sp", lambda h, j=j, kx=kx: h.dma_start(out=yg[j][:], in_=YS[kx * YT_STRIDE + r0:kx * YT_STRIDE + r0 + 128, :]),
                     reads=[b_YS], writes=[b_yg[j]], dma=True)
            S.op("dve", lambda h: h.tensor_tensor(out=hb[i][:], in0=hb[i][:], in1=yg[i * 2][:], op=ALU.add),
                 reads=[b_hb[i], b_yg[i * 2]], writes=[b_hb[i]])
            S.op("dve", lambda h: h.tensor_tensor(out=hb[i][:], in0=hb[i][:], in1=yg[i * 2 + 1][:], op=ALU.add),
                 reads=[b_hb[i], b_yg[i * 2 + 1]], writes=[b_hb[i]])
            S.op("act", lambda h: h.activation(out=junk[:], in_=hb[i][:], func=AF.Square, accum_out=st[i][:, 0:1]),
                 reads=[b_hb[i]], writes=[b_ssq[i]])

        def stage_b(bi):
            r0 = blocks[bi]
            i = bi % NB5
            S.op("dve", lambda h: h.tensor_scalar(out=st[i][:, 1:2], in0=st[i][:, 0:1], scalar1=1.0 / D, scalar2=EPS,
                                                  op0=ALU.mult, op1=ALU.add), reads=[b_ssq[i]], writes=[b_ms[i]])
            S.op("pool", lambda h: h.tensor_tensor(out=st[i][:, 2:3], in0=st[i][:, 1:2], in1=neghalf[:], op=ALU.pow),
                 reads=[b_ms[i], b_const], writes=[b_rstd[i]])
            S.op("dve", lambda h: h.scalar_tensor_tensor(out=ob[i][:], in0=hb[i][:], scalar=st[i][:, 2:3], in1=gfb[:],
                                                         op0=ALU.mult, op1=ALU.mult),
                 reads=[b_hb[i], b_rstd[i], b_gfb], writes=[b_ob[i]])
            S.op("pool", lambda h: h.dma_start(out=yout[r0:r0 + 128, :], in_=ob[i][:]), reads=[b_ob[i]], pwrites=[b_yout], dma=True)

        LAG = 3
        for bi in range(min(LAG, len(blocks))):
            stage_a(bi)
        for bi in range(len(blocks)):
            if bi + LAG < len(blocks):
                stage_a(bi + LAG)
            stage_b(bi)

    if 1 in cfg["phases"]:
        run_phase(phase1, 8)
    if 2 in cfg["phases"]:
        run_phase(phase2)
    if 3 in cfg["phases"]:
        run_phase(phase3)
    if 4 in cfg["phases"]:
        run_phase(phase4, 16)
    if 5 in cfg["phases"]:
        run_phase(phase5)
    S.barrier()
    emit_program(nc, S, es)
    es.close()
    return nc


def _bf16(a):
    return np.ascontiguousarray(a.astype(ml_dtypes.bfloat16))


def position_tables():
    k = np.arange(128)[:, None].astype(np.float64)
    sl_a = np.power(2.0, -8.0 * (np.arange(16) + 1) / 16)
    sl_b = np.power(2.0, -8.0 * (np.arange(12) + 1) / 12)
    eA = np.zeros((128, 4, 3, 4, 128))
    q = np.arange(128)[None, :].astype(np.float64)
    for kvh in range(4):
        for s in range(3):
            rel = (128 * s - 128 + k) - q
            for g in range(4):
                e = np.exp(-sl_a[kvh * 4 + g] * np.abs(rel))
                eA[:, kvh, s, g, :] = np.where(np.abs(rel) <= 128, e, 0.0)
    eB01 = np.zeros((128, 2, 4, 2, 128))
    for gi, r in ((0, 1), (1, 4)):
        for hh in range(4):
            for s in range(2):
                rel = (k + 128 * s - 64) - q
                e = np.exp(-sl_b[gi * 4 + hh] * r * np.abs(rel))
                eB01[:, gi, hh, s, :] = np.where(np.abs(rel) <= 64, e, 0.0)
    eB2 = np.zeros((128, 4, 2, 32))
    q32 = np.arange(32)[None, :].astype(np.float64)
    for hh in range(4):
        for s in range(2):
            rel = (k + 128 * s - 64) - q32
            e = np.exp(-sl_b[8 + hh] * 16 * np.abs(rel))
            e = np.where(np.abs(rel) <= 64, e, 0.0)
            if s == 1:
                e = np.where(k < 32, e, 0.0)
            eB2[:, hh, s, :] = e
    eB = np.concatenate([eB01.reshape(128, -1), eB2.reshape(128, -1)], axis=1)
    return _bf16(eA.reshape(128, -1)), _bf16(eB)


def valid_mask_table(valid_ext, tiles):
    p = np.arange(128)
    out = np.zeros((len(tiles), 128, 51), np.float32)
    for ti, o in enumerate(tiles):
        cols = []
        for j in range(6):
            cols.append(valid_ext[o - 128 + 128 * j + p])
        for j in range(5):
            cols.append(valid_ext[o - 64 + 128 * j + p])
        for ph in range(4):
            for s in range(2):
                cols.append(valid_ext[o - 256 + ph + 512 * s + 4 * p])
        for ph in range(16):
            for s in range(2):
                tok = o - 1024 + ph + 2048 * s + 16 * p
                if s == 0:
                    cols.append(valid_ext[tok])
                else:
                    cols.append(np.where(p < 32, valid_ext[np.minimum(tok, len(valid_ext) - 1)], 0.0))
        out[ti] = np.stack(cols, axis=1)
    full = np.repeat(out[:, :, :, None], 128, axis=3)
    return _bf16(full.reshape(len(tiles) * 128, 51 * 128))


def make_shared_inputs(inp):
    f = lambda a: np.ascontiguousarray(np.asarray(a, dtype=np.float32))
    eA, eB = position_tables()
    w_rg = f(inp["w_router_group"])[0]
    w_re = f(inp["w_router_expert"])[0]
    wrm = np.concatenate([w_rg, w_re.transpose(1, 0, 2).reshape(D, 64)], axis=1)
    wrm = wrm.reshape(KC, 128, 72).transpose(1, 0, 2).reshape(128, KC * 72)
    brm = np.concatenate([f(inp["b_router_group"])[0], f(inp["b_router_expert"])[0].reshape(64)])[None, :]
    return dict(
        w_in=f(inp["w_in"])[0], w_gate=f(inp["w_gate"])[0], w_pa=f(inp["w_proj_a"])[0], w_pb=f(inp["w_proj_b"])[0],
        w_out=f(inp["w_out"])[0],
        w1=f(inp["w_expert_gate"])[0].reshape(N_EXP * D, D_FF), w3=f(inp["w_expert_up"])[0].reshape(N_EXP * D, D_FF),
        w2=f(inp["w_expert_down"])[0].reshape(N_EXP * D_FF, D),
        g1=f(inp["norm1"]).reshape(1, D), g2=f(inp["norm2"]).reshape(1, D), gf=f(inp["norm_final"]).reshape(1, D),
        bgT=np.ascontiguousarray(f(inp["b_gate"])[0].reshape(32, 128).T), wr=np.ascontiguousarray(wrm), br=f(brm),
        sink=f(inp["attn_sink"]).reshape(1, 16), eA=eA, eB=eB,
    )


def make_core_inputs(inp, c, tiles):
    xp = np.asarray(inp["x_prompt"], dtype=np.float32)[0]
    xs = np.asarray(inp["x_sample"], dtype=np.float32)[c // 2]
    xin = np.zeros((EXT, D), np.float32)
    valid = np.zeros((EXT,), np.float32)
    p0 = 1024 * c - HALO
    lo, hi = max(p0, 0), min(p0 + P_EXT, xp.shape[0])
    xin[lo - p0:hi - p0] = xp[lo:hi]
    valid[lo - p0:hi - p0] = 1.0
    seq = xs if c % 2 == 0 else xs[::-1]
    n = S_OWN + HALO
    xin[P_EXT + HALO:P_EXT + HALO + n] = seq[0:n]
    valid[P_EXT + HALO:P_EXT + HALO + n] = 1.0
    return dict(xin=xin, vmask=valid_mask_table(valid, tiles))


_NC_CACHE = {}


def kernel(**inputs):
    cfg = default_cfg()
    if "nc" not in _NC_CACHE:
        _NC_CACHE["nc"] = build(cfg)
    nc = _NC_CACHE["nc"]
    shared = make_shared_inputs(inputs)
    in_maps = []
    for c in range(NCORES):
        m = dict(shared)
        m.update(make_core_inputs(inputs, c, cfg["tiles"]))
        in_maps.append(m)
    res = run_bass_kernel_spmd(nc, in_maps, core_ids=list(range(NCORES)))
    yp = np.zeros((1, 8192, D), np.float32)
    ys = np.zeros((4, 4096, D), np.float32)
    for c in range(NCORES):
        y = np.asarray(res.results[c]["yout"])
        yp[0, 1024 * c:1024 * c + 1024] = y[:P_OWN]
        ys[c // 2, 2048 * (c % 2):2048 * (c % 2) + 2048] = y[P_OWN:] if c % 2 == 0 else y[P_OWN:][::-1]
    return (yp, ys)
```
